# Optimizing a Trainium2 kernel written in Bass

```python
import jax, jax.numpy as jnp
from jax import lax
import numpy as np

D_MODEL = 1024
BATCH = 16
SEQ = 4096
DEPTH = 2

POOL_WIDTH = D_MODEL // 2
POOL_WINDOWS = (2, 4, 8, 16)
N_POOL_GROUPS = len(POOL_WINDOWS)
POOL_GROUP = POOL_WIDTH // N_POOL_GROUPS
ATTN_WIDTH = D_MODEL - POOL_WIDTH
HEAD_DIM = 64
N_HEADS = ATTN_WIDTH // HEAD_DIM
N_KV_GROUPS = 2
HEADS_PER_GROUP = N_HEADS // N_KV_GROUPS
KV_WIDTH = N_KV_GROUPS * HEAD_DIM
N_BRANCHES = 3
CMP_LEN = 32
CMP_STRIDE = 16
CMP_HIDDEN = 2 * HEAD_DIM
SEL_BLOCK = 64
SEL_TOPK = 16
WINDOW = 512
Q_CHUNK = 32
D_FF = 2816
CONV_WIDTH = 3
EPS = 1e-6
NEG_INF = -1e30
FORCE_SCORE = 1e6
IN_SIZES = [POOL_WIDTH, ATTN_WIDTH] + [KV_WIDTH] * (2 * N_BRANCHES) + [N_HEADS * N_BRANCHES]
IN_WIDTH = sum(IN_SIZES)
SPLIT_POINTS = [int(v) for v in np.cumsum(IN_SIZES)[:-1]]

kernel_name = 'hybrid_pool_nsa_convffn'


def rms_norm(x, g):
    xf = x.astype(jnp.float32)
    y = xf * lax.rsqrt(jnp.mean(xf * xf, axis=-1, keepdims=True) + EPS)
    return (y * g.astype(jnp.float32)).astype(x.dtype)


def masked_softmax(s, mask):
    return jax.nn.softmax(jnp.where(mask, s, NEG_INF), axis=-1) * mask


def pool_mixer(u, w_pool, s_pool):
    B, T, _ = u.shape
    uf = u.astype(jnp.float32).reshape(B, T, N_POOL_GROUPS, POOL_GROUP)
    csum = jnp.concatenate([jnp.zeros_like(uf[:, :1]), jnp.cumsum(uf, axis=1)], axis=1)
    pos = jnp.arange(T)
    groups = []
    for gi, w in enumerate(POOL_WINDOWS):
        c = csum[:, :, gi]
        lo = jnp.maximum(pos + 1 - w, 0)
        cnt = jnp.minimum(pos + 1, w).astype(jnp.float32)[None, :, None]
        groups.append((c[:, 1:] - c[:, lo]) / cnt)
    pooled = jnp.stack(groups, axis=2) - uf
    mixed = jnp.einsum('btpc,pcd->btpd', pooled, w_pool.astype(jnp.float32))
    return (mixed.reshape(B, T, POOL_WIDTH) * s_pool.astype(jnp.float32)).astype(u.dtype)


def compress(a, pe, w1, w2):
    B, T = a.shape[0], a.shape[1]
    n_cmp = (T - CMP_LEN) // CMP_STRIDE + 1
    idx = jnp.arange(n_cmp)[:, None] * CMP_STRIDE + jnp.arange(CMP_LEN)[None, :]
    blocks = a[:, idx] + pe[None, None, :, None, :]
    blocks = blocks.transpose(0, 1, 3, 2, 4).reshape(B, n_cmp, N_KV_GROUPS, CMP_LEN * HEAD_DIM)
    return jax.nn.gelu(blocks @ w1) @ w2


def nsa_attention(q, k_cmp, v_cmp, k_sel, v_sel, k_win, v_win, gates):
    B, T = q.shape[0], q.shape[1]
    n_cmp = k_cmp.shape[1]
    n_sel = T // SEL_BLOCK
    topk = min(SEL_TOPK, n_sel)
    scale = HEAD_DIM ** -0.5
    cmp_start = jnp.arange(n_cmp) * CMP_STRIDE
    cmp_end = cmp_start + CMP_LEN - 1
    blk = jnp.arange(n_sel)
    blk_start = blk * SEL_BLOCK
    overlap = ((cmp_start[:, None] < blk_start[None, :] + SEL_BLOCK)
               & (cmp_end[:, None] >= blk_start[None, :])).astype(jnp.float32)

    def to_blocks(a):
        a = a.reshape(B, n_sel, SEL_BLOCK, N_KV_GROUPS, HEAD_DIM).transpose(0, 3, 1, 2, 4)
        return a.reshape(B * N_KV_GROUPS * n_sel, SEL_BLOCK, HEAD_DIM)

    kb, vb = to_blocks(k_sel), to_blocks(v_sel)
    base = (jnp.arange(B)[:, None] * N_KV_GROUPS + jnp.arange(N_KV_GROUPS)[None, :]) * n_sel
    pad = ((0, 0), (WINDOW, 0), (0, 0), (0, 0))
    kw_pad, vw_pad = jnp.pad(k_win, pad), jnp.pad(v_win, pad)
    in_blk = jnp.arange(SEL_BLOCK)
    win_off = jnp.arange(WINDOW + Q_CHUNK) - WINDOW

    def one_chunk(start):
        t = start + jnp.arange(Q_CHUNK)
        qc = lax.dynamic_slice_in_dim(q, start, Q_CHUNK, axis=1)
        gc = lax.dynamic_slice_in_dim(gates, start, Q_CHUNK, axis=1)
        s_c = jnp.einsum('bqgrd,bngd->bgrqn', qc, k_cmp).astype(jnp.float32) * scale
        p_c = masked_softmax(s_c, cmp_end[None, :] <= t[:, None])
        o_c = jnp.einsum('bgrqn,bngd->bqgrd', p_c.astype(v_cmp.dtype), v_cmp)
        imp = jnp.einsum('bgrqn,nj->bgqj', p_c, overlap)
        cur = t // SEL_BLOCK
        forced = (blk[None, :] == 0) | (blk[None, :] == cur[:, None]) | (blk[None, :] == cur[:, None] - 1)
        imp = jnp.where(forced, FORCE_SCORE, imp)
        imp = jnp.where(blk_start[None, :] <= t[:, None], imp, NEG_INF)
        _, idx = lax.top_k(imp, topk)
        flat = base[:, :, None, None] + idx
        kg = jnp.take(kb, flat, axis=0)
        vg = jnp.take(vb, flat, axis=0)
        kpos = idx[..., None] * SEL_BLOCK + in_blk
        m_s = (kpos <= t[None, None, :, None, None]).reshape(B, N_KV_GROUPS, 1, Q_CHUNK, topk * SEL_BLOCK)
        s_s = jnp.einsum('bqgrd,bgqksd->bgrqks', qc, kg).astype(jnp.float32) * scale
        p_s = masked_softmax(s_s.reshape(B, N_KV_GROUPS, HEADS_PER_GROUP, Q_CHUNK, topk * SEL_BLOCK), m_s)
        p_s = p_s.reshape(s_s.shape)
        o_s = jnp.einsum('bgrqks,bgqksd->bqgrd', p_s.astype(vg.dtype), vg)
        kw = lax.dynamic_slice_in_dim(kw_pad, start, WINDOW + Q_CHUNK, axis=1)
        vw = lax.dynamic_slice_in_dim(vw_pad, start, WINDOW + Q_CHUNK, axis=1)
        kpos_w = start + win_off
        m_w = ((kpos_w[None, :] <= t[:, None]) & (kpos_w[None, :] > t[:, None] - WINDOW)
               & (kpos_w[None, :] >= 0))
        s_w = jnp.einsum('bqgrd,bkgd->bgrqk', qc, kw).astype(jnp.float32) * scale
        p_w = masked_softmax(s_w, m_w)
        o_w = jnp.einsum('bgrqk,bkgd->bqgrd', p_w.astype(vw.dtype), vw)
        o = gc[..., 0:1] * o_c + gc[..., 1:2] * o_s + gc[..., 2:3] * o_w
        return o.reshape(B, Q_CHUNK, ATTN_WIDTH)

    starts = jnp.arange(T // Q_CHUNK) * Q_CHUNK
    out = lax.map(one_chunk, starts)
    return out.transpose(1, 0, 2, 3).reshape(B, T, ATTN_WIDTH)


def conv_ffn(h, w_gate, w_up, conv_w, conv_b, w_down):
    g = h @ w_gate
    g = lax.conv_general_dilated(g, conv_w[:, None, :].astype(g.dtype), window_strides=(1,),
                                 padding=[(CONV_WIDTH - 1, 0)],
                                 dimension_numbers=('NWC', 'WIO', 'NWC'),
                                 feature_group_count=D_FF) + conv_b
    return (jax.nn.silu(g) * (h @ w_up)) @ w_down


def setup_inputs(seed: int = 0) -> dict:
    key = jax.random.key(seed)
    ks = jax.random.split(key, 24)
    L = DEPTH

    def nrm(k, shape, scale):
        return jax.random.normal(k, shape, jnp.float32) * scale

    def gain(k, shape):
        return 1.0 + 0.02 * jax.random.normal(k, shape, jnp.float32)

    return {
        'x': nrm(ks[0], (BATCH, SEQ, D_MODEL), 1.0),
        'norm1': gain(ks[1], (L, D_MODEL)),
        'w_in': nrm(ks[2], (L, D_MODEL, IN_WIDTH), D_MODEL ** -0.5),
        'w_pool': nrm(ks[3], (L, N_POOL_GROUPS, POOL_GROUP, POOL_GROUP), POOL_GROUP ** -0.5),
        's_pool': gain(ks[4], (L, POOL_WIDTH)),
        'cmp_pe_k': nrm(ks[5], (L, CMP_LEN, HEAD_DIM), 0.1),
        'cmp_w1_k': nrm(ks[6], (L, CMP_LEN * HEAD_DIM, CMP_HIDDEN), (CMP_LEN * HEAD_DIM) ** -0.5),
        'cmp_w2_k': nrm(ks[7], (L, CMP_HIDDEN, HEAD_DIM), CMP_HIDDEN ** -0.5),
        'cmp_pe_v': nrm(ks[8], (L, CMP_LEN, HEAD_DIM), 0.1),
        'cmp_w1_v': nrm(ks[9], (L, CMP_LEN * HEAD_DIM, CMP_HIDDEN), (CMP_LEN * HEAD_DIM) ** -0.5),
        'cmp_w2_v': nrm(ks[10], (L, CMP_HIDDEN, HEAD_DIM), CMP_HIDDEN ** -0.5),
        'norm_pool_out': gain(ks[11], (L, POOL_WIDTH)),
        'norm_attn_out': gain(ks[12], (L, ATTN_WIDTH)),
        'w_out': nrm(ks[13], (L, D_MODEL, D_MODEL), D_MODEL ** -0.5),
        'norm2': gain(ks[14], (L, D_MODEL)),
        'w_gate': nrm(ks[15], (L, D_MODEL, D_FF), D_MODEL ** -0.5),
        'w_up': nrm(ks[16], (L, D_MODEL, D_FF), D_MODEL ** -0.5),
        'conv_w': nrm(ks[17], (L, CONV_WIDTH, D_FF), CONV_WIDTH ** -0.5),
        'conv_b': nrm(ks[18], (L, D_FF), 0.01),
        'w_down': nrm(ks[19], (L, D_FF, D_MODEL), D_FF ** -0.5),
        'norm_f': gain(ks[20], (D_MODEL,)),
    }


def reference(x, norm1, w_in, w_pool, s_pool, cmp_pe_k, cmp_w1_k, cmp_w2_k, cmp_pe_v, cmp_w1_v, cmp_w2_v,
              norm_pool_out, norm_attn_out, w_out, norm2, w_gate, w_up, conv_w, conv_b, w_down, norm_f):
    B, T, _ = x.shape

    def kv(a):
        return a.reshape(B, T, N_KV_GROUPS, HEAD_DIM)

    for l in range(DEPTH):
        h = rms_norm(x, norm1[l])
        z = h @ w_in[l]
        u, q, kc, vc, ksel, vsel, kwin, vwin, gl = jnp.split(z, SPLIT_POINTS, axis=-1)
        pool_out = pool_mixer(u, w_pool[l], s_pool[l])
        k_cmp = compress(kv(kc), cmp_pe_k[l], cmp_w1_k[l], cmp_w2_k[l])
        v_cmp = compress(kv(vc), cmp_pe_v[l], cmp_w1_v[l], cmp_w2_v[l])
        gates = jax.nn.sigmoid(gl.astype(jnp.float32)).astype(x.dtype)
        gates = gates.reshape(B, T, N_KV_GROUPS, HEADS_PER_GROUP, N_BRANCHES)
        attn_out = nsa_attention(q.reshape(B, T, N_KV_GROUPS, HEADS_PER_GROUP, HEAD_DIM),
                                 k_cmp, v_cmp, kv(ksel), kv(vsel), kv(kwin), kv(vwin), gates)
        mixed = jnp.concatenate([rms_norm(pool_out, norm_pool_out[l]),
                                 rms_norm(attn_out, norm_attn_out[l])], axis=-1)
        x = x + mixed @ w_out[l]
        x = x + conv_ffn(rms_norm(x, norm2[l]), w_gate[l], w_up[l], conv_w[l], conv_b[l], w_down[l])
    return rms_norm(x, norm_f)
```

```python
import contextlib
import math
import numpy as np
import concourse.bass as bass
import concourse.mybir as mybir
from concourse.bass_utils import run_bass_kernel_spmd

F32 = mybir.dt.float32
BF16 = mybir.dt.bfloat16
AF = mybir.ActivationFunctionType
ALU = mybir.AluOpType

D = 1024
DFF = 2816
NFC = DFF // 128
EPS = 1e-6
NEG = -30000.0
POOL_W = (2, 4, 8, 16)
N_DMA_SEMS = 24


class Buf:
    __slots__ = ("name", "w", "r", "excl")

    def __init__(self, name="", excl=False):
        self.name = name
        self.w = None
        self.r = {}
        self.excl = excl


class Op:
    __slots__ = ("eng", "pos", "fn", "deps", "needed", "sem", "value", "is_dma")

    def __init__(self, eng, pos, fn, is_dma):
        self.eng = eng
        self.pos = pos
        self.fn = fn
        self.deps = []
        self.needed = False
        self.sem = None
        self.value = None
        self.is_dma = is_dma


class Sched:
    ENGS = ("pe", "act", "dve", "pool", "sp")

    def __init__(self, nc):
        self.nc = nc
        self.ops = {e: [] for e in self.ENGS}
        self.waited = {e: {} for e in self.ENGS}
        self.n_dma = 0
        self.dma_ops = []
        self.barrier_deps = {e: [] for e in self.ENGS}

    def barrier(self):
        last = []
        for e in self.ENGS:
            for op in reversed(self.ops[e]):
                if not op.is_dma:
                    last.append(op)
                    break
        last += self.dma_ops[-N_DMA_SEMS:]
        for e in self.ENGS:
            self.barrier_deps[e] = list(last)

    def _add(self, eng, fn, reads, writes, is_dma=False):
        lst = self.ops[eng]
        op = Op(eng, len(lst), fn, is_dma)
        deps = {}

        def need(d, same_ok):
            if d is None:
                return
            if d.eng == eng and (not d.is_dma) and same_ok:
                return
            deps[id(d)] = d

        for b in reads:
            need(b.w, False)
            if b.excl:
                for d in b.r.values():
                    need(d, True)
        for b in writes:
            need(b.w, True)
            for d in b.r.values():
                need(d, True)
        if self.barrier_deps[eng]:
            for d in self.barrier_deps[eng]:
                if not (d.eng == eng and not d.is_dma):
                    deps[id(d)] = d
            self.barrier_deps[eng] = []
        wt = self.waited[eng]
        if is_dma:
            idx = self.n_dma
            self.n_dma += 1
            if idx >= N_DMA_SEMS:
                prev = self.dma_ops[idx - N_DMA_SEMS]
                deps[id(prev)] = prev
        for d in deps.values():
            if d.is_dma:
                key = ("dma", d.pos % N_DMA_SEMS)
            else:
                key = d.eng
            if wt.get(key, -1) >= d.pos:
                continue
            wt[key] = d.pos
            d.needed = True
            op.deps.append(d)
        if is_dma:
            op.pos = idx
            op.needed = True
            self.dma_ops.append(op)
        for b in reads:
            b.r[("dma", op.pos) if is_dma else eng] = op
        for b in writes:
            b.w = op
            b.r = {}
        lst.append(op)
        return op

    def pe(self, fn, reads=(), writes=()):
        return self._add("pe", fn, reads, writes)

    def act(self, fn, reads=(), writes=()):
        return self._add("act", fn, reads, writes)

    def dve(self, fn, reads=(), writes=()):
        return self._add("dve", fn, reads, writes)

    def pool(self, fn, reads=(), writes=()):
        return self._add("pool", fn, reads, writes)

    def dma(self, fn, reads=(), writes=(), q="sp"):
        return self._add(q, fn, reads, writes, is_dma=True)

    def emit(self, final_wait_ops=()):
        nc = self.nc
        with contextlib.ExitStack() as es:
            esem = {e: es.enter_context(nc.semaphore("s_" + e)) for e in self.ENGS}
            dsem = [es.enter_context(nc.semaphore("d%d" % i)) for i in range(N_DMA_SEMS)]
            for e in self.ENGS:
                cnt = 0
                for op in self.ops[e]:
                    if op.is_dma:
                        op.sem = dsem[op.pos % N_DMA_SEMS]
                        op.value = 16 * (op.pos // N_DMA_SEMS + 1)
                    elif op.needed:
                        cnt += 1
                        op.sem = esem[e]
                        op.value = cnt
            block = es.enter_context(nc.Block())
            decos = {"pe": block.tensor, "act": block.scalar, "dve": block.vector,
                     "pool": block.gpsimd, "sp": block.sync}

            def mk(e, extra):
                def body(eng):
                    for op in self.ops[e]:
                        for d in op.deps:
                            eng.wait_ge(d.sem, d.value)
                        ins = op.fn(eng)
                        if op.is_dma:
                            ins.then_inc(op.sem, 16)
                        elif op.needed:
                            ins.then_inc(op.sem, 1)
                    for d in extra:
                        eng.wait_ge(d.sem, d.value)
                return body

            for e in self.ENGS:
                extra = list(final_wait_ops) if e == "sp" else []
                if not self.ops[e] and not extra:
                    continue
                decos[e](mk(e, extra))


def _ap(t, off, dims):
    return bass.AP(t.tensor if hasattr(t, "tensor") else t, off, [list(d) for d in dims])


def build(T, NSEQ, L, debug=False, stop_after_M=False):
    NT = T // 128
    NST = T // 512
    NSLOT = T // 16
    NCT = NSLOT // 128
    nc = bass.Bass("TRN2", target_bir_lowering=False)

    def din(name, shape):
        return nc.dram_tensor(name, list(shape), F32, kind="ExternalInput").ap()

    x_in = din("x", [NSEQ, T, D])
    w_in = din("w_in", [L, D, 1816])
    w_out = din("w_out", [L, D, D])
    w_gate = din("w_gate", [L, D, DFF])
    w_up = din("w_up", [L, D, DFF])
    w_down = din("w_down", [L, DFF, D])
    w_pool = din("w_pool", [L, 128, 4 * 128])
    w1 = din("cmp_w1", [L, 2, 64, 32 * 128])
    w2 = din("cmp_w2", [L, 2, 128, 64])
    peT = din("cmp_peT", [L, 2, 64, 32])
    vecs = din("vecs", [L, 128, 112])
    nao = din("nao", [L, 1, 512])
    normf = din("normf", [1, D])
    c_ident = din("c_ident", [128, 128])
    c_tri = din("c_tri", [128, 256])
    c_dcmp = din("c_dcmp", [128, 256])
    c_ovl = din("c_ovl", [128, 128])
    c_F = din("c_F", [128, 128])
    c_E = din("c_E", [128, 32 * 128])
    c_cinv = din("c_cinv", [128, 64])
    y_out = nc.dram_tensor("y", [NSEQ, T, D], F32, kind="ExternalOutput").ap()
    xbuf = nc.dram_tensor("xbuf", [NSEQ, T, D], F32).ap()
    dbg_out = {}
    if debug:
        dbg_attn = nc.dram_tensor("dbg_attn", [T, 512], F32, kind="ExternalOutput").ap()
        dbg_imp = nc.dram_tensor("dbg_imp", [T, 2, 64], F32, kind="ExternalOutput").ap()
        dbg_selb = nc.dram_tensor("dbg_selb", [T, 2, 64], BF16, kind="ExternalOutput").ap()

    S = Sched(nc)
    out_dmas = []

    with contextlib.ExitStack() as top:
        def sb(name, shape, dt=F32, es=top):
            return es.enter_context(nc.sbuf_tensor(name, list(shape), dt))

        banks = [top.enter_context(nc.psum_tensor("bank%d" % i, [128, 512], F32)) for i in range(8)]
        bbuf = [Buf("bank%d" % i, excl=True) for i in range(8)]
        rr = {"mm": [0, 1], "s": [2, 3], "acc": [4, 5]}
        rr_i = {"mm": 0, "s": 0, "acc": 0}

        def bank(kind):
            i = rr[kind][rr_i[kind] % len(rr[kind])]
            rr_i[kind] += 1
            return banks[i], bbuf[i]

        ident = sb("ident", [128, 128], BF16); Bconst = Buf("const")
        ones = sb("ones", [128, 128], BF16)
        epsc = sb("epsc", [128, 1], F32)
        S.dma(lambda e: e.dma_start(out=ident[:], in_=c_ident), writes=[Bconst], q="pool")
        S.dve(lambda e: e.memset(ones[:], 1.0), writes=[Bconst])
        S.dve(lambda e: e.memset(epsc[:], EPS), writes=[Bconst])

        xd = [[Buf("xd%d_%d" % (s, i)) for i in range(NST)] for s in range(NSEQ)]

        def x_src(l, seq):
            return x_in if l == 0 else xbuf

        def dbg(name, ap_sb, shape, reads):
            if not debug:
                return
            t = nc.dram_tensor("dbg_" + name, list(shape), ap_sb.dtype, kind="ExternalOutput").ap()
            dbg_out[name] = t
            out_dmas.append(S.dma(lambda e: e.dma_start(out=t, in_=ap_sb), reads=reads))

        def rstd_from_ss(ss_ap, out_ap, n, Bss, Bout):
            S.act(lambda e: e.activation(out_ap, ss_ap, AF.Ln, scale=1.0 / n, bias=epsc[0:ss_ap.shape[0], 0:1]),
                  reads=[Bss, Bconst], writes=[Bout])
            S.act(lambda e: e.activation(out_ap, out_ap, AF.Exp, scale=-0.5), reads=[Bout], writes=[Bout])

        def phase_M(l):
            with contextlib.ExitStack() as es:
                def t(name, shape, dt=F32):
                    return sb("M%d_" % l + name, shape, dt, es)
                wfm = t("wfm", [128, 8, 1536], BF16); wtm = t("wtm", [128, 8, 280], BF16)
                wo = t("wo", [128, 8, D], BF16); wpl = t("wpl", [128, 512], BF16)
                w1k = t("w1k", [128, 32 * 128], BF16); w1v = t("w1v", [128, 32 * 128], BF16)
                w2k = t("w2k", [128, 128], BF16); w2v = t("w2v", [128, 64], BF16)
                pet = t("pet", [64, 64], BF16); peb = t("peb", [128, 2], F32)
                vc_ = t("vecs", [128, 112], F32); naob = t("naob", [128, 512], F32)
                Bw = Buf("Mweights")
                tri = t("tri", [128, 256], BF16); dcmp = t("dcmp", [128, 256], F32); ovl = t("ovl", [128, 128], BF16)
                Fc = t("Fc", [128, 128], F32); Ec = t("Ec", [128, 32 * 128], BF16); cinv = t("cinv", [128, 64], F32)
                for dst, src in ((tri, c_tri), (ovl, c_ovl), (Ec, c_E)):
                    S.dma(lambda e, dst=dst, src=src: e.dma_start(out=dst[:], in_=src), writes=[Bconst], q="pool")
                for dst, src in ((dcmp, c_dcmp), (Fc, c_F), (cinv, c_cinv)):
                    S.dma(lambda e, dst=dst, src=src: e.dma_start(out=dst[:], in_=src), writes=[Bconst])
                kselT = t("kselT", [128, T], BF16); kwinT = t("kwinT", [128, T], BF16)
                vselA = t("vselA", [128, NT, 2, 65], BF16); vwinA = t("vwinA", [128, NT, 2, 65], BF16)
                kcmpT = t("kcmpT", [128, NSLOT], BF16); vcmpA = t("vcmpA", [128, NCT, 2, 65], BF16)
                glv = t("glv", [128, 2, NSLOT], BF16)
                Bksel, Bkwin, Bvsel, Bvwin = Buf("ksel"), Buf("kwin"), Buf("vsel"), Buf("vwin")
                Bkcmp, Bvcmp, Bglv = Buf("kcmp"), Buf("vcmp"), Buf("glv")
                xt = t("xt", [128, 4, D], F32); Bxt = Buf("xt")
                xn = [t("xn%d" % i, [128, D], BF16) for i in range(2)]; Bxn = [Buf("xn0"), Buf("xn1")]
                ss = t("ss", [128, 8], F32); Bss = Buf("ss")
                rs = t("rs", [128, 8], F32); Brs = Buf("rs")
                junk = t("junk", [128, D], BF16); Bjunk = Buf("junk")
                hT = t("hT", [128, 8, 512], BF16); BhT = Buf("hT")
                uT = t("uT", [128, 4, 528], F32); BuT = Buf("uT")
                qT = t("qT", [128, 4, 4, 128], BF16); BqT = Buf("qT")
                kcT = t("kcT", [128, 528], BF16); vcT = t("vcT", [128, 528], BF16); BkcT = Buf("kcT"); BvcT = Buf("vcT")
                gat = t("gat", [128, 4, 24], F32); Bgat = Buf("gat")
                ptA = t("ptA", [128, 528], F32); ptB = t("ptB", [128, 528], F32); BptA = Buf("ptA"); BptB = Buf("ptB")
                pooled = [t("pooled%d" % i, [128, 512], BF16) for i in range(2)]; Bpooled = [Buf(), Buf()]
                po = t("po", [128, 4, 512], F32); Bpo = Buf("po")
                sq = [t("sq%d" % i, [128, 512], BF16) for i in range(2)]; Bsq = [Buf(), Buf()]
                rbc = t("rbc", [128, 512], F32); Brbc = Buf("rbc")
                mixT = t("mixT", [128, 8, 512], BF16); BmixT = Buf("mixT")
                PT = [t("PT%d" % i, [128, 512], BF16) for i in range(3)]; BPT = [Buf(), Buf(), Buf()]
                pt_i = [0]
                o_t = t("o", [128, 512], F32); Bo = Buf("o")
                on = t("on", [128, 512], BF16); Bon = Buf("on")
                otmp = t("otmp", [128, 256], F32); Botmp = Buf("otmp")
                hb = t("hb", [128, 128], F32); Bhb = Buf("hb")
                glt = t("glt", [128, 128], BF16); Bglt = Buf("glt")
                cb = t("cb", [128, 128], BF16); Bcb = Buf("cb")
                sm = t("sm", [128, 64], F32); Bsm = Buf("sm")
                imp = t("imp", [128, 64], F32); Bimp = Buf("imp")
                imp2 = t("imp2", [128, 64], F32); Bimp2 = Buf("imp2")
                m8 = t("m8", [128, 16], F32); Bm8 = Buf("m8")
                selb = t("selb", [128, 64], BF16); Bselb = Buf("selb")
                selbT = t("selbT", [128, 128], BF16); BselbT = Buf("selbT")
                ssa = t("ssa", [128, 2], F32); Bssa = Buf("ssa")

                def wdma(dst_ap, src_ap):
                    S.dma(lambda e: e.dma_start(out=dst_ap, in_=src_ap), writes=[Bw], q="pool")
                wv = w_in[l].rearrange("(k p) c -> p k c", p=128)
                for k in range(8):
                    wdma(wfm[:, k, :], wv[:, k, 0:1536])
                wdma(wtm[:, :, :], wv[:, :, 1536:1816])
                wov = w_out[l].rearrange("(k p) c -> p k c", p=128)
                for k in range(8):
                    wdma(wo[:, k, :], wov[:, k, :])
                wdma(wpl[:], w_pool[l])
                for h in range(2):
                    wdma(w1k[64 * h:64 * h + 64, :], w1[l, 0])
                    wdma(w1v[64 * h:64 * h + 64, :], w1[l, 1])
                    wdma(w2k[:, 64 * h:64 * h + 64], w2[l, 0])
                wdma(w2v[:], w2[l, 1])
                wdma(pet[:, 0:32], peT[l, 0]); wdma(pet[:, 32:64], peT[l, 1])
                S.dma(lambda e: e.dma_start(out=vc_[:], in_=vecs[l]), writes=[Bw])
                S.dma(lambda e: e.dma_start(out=naob[:], in_=nao[l].partition_broadcast(128)), writes=[Bw])
                g1col = vc_[:, 0:8]; spool = vc_[:, 8:12]; npo = vc_[:, 12:16]

                S.pool(lambda e: e.memset(vselA[:], 1.0), writes=[Bvsel])
                S.pool(lambda e: e.memset(vwinA[:], 1.0), writes=[Bvwin])
                S.pool(lambda e: e.memset(vcmpA[:], 1.0), writes=[Bvcmp])
                S.pool(lambda e: e.memset(kcmpT[:], 0.0), writes=[Bkcmp])
                S.pool(lambda e: e.memset(glv[:], 0.0), writes=[Bglv])
                S.pool(lambda e: e.memset(kselT[:], 0.0), writes=[Bksel])
                S.pool(lambda e: e.memset(kwinT[:], 0.0), writes=[Bkwin])
                S.pool(lambda e: e.memset(selbT[:], 0.0), writes=[BselbT])
                bk, Bb = banks[7], bbuf[7]
                for kv, wt_ in ((0, w1k), (1, w1v)):
                    for li in range(32):
                        S.pe(lambda e, kv=kv, wt_=wt_, li=li: e.matmul(bk[:, kv:kv + 1], wt_[0:64, li * 128:(li + 1) * 128],
                                                                      pet[:, kv * 32 + li:kv * 32 + li + 1], start=(li == 0), stop=(li == 31)),
                             reads=[Bw], writes=[Bb])
                S.dve(lambda e: e.tensor_copy(peb[:], bk[:, 0:2]), reads=[Bb], writes=[Bw])

                for seq in range(NSEQ):
                    xs = x_src(l, seq)
                    S.pool(lambda e: e.memset(uT[:, :, 0:16], 0.0), writes=[BuT])
                    S.pool(lambda e: e.memset(kcT[:, 0:16], 0.0), writes=[BkcT])
                    S.pool(lambda e: e.memset(vcT[:, 0:16], 0.0), writes=[BvcT])
                    for st in range(NST):
                        T0 = st * 512
                        S.dma(lambda e, xs=xs, seq=seq, T0=T0: e.dma_start(
                            out=xt[:], in_=xs[seq, T0:T0 + 512, :].rearrange("(s p) d -> p s d", p=128)),
                            reads=[xd[seq][st]], writes=[Bxt])
                        if st > 0:
                            S.pool(lambda e: e.tensor_copy(uT[:, :, 0:16], uT[:, :, 512:528]), reads=[BuT], writes=[BuT])
                            S.pool(lambda e: e.tensor_copy(kcT[:, 0:16], kcT[:, 512:528]), reads=[BkcT], writes=[BkcT])
                            S.pool(lambda e: e.tensor_copy(vcT[:, 0:16], vcT[:, 512:528]), reads=[BvcT], writes=[BvcT])
                        S.dve(lambda e: e.memset(ss[:], 0.0), writes=[Bss])
                        for s in range(4):
                            S.act(lambda e, s=s: e.activation(junk[:], xt[:, s, :], AF.Square, accum_out=ss[:, s:s + 1]),
                                  reads=[Bxt], writes=[Bjunk, Bss])
                        rstd_from_ss(ss[:, 0:4], rs[:, 0:4], D, Bss, Brs)
                        for s in range(4):
                            S.dve(lambda e, s=s: e.tensor_scalar(xn[s % 2][:], xt[:, s, :], rs[:, s:s + 1], None, ALU.mult),
                                  reads=[Bxt, Brs], writes=[Bxn[s % 2]])
                            for kh in range(2):
                                bk, Bb = bank("mm")
                                for kk in range(4):
                                    k = kh * 4 + kk
                                    S.pe(lambda e, s=s, k=k, kk=kk, bk=bk: e.matmul(bk[:, kk * 128:(kk + 1) * 128], xn[s % 2][:, k * 128:(k + 1) * 128],
                                                                                   ident[:], start=True, stop=True),
                                         reads=[Bxn[s % 2], Bconst], writes=[Bb])
                                for kk in range(4):
                                    k = kh * 4 + kk
                                    eng = S.act if kk % 2 == 0 else S.dve
                                    if kk % 2 == 0:
                                        S.act(lambda e, s=s, k=k, kk=kk, bk=bk: e.activation(hT[:, k, s * 128:(s + 1) * 128], bk[:, kk * 128:(kk + 1) * 128],
                                                                                          AF.Copy, scale=g1col[:, k:k + 1]),
                                              reads=[Bb, Bw], writes=[BhT])
                                    else:
                                        S.dve(lambda e, s=s, k=k, kk=kk, bk=bk: e.tensor_scalar(hT[:, k, s * 128:(s + 1) * 128], bk[:, kk * 128:(kk + 1) * 128],
                                                                                              g1col[:, k:k + 1], None, ALU.mult),
                                              reads=[Bb, Bw], writes=[BhT])
                        for c in range(12):
                            bk, Bb = bank("mm")
                            for k in range(8):
                                S.pe(lambda e, c=c, k=k, bk=bk: e.matmul(bk[:], wfm[:, k, c * 128:(c + 1) * 128], hT[:, k, :], start=(k == 0), stop=(k == 7)),
                                     reads=[Bw, BhT], writes=[Bb])
                            if c < 4:
                                S.act(lambda e, c=c, bk=bk: e.activation(uT[:, c, 16:528], bk[:], AF.Copy), reads=[Bb], writes=[BuT])
                            elif c < 8:
                                r = c - 4
                                dst = _ap(qT, r * 128, [[2048, 128], [512, 4], [1, 128]])
                                src = _ap(bk, 0, [[512, 128], [128, 4], [1, 128]])
                                S.dve(lambda e, dst=dst, src=src: e.tensor_copy(dst, src), reads=[Bb], writes=[BqT])
                            elif c == 8:
                                S.act(lambda e, bk=bk: e.activation(kcT[:, 16:528], bk[:], AF.Copy), reads=[Bb], writes=[BkcT])
                            elif c == 9:
                                S.dve(lambda e, bk=bk: e.tensor_copy(vcT[:, 16:528], bk[:]), reads=[Bb], writes=[BvcT])
                            elif c == 10:
                                S.act(lambda e, bk=bk, T0=T0: e.activation(kselT[:, T0:T0 + 512], bk[:], AF.Copy), reads=[Bb], writes=[Bksel])
                            else:
                                S.dve(lambda e, bk=bk, T0=T0: e.tensor_copy(kwinT[:, T0:T0 + 512], bk[:]), reads=[Bb], writes=[Bkwin])
                        for s in range(4):
                            ti = st * 4 + s
                            bk, Bb = bank("mm")
                            for k in range(8):
                                S.pe(lambda e, s=s, k=k, bk=bk: e.matmul(bk[:, 0:280], hT[:, k, s * 128:(s + 1) * 128], wtm[:, k, :], start=(k == 0), stop=(k == 7)),
                                     reads=[Bw, BhT], writes=[Bb])
                            src = _ap(bk, 0, [[512, 128], [64, 2], [1, 64]])
                            S.act(lambda e, ti=ti, src=src: e.activation(vselA[:, ti, :, 0:64], src, AF.Copy), reads=[Bb], writes=[Bvsel])
                            src2 = _ap(bk, 128, [[512, 128], [64, 2], [1, 64]])
                            S.dve(lambda e, ti=ti, src2=src2: e.tensor_copy(vwinA[:, ti, :, 0:64], src2), reads=[Bb], writes=[Bvwin])
                            S.act(lambda e, s=s, bk=bk: e.activation(gat[:, s, :], bk[:, 256:280], AF.Exp, scale=-1.0), reads=[Bb], writes=[Bgat])
                            S.dve(lambda e, s=s: e.tensor_scalar(gat[:, s, :], gat[:, s, :], 1.0, None, ALU.add), reads=[Bgat], writes=[Bgat])
                            S.dve(lambda e, s=s: e.reciprocal(gat[:, s, :], gat[:, s, :]), reads=[Bgat], writes=[Bgat])
                        bst, Bbst = banks[6], bbuf[6]
                        for p in range(4):
                            w = POOL_W[p]
                            cur, Bcur = uT[:, p, :], BuT
                            tmp = [(ptA, BptA), (ptB, BptB)]
                            step = 1
                            i = 0
                            while step < w:
                                dstt, Bd = tmp[i % 2]
                                lo = 2 * step - 1
                                S.pool(lambda e, dstt=dstt, cur=cur, lo=lo, step=step: e.tensor_tensor(dstt[:, lo:528], cur[:, lo:528], cur[:, lo - step:528 - step], ALU.add),
                                       reads=[Bcur], writes=[Bd])
                                cur, Bcur = dstt[:, :], Bd
                                step *= 2
                                i += 1
                            pl, Bpl = pooled[p % 2], Bpooled[p % 2]
                            S.dve(lambda e, pl=pl, cur=cur, p=p, w=w: e.scalar_tensor_tensor(pl[:], cur[:, 16:528], 1.0 / w, uT[:, p, 16:528], ALU.mult, ALU.subtract),
                                   reads=[Bcur, BuT], writes=[Bpl])
                            if st == 0:
                                S.pool(lambda e, cur=cur, p=p: e.tensor_tensor(sm[:, 0:16], cur[:, 16:32], cinv[:, p * 16:(p + 1) * 16], ALU.mult),
                                       reads=[Bcur, Bconst], writes=[Bsm])
                                S.pool(lambda e, pl=pl, p=p: e.tensor_tensor(pl[:, 0:16], sm[:, 0:16], uT[:, p, 16:32], ALU.subtract),
                                       reads=[Bsm, BuT], writes=[Bpl])
                            bk, Bb = bank("mm")
                            S.pe(lambda e, p=p, pl=pl, bk=bk: e.matmul(bk[:], wpl[:, p * 128:(p + 1) * 128], pl[:], start=True, stop=True),
                                 reads=[Bw, Bpl], writes=[Bb])
                            S.act(lambda e, p=p, bk=bk: e.activation(po[:, p, :], bk[:], AF.Copy, scale=spool[:, p:p + 1]), reads=[Bb, Bw], writes=[Bpo])
                            S.dve(lambda e, p=p: e.tensor_tensor(sq[p % 2][:], po[:, p, :], po[:, p, :], ALU.mult), reads=[Bpo], writes=[Bsq[p % 2]])
                            S.pe(lambda e, p=p: e.matmul(bst[:], ones[:], sq[p % 2][:], start=(p == 0), stop=(p == 3)),
                                 reads=[Bconst, Bsq[p % 2]], writes=[Bbst])
                        rstd_from_ss(bst[:], rbc[:], 512, Bbst, Brbc)
                        for p in range(4):
                            S.dve(lambda e, p=p: e.scalar_tensor_tensor(mixT[:, p, :], po[:, p, :], npo[:, p:p + 1], rbc[:], ALU.mult, ALU.mult),
                                  reads=[Bpo, Bw, Brbc], writes=[BmixT])
                        s0 = 32 * st
                        for g in range(2):
                            bk, Bb = banks[6 + g], bbuf[6 + g]
                            R0 = 64 * g
                            for kv, (src_t, Bsrc, wt_) in enumerate(((kcT, BkcT, w1k), (vcT, BvcT, w1v))):
                                for li in range(32):
                                    S.pe(lambda e, kv=kv, src_t=src_t, wt_=wt_, li=li, R0=R0, bk=bk: e.matmul(
                                        bk[:, kv * 32:(kv + 1) * 32], wt_[R0:R0 + 64, li * 128:(li + 1) * 128],
                                        src_t[R0:R0 + 64, li:li + 16 * 31 + 1:16], start=(li == 0), stop=(li == 31)),
                                        reads=[Bw, Bsrc], writes=[Bb])
                            for kv in range(2):
                                S.act(lambda e, kv=kv, g=g, bk=bk: e.activation(glt[:, g * 64 + kv * 32:g * 64 + kv * 32 + 32], bk[:, kv * 32:(kv + 1) * 32],
                                                                            AF.Gelu_apprx_tanh, bias=peb[:, kv:kv + 1]),
                                      reads=[Bb, Bw], writes=[Bglt])
                            S.dve(lambda e, g=g, s0=s0: e.tensor_copy(glv[:, g, s0:s0 + 32], glt[:, g * 64 + 32:g * 64 + 64]), reads=[Bglt], writes=[Bglv])
                        for g in range(2):
                            bk, Bb = bank("mm")
                            S.pe(lambda e, g=g, bk=bk: e.matmul(bk[:, 0:32], w2k[:], glt[:, g * 64:g * 64 + 32], start=True, stop=True),
                                 reads=[Bw, Bglt], writes=[Bb])
                            S.act(lambda e, g=g, bk=bk, s0=s0: e.activation(kcmpT[64 * g:64 * g + 64, s0:s0 + 32], bk[64 * g:64 * g + 64, 0:32], AF.Copy),
                                  reads=[Bb], writes=[Bkcmp])
                            jt = st // 4
                            S.pe(lambda e, g=g, bk=bk, jt=jt: e.matmul(bk[:, 64:128], glv[:, g, jt * 128:(jt + 1) * 128], w2v[:], start=True, stop=True),
                                 reads=[Bw, Bglv], writes=[Bb])
                            S.dve(lambda e, g=g, bk=bk, jt=jt: e.tensor_copy(vcmpA[:, jt, g, 0:64], bk[:, 64:128]), reads=[Bb], writes=[Bvcmp])
                        for s in range(4):
                            qi = st * 4 + s
                            t0 = qi * 128
                            ncmp = 1 if (NCT == 1 or qi < 16) else 2
                            for g in range(2):
                                R0 = 64 * g
                                attn_group(l, seq, st, s, qi, t0, g, R0, ncmp, locals())
                            if debug and seq == 0 and l == 0:
                                out_dmas.append(S.dma(lambda e, t0=t0: e.dma_start(out=dbg_attn[t0:t0 + 128, :], in_=o_t[:]), reads=[Bo]))
                            S.dve(lambda e: e.memset(ssa[:], 0.0), writes=[Bssa])
                            S.act(lambda e: e.activation(junk[:, 0:512], o_t[:], AF.Square, accum_out=ssa[:, 0:1]), reads=[Bo], writes=[Bjunk, Bssa])
                            rstd_from_ss(ssa[:, 0:1], ssa[:, 1:2], 512, Bssa, Bssa)
                            S.dve(lambda e: e.scalar_tensor_tensor(on[:], o_t[:], ssa[:, 1:2], naob[:], ALU.mult, ALU.mult),
                                  reads=[Bo, Bssa, Bw], writes=[Bon])
                            bk, Bb = bank("mm")
                            for c in range(4):
                                S.pe(lambda e, c=c, bk=bk: e.matmul(bk[:, c * 128:(c + 1) * 128], on[:, c * 128:(c + 1) * 128], ident[:], start=True, stop=True),
                                     reads=[Bon, Bconst], writes=[Bb])
                            dst = _ap(mixT, 4 * 512 + s * 128, [[4096, 128], [512, 4], [1, 128]])
                            src = _ap(bk, 0, [[512, 128], [128, 4], [1, 128]])
                            S.act(lambda e, dst=dst, src=src: e.activation(dst, src, AF.Copy), reads=[Bb], writes=[BmixT])
                        for s in range(4):
                            for nh in range(2):
                                bk, Bb = bank("mm")
                                for k in range(8):
                                    S.pe(lambda e, s=s, nh=nh, k=k, bk=bk: e.matmul(bk[:], mixT[:, k, s * 128:(s + 1) * 128], wo[:, k, nh * 512:(nh + 1) * 512],
                                                                                 start=(k == 0), stop=(k == 7)),
                                         reads=[BmixT, Bw], writes=[Bb])
                                S.dve(lambda e, s=s, nh=nh, bk=bk: e.tensor_tensor(xt[:, s, nh * 512:(nh + 1) * 512], xt[:, s, nh * 512:(nh + 1) * 512], bk[:], ALU.add),
                                      reads=[Bb, Bxt], writes=[Bxt])
                        xdst = y_out if stop_after_M else xbuf
                        od = S.dma(lambda e, seq=seq, T0=T0, xdst=xdst: e.dma_start(out=xdst[seq, T0:T0 + 512, :].rearrange("(s p) d -> p s d", p=128), in_=xt[:]),
                                   reads=[Bxt], writes=[xd[seq][st]])
                        if stop_after_M:
                            out_dmas.append(od)

        def attn_group(l, seq, st, s, qi, t0, g, R0, ncmp, L_):
            qT = L_["qT"]; BqT = L_["BqT"]; PT = L_["PT"]; BPT = L_["BPT"]; pt_i = L_["pt_i"]
            kcmpT, Bkcmp, vcmpA, Bvcmp = L_["kcmpT"], L_["Bkcmp"], L_["vcmpA"], L_["Bvcmp"]
            kselT, Bksel, vselA, Bvsel = L_["kselT"], L_["Bksel"], L_["vselA"], L_["Bvsel"]
            kwinT, Bkwin, vwinA, Bvwin = L_["kwinT"], L_["Bkwin"], L_["vwinA"], L_["Bvwin"]
            cb, Bcb, sm, Bsm, imp, Bimp, imp2, Bimp2 = L_["cb"], L_["Bcb"], L_["sm"], L_["Bsm"], L_["imp"], L_["Bimp"], L_["imp2"], L_["Bimp2"]
            m8, Bm8, selb, Bselb, selbT, BselbT = L_["m8"], L_["Bm8"], L_["selb"], L_["Bselb"], L_["selbT"], L_["BselbT"]
            gat, Bgat, o_t, Bo, otmp, Botmp = L_["gat"], L_["Bgat"], L_["o_t"], L_["Bo"], L_["otmp"], L_["Botmp"]
            tri, dcmp, ovl, Fc, Ec = L_["tri"], L_["dcmp"], L_["ovl"], L_["Fc"], L_["Ec"]
            qrhs = qT[R0:R0 + 64, s, :, :]

            def s_tile(kT_ap, Bk, bias_fns):
                bk, Bb = bank("s")
                n = len(bias_fns)
                for i, (fn, rd) in enumerate(bias_fns):
                    S.pe(lambda e, fn=fn, i=i, bk=bk: fn(e, bk, i == 0), reads=rd, writes=[Bb])
                S.pe(lambda e, bk=bk, n=n: e.matmul(bk[:], kT_ap, qrhs, start=(n == 0), stop=True), reads=[Bk, BqT], writes=[Bb])
                i = pt_i[0] % 3
                pt_i[0] += 1
                S.act(lambda e, bk=bk, i=i: e.activation(PT[i][:], bk[:], AF.Exp, scale=0.125), reads=[Bb], writes=[BPT[i]])
                return PT[i], BPT[i]

            def pv(acc, Bacc, P, BP, v_ap, Bv, first, last):
                for r in range(4):
                    S.pe(lambda e, r=r: e.matmul(acc[:, r * 65:(r + 1) * 65], P[:, r * 128:(r + 1) * 128], v_ap, start=(first and r == 0), stop=last),
                         reads=[BP, Bv], writes=[Bacc])

            def combine(acc, Bacc, b, first):
                sums = _ap(acc, 64, [[512, 128], [65, 4]])
                S.dve(lambda e: e.tensor_scalar(sm[:, 0:4], sums, 1e-30, None, ALU.max), reads=[Bacc], writes=[Bsm])
                S.dve(lambda e: e.reciprocal(sm[:, 4:8], sm[:, 0:4]), reads=[Bsm], writes=[Bsm])
                gsl = _ap(gat, s * 24 + g * 12 + b, [[96, 128], [3, 4]])
                S.dve(lambda e: e.tensor_tensor(sm[:, 8:12], sm[:, 4:8], gsl, ALU.mult), reads=[Bsm, Bgat], writes=[Bsm])
                in0 = _ap(acc, 0, [[512, 128], [65, 4], [1, 64]])
                in1 = _ap(sm, 8, [[64, 128], [1, 4], [0, 64]])
                if first:
                    S.dve(lambda e: e.tensor_tensor(_ap(o_t, g * 256, [[512, 128], [64, 4], [1, 64]]), in0, in1, ALU.mult), reads=[Bacc, Bsm], writes=[Bo])
                else:
                    S.dve(lambda e: e.tensor_tensor(_ap(otmp, 0, [[256, 128], [64, 4], [1, 64]]), in0, in1, ALU.mult), reads=[Bacc, Bsm], writes=[Botmp])
                    S.pool(lambda e: e.tensor_tensor(o_t[:, g * 256:(g + 1) * 256], o_t[:, g * 256:(g + 1) * 256], otmp[:], ALU.add),
                           reads=[Botmp, Bo], writes=[Bo])

            acc, Bacc = bank("acc")
            bimp, Bbimp = banks[6], bbuf[6]
            for j in range(ncmp):
                cval = 30000.0 * (t0 - 2048 * j - 15)
                S.dve(lambda e, j=j, cval=cval: e.tensor_scalar(cb[:], dcmp[:, j * 128:(j + 1) * 128], cval, 0.0, ALU.add, ALU.min),
                      reads=[Bconst], writes=[Bcb])
                rep = _ap(cb, 0, [[128, 128], [0, 4], [1, 128]])
                P, BP = s_tile(kcmpT[R0:R0 + 64, j * 128:(j + 1) * 128], Bkcmp,
                               [(lambda e, bk, st_, rep=rep: e.matmul(bk[:], ident[:], rep, start=st_, stop=False), [Bconst, Bcb])])
                pv(acc, Bacc, P, BP, vcmpA[:, j, g, :], Bvcmp, j == 0, j == ncmp - 1)
                for r in range(4):
                    S.pe(lambda e, r=r, j=j, P=P: e.matmul(bimp[:, r * 64:(r + 1) * 64], P[:, r * 128:(r + 1) * 128], ovl[:, j * 64:(j + 1) * 64],
                                                        start=(j == 0 and r == 0), stop=(j == ncmp - 1)),
                         reads=[BP, Bconst], writes=[Bbimp])
            combine(acc, Bacc, 0, True)
            for r in range(4):
                if r == 0:
                    S.dve(lambda e: e.tensor_scalar(imp[:], bimp[:, 0:64], sm[:, 4:5], None, ALU.mult), reads=[Bbimp, Bsm], writes=[Bimp])
                else:
                    S.dve(lambda e, r=r: e.scalar_tensor_tensor(imp[:], bimp[:, r * 64:(r + 1) * 64], sm[:, 4 + r:5 + r], imp[:], ALU.mult, ALU.add),
                          reads=[Bbimp, Bsm, Bimp], writes=[Bimp])
            S.dve(lambda e: e.tensor_tensor(imp[:], imp[:], Fc[:, 62 - 2 * qi:126 - 2 * qi], ALU.add), reads=[Bimp, Bconst], writes=[Bimp])
            S.dve(lambda e: e.max(out=m8[:, 0:8], in_=imp[:]), reads=[Bimp], writes=[Bm8])
            S.dve(lambda e: e.match_replace(out=imp2[:], in_to_replace=m8[:, 0:8], in_values=imp[:], imm_value=-3e9), reads=[Bimp, Bm8], writes=[Bimp2])
            S.dve(lambda e: e.max(out=m8[:, 8:16], in_=imp2[:]), reads=[Bimp2], writes=[Bm8])
            S.dve(lambda e: e.tensor_scalar(selb[:], imp[:], m8[:, 15:16], None, ALU.is_lt), reads=[Bimp, Bm8], writes=[Bselb])
            if debug and seq == 0 and l == 0:
                out_dmas.append(S.dma(lambda e: e.dma_start(out=dbg_imp[t0:t0 + 128, g, :], in_=imp[:]), reads=[Bimp]))
                out_dmas.append(S.dma(lambda e: e.dma_start(out=dbg_selb[t0:t0 + 128, g, :], in_=selb[:]), reads=[Bselb]))
            bk, Bb = bank("mm")
            S.pe(lambda e, bk=bk: e.matmul(bk[0:64, 0:128], selb[:], ident[:], start=True, stop=True), reads=[Bselb, Bconst], writes=[Bb])
            S.act(lambda e, bk=bk: e.activation(selbT[0:64, :], bk[0:64, 0:128], AF.Copy, scale=NEG), reads=[Bb], writes=[BselbT])
            acc, Bacc = bank("acc")
            kts = [kt for kt in range(qi - 4, qi + 1) if kt >= 0]
            for i, kt in enumerate(kts):
                bias = []
                if kt == qi:
                    rep = _ap(tri, 0, [[256, 128], [0, 4], [1, 128]])
                    bias.append((lambda e, bk, st_, rep=rep: e.matmul(bk[:], ident[:], rep, start=st_, stop=False), [Bconst]))
                if kt == qi - 4:
                    rep = _ap(tri, 128, [[256, 128], [0, 4], [1, 128]])
                    bias.append((lambda e, bk, st_, rep=rep: e.matmul(bk[:], ident[:], rep, start=st_, stop=False), [Bconst]))
                P, BP = s_tile(kwinT[R0:R0 + 64, kt * 128:(kt + 1) * 128], Bkwin, bias)
                pv(acc, Bacc, P, BP, vwinA[:, kt, g, :], Bvwin, i == 0, i == len(kts) - 1)
            combine(acc, Bacc, 2, False)
            acc, Bacc = bank("acc")
            srep = _ap(selbT, 0, [[128, 128], [0, 4], [1, 128]])
            for kt in range(qi + 1):
                bias = [(lambda e, bk, st_, kt=kt: e.matmul(bk[:], Ec[:, kt * 128:(kt + 1) * 128], srep, start=st_, stop=False), [Bconst, BselbT])]
                if kt == qi:
                    rep = _ap(tri, 0, [[256, 128], [0, 4], [1, 128]])
                    bias.append((lambda e, bk, st_, rep=rep: e.matmul(bk[:], ident[:], rep, start=st_, stop=False), [Bconst]))
                P, BP = s_tile(kselT[R0:R0 + 64, kt * 128:(kt + 1) * 128], Bksel, bias)
                pv(acc, Bacc, P, BP, vselA[:, kt, g, :], Bvsel, kt == 0, kt == qi)
            combine(acc, Bacc, 1, False)

        def phase_F(l, last):
            with contextlib.ExitStack() as es:
                def t(name, shape, dt=F32):
                    return sb("F%d_" % l + name, shape, dt, es)
                wg = t("wg", [128, 8, DFF], BF16); wu = t("wu", [128, 8, DFF], BF16); wd = t("wd", [128, NFC, D], BF16)
                vc_ = t("vecs", [128, 112], F32); Bw = Buf("Fweights")
                nfb = t("nfb", [128, D], F32) if last else None
                xt = t("xt", [128, 4, D], F32); Bxs = [Buf("xs%d" % i) for i in range(4)]
                xn = [t("xn%d" % i, [128, D], BF16) for i in range(2)]; Bxn = [Buf(), Buf()]
                junk = t("junk", [128, D], BF16); Bjunk = Buf()
                ss = t("ss", [128, 8], F32); Bss = Buf(); rs = t("rs", [128, 8], F32); Brs = Buf()
                hT = t("hT", [128, 8, 512], BF16); BhT = Buf()
                aT = t("aT", [128, NFC, 512], BF16); BaT = Buf()
                gb = [t("gb%d" % i, [128, 514], F32) for i in range(2)]; Bgb = [Buf(), Buf()]
                tA = [t("tA%d" % i, [128, 512], F32) for i in range(2)]; BtA = [Buf(), Buf()]
                tB = [t("tB%d" % i, [128, 512], F32) for i in range(2)]; BtB = [Buf(), Buf()]
                gh = t("gh", [128, 2, NFC, 2], F32); Bgh = Buf()

                def wdma(dst_ap, src_ap):
                    S.dma(lambda e: e.dma_start(out=dst_ap, in_=src_ap), writes=[Bw], q="pool")
                gv = w_gate[l].rearrange("(k p) c -> p k c", p=128)
                uv = w_up[l].rearrange("(k p) c -> p k c", p=128)
                dv = w_down[l].rearrange("(c p) d -> p c d", p=128)
                for k in range(8):
                    wdma(wg[:, k, :], gv[:, k, :])
                for k in range(8):
                    wdma(wu[:, k, :], uv[:, k, :])
                for c in range(NFC):
                    wdma(wd[:, c, :], dv[:, c, :])
                S.dma(lambda e: e.dma_start(out=vc_[:], in_=vecs[l]), writes=[Bw])
                if last:
                    S.dma(lambda e: e.dma_start(out=nfb[:], in_=normf.partition_broadcast(128)), writes=[Bw])
                n2col = vc_[:, 16:24]
                cw = vc_[:, 24:90]
                cbias = vc_[:, 90:112]

                for seq in range(NSEQ):
                    S.pool(lambda e: e.memset(gh[:], 0.0), writes=[Bgh])
                    for st in range(NST):
                        T0 = st * 512
                        for s in range(4):
                            S.dma(lambda e, seq=seq, T0=T0, s=s: e.dma_start(out=xt[:, s, :], in_=xbuf[seq, T0 + s * 128:T0 + (s + 1) * 128, :]),
                                  reads=[xd[seq][st]], writes=[Bxs[s]])
                        S.dve(lambda e: e.memset(ss[:], 0.0), writes=[Bss])
                        for s in range(4):
                            S.act(lambda e, s=s: e.activation(junk[:], xt[:, s, :], AF.Square, accum_out=ss[:, s:s + 1]), reads=[Bxs[s]], writes=[Bjunk, Bss])
                        rstd_from_ss(ss[:, 0:4], rs[:, 0:4], D, Bss, Brs)
                        for s in range(4):
                            S.dve(lambda e, s=s: e.tensor_scalar(xn[s % 2][:], xt[:, s, :], rs[:, s:s + 1], None, ALU.mult),
                                  reads=[Bxs[s], Brs], writes=[Bxn[s % 2]])
                            for kh in range(2):
                                bk, Bb = bank("mm")
                                for kk in range(4):
                                    k = kh * 4 + kk
                                    S.pe(lambda e, s=s, k=k, kk=kk, bk=bk: e.matmul(bk[:, kk * 128:(kk + 1) * 128], xn[s % 2][:, k * 128:(k + 1) * 128],
                                                                                   ident[:], start=True, stop=True),
                                         reads=[Bxn[s % 2], Bconst], writes=[Bb])
                                for kk in range(4):
                                    k = kh * 4 + kk
                                    if kk % 2 == 0:
                                        S.act(lambda e, s=s, k=k, kk=kk, bk=bk: e.activation(hT[:, k, s * 128:(s + 1) * 128], bk[:, kk * 128:(kk + 1) * 128],
                                                                                          AF.Copy, scale=n2col[:, k:k + 1]), reads=[Bb, Bw], writes=[BhT])
                                    else:
                                        S.dve(lambda e, s=s, k=k, kk=kk, bk=bk: e.tensor_scalar(hT[:, k, s * 128:(s + 1) * 128], bk[:, kk * 128:(kk + 1) * 128],
                                                                                              n2col[:, k:k + 1], None, ALU.mult), reads=[Bb, Bw], writes=[BhT])
                        hi, ho = st % 2, (st + 1) % 2
                        for c in range(NFC):
                            bg, Bbg = bank("mm")
                            for k in range(8):
                                S.pe(lambda e, c=c, k=k, bg=bg: e.matmul(bg[:], wg[:, k, c * 128:(c + 1) * 128], hT[:, k, :], start=(k == 0), stop=(k == 7)),
                                     reads=[Bw, BhT], writes=[Bbg])
                            bu, Bbu = bank("s")
                            for k in range(8):
                                S.pe(lambda e, c=c, k=k, bu=bu: e.matmul(bu[:], wu[:, k, c * 128:(c + 1) * 128], hT[:, k, :], start=(k == 0), stop=(k == 7)),
                                     reads=[Bw, BhT], writes=[Bbu])
                            i = c % 2
                            S.act(lambda e, i=i, bg=bg: e.activation(gb[i][:, 2:514], bg[:], AF.Copy), reads=[Bbg], writes=[Bgb[i]])
                            S.pool(lambda e, i=i, c=c, hi=hi: e.tensor_copy(gb[i][:, 0:2], gh[:, hi, c, :]), reads=[Bgh], writes=[Bgb[i]])
                            S.pool(lambda e, i=i, c=c, ho=ho: e.tensor_copy(gh[:, ho, c, :], gb[i][:, 512:514]), reads=[Bgb[i]], writes=[Bgh])
                            S.act(lambda e, i=i, c=c: e.activation(tA[i][:], gb[i][:, 2:514], AF.Identity, scale=cw[:, 3 * c + 2:3 * c + 3], bias=cbias[:, c:c + 1]),
                                  reads=[Bgb[i], Bw], writes=[BtA[i]])
                            S.dve(lambda e, i=i, c=c: e.scalar_tensor_tensor(tB[i][:], gb[i][:, 1:513], cw[:, 3 * c + 1:3 * c + 2], tA[i][:], ALU.mult, ALU.add),
                                  reads=[Bgb[i], Bw, BtA[i]], writes=[BtB[i]])
                            S.dve(lambda e, i=i, c=c: e.scalar_tensor_tensor(tA[i][:], gb[i][:, 0:512], cw[:, 3 * c:3 * c + 1], tB[i][:], ALU.mult, ALU.add),
                                   reads=[Bgb[i], Bw, BtB[i]], writes=[BtA[i]])
                            S.act(lambda e, i=i: e.activation(tB[i][:], tA[i][:], AF.Silu), reads=[BtA[i]], writes=[BtB[i]])
                            S.dve(lambda e, i=i, c=c, bu=bu: e.tensor_tensor(aT[:, c, :], tB[i][:], bu[:], ALU.mult), reads=[BtB[i], Bbu], writes=[BaT])
                        for s in range(4):
                            for nh in range(2):
                                bk, Bb = bank("acc")
                                for c in range(NFC):
                                    S.pe(lambda e, s=s, nh=nh, c=c, bk=bk: e.matmul(bk[:], aT[:, c, s * 128:(s + 1) * 128], wd[:, c, nh * 512:(nh + 1) * 512],
                                                                                 start=(c == 0), stop=(c == NFC - 1)),
                                         reads=[BaT, Bw], writes=[Bb])
                                S.dve(lambda e, s=s, nh=nh, bk=bk: e.tensor_tensor(xt[:, s, nh * 512:(nh + 1) * 512], xt[:, s, nh * 512:(nh + 1) * 512], bk[:], ALU.add),
                                      reads=[Bb, Bxs[s]], writes=[Bxs[s]])
                            if not last:
                                S.dma(lambda e, seq=seq, T0=T0, s=s: e.dma_start(out=xbuf[seq, T0 + s * 128:T0 + (s + 1) * 128, :], in_=xt[:, s, :]),
                                      reads=[Bxs[s]], writes=[xd[seq][st]])
                            else:
                                S.act(lambda e, s=s: e.activation(junk[:], xt[:, s, :], AF.Square, accum_out=ss[:, 4 + s:5 + s]), reads=[Bxs[s]], writes=[Bjunk, Bss])
                                rstd_from_ss(ss[:, 4 + s:5 + s], rs[:, 4 + s:5 + s], D, Bss, Brs)
                                S.dve(lambda e, s=s: e.scalar_tensor_tensor(xt[:, s, :], xt[:, s, :], rs[:, 4 + s:5 + s], nfb[:], ALU.mult, ALU.mult),
                                      reads=[Bxs[s], Brs, Bw], writes=[Bxs[s]])
                                out_dmas.append(S.dma(lambda e, seq=seq, T0=T0, s=s: e.dma_start(out=y_out[seq, T0 + s * 128:T0 + (s + 1) * 128, :], in_=xt[:, s, :]),
                                                      reads=[Bxs[s]], writes=[xd[seq][st]]))

        for l in range(L):
            S.barrier()
            phase_M(l)
            if stop_after_M:
                break
            S.barrier()
            phase_F(l, l == L - 1)
        S.emit(final_wait_ops=out_dmas)
    return nc, dbg_out


def _consts():
    p = np.arange(128)[:, None].astype(np.float64)
    q = np.arange(128)[None, :].astype(np.float64)
    c = {}
    c["c_ident"] = np.eye(128, dtype=np.float32)
    tri = np.where(p <= q, 0.0, NEG)
    tri2 = np.where(p > q, 0.0, NEG)
    c["c_tri"] = np.concatenate([tri, tri2], axis=1).astype(np.float32)
    d1 = 30000.0 * (q - 16.0 * p)
    d0 = d1.copy()
    d0[0, :] = -1e9
    c["c_dcmp"] = np.concatenate([d0, d1], axis=1).astype(np.float32)
    ovl = np.zeros((128, 2, 64), np.float32)
    for j in range(2):
        for pp in range(128):
            s = 128 * j + pp
            if s == 0:
                continue
            n = s - 1
            for b in range(64):
                if (16 * n < 64 * b + 64) and (16 * n + 31 >= 64 * b):
                    ovl[pp, j, b] = 1.0
            ovl[pp, j, 0] = 1e6
    c["c_ovl"] = ovl.reshape(128, 128)
    F = np.zeros((128, 128), np.float32)
    for qq in range(128):
        cur = qq // 64
        for j in range(128):
            br = j - 62
            if br == cur:
                F[qq, j] = 3e6
            elif br == cur - 1:
                F[qq, j] = 2e6
            elif br > cur:
                F[qq, j] = -1e9
    c["c_F"] = F
    E = np.zeros((128, 32, 128), np.float32)
    for kt in range(32):
        for pp in range(128):
            E[2 * kt + pp // 64, kt, pp] = 1.0
    c["c_E"] = E.reshape(128, 32 * 128)
    cinv = np.zeros((128, 4, 16), np.float32)
    for gi, w in enumerate(POOL_W):
        for j in range(16):
            cinv[:, gi, j] = 1.0 / min(j + 1, w)
    c["c_cinv"] = cinv.reshape(128, 64)
    return c


def _prep_weights(inp, L):
    perm = list(range(0, 512))
    for r in range(4):
        perm += list(range(512 + r * 64, 512 + r * 64 + 64)) + list(range(512 + (4 + r) * 64, 512 + (4 + r) * 64 + 64))
    perm += list(range(1024, 1152)) + list(range(1152, 1280)) + list(range(1280, 1408)) + list(range(1536, 1664))
    perm += list(range(1408, 1536)) + list(range(1664, 1792)) + list(range(1792, 1816))
    perm = np.asarray(perm)
    out = {}
    out["w_in"] = np.ascontiguousarray(np.asarray(inp["w_in"])[:, :, perm])
    for k in ("w_out", "w_gate", "w_up", "w_down"):
        out[k] = np.ascontiguousarray(np.asarray(inp[k]))
    out["w_pool"] = np.ascontiguousarray(np.asarray(inp["w_pool"]).transpose(0, 2, 1, 3).reshape(L, 128, 512))
    w1 = np.stack([np.asarray(inp["cmp_w1_k"]), np.asarray(inp["cmp_w1_v"])], axis=1)
    out["cmp_w1"] = np.ascontiguousarray(w1.reshape(L, 2, 32, 64, 128).transpose(0, 1, 3, 2, 4).reshape(L, 2, 64, 32 * 128))
    out["cmp_w2"] = np.ascontiguousarray(np.stack([np.asarray(inp["cmp_w2_k"]), np.asarray(inp["cmp_w2_v"])], axis=1))
    pe = np.stack([np.asarray(inp["cmp_pe_k"]), np.asarray(inp["cmp_pe_v"])], axis=1)
    out["cmp_peT"] = np.ascontiguousarray(pe.transpose(0, 1, 3, 2))
    vec = np.zeros((L, 128, 112), np.float32)
    vec[:, :, 0:8] = np.asarray(inp["norm1"]).reshape(L, 8, 128).transpose(0, 2, 1)
    vec[:, :, 8:12] = np.asarray(inp["s_pool"]).reshape(L, 4, 128).transpose(0, 2, 1)
    vec[:, :, 12:16] = np.asarray(inp["norm_pool_out"]).reshape(L, 4, 128).transpose(0, 2, 1)
    vec[:, :, 16:24] = np.asarray(inp["norm2"]).reshape(L, 8, 128).transpose(0, 2, 1)
    cwv = np.asarray(inp["conv_w"]).reshape(L, 3, NFC, 128).transpose(0, 3, 2, 1)
    vec[:, :, 24:90] = cwv.reshape(L, 128, 66)
    vec[:, :, 90:112] = np.asarray(inp["conv_b"]).reshape(L, NFC, 128).transpose(0, 2, 1)
    out["vecs"] = vec
    out["nao"] = np.ascontiguousarray(np.asarray(inp["norm_attn_out"]).reshape(L, 1, 512))
    out["normf"] = np.ascontiguousarray(np.asarray(inp["norm_f"]).reshape(1, D))
    out.update(_consts())
    return {k: np.ascontiguousarray(v, dtype=np.float32) for k, v in out.items()}


_CACHE = {}


def kernel(**inputs):
    x = np.asarray(inputs["x"], dtype=np.float32)
    B, T, _ = x.shape
    L = np.asarray(inputs["w_in"]).shape[0]
    n_cores = 8
    nseq = B // n_cores
    key = (T, nseq, L)
    if key not in _CACHE:
        _CACHE[key] = build(T, nseq, L)[0]
    nc = _CACHE[key]
    shared = _prep_weights(inputs, L)
    in_maps = []
    for c in range(n_cores):
        m = dict(shared)
        m["x"] = np.ascontiguousarray(x[c * nseq:(c + 1) * nseq])
        in_maps.append(m)
    res = run_bass_kernel_spmd(nc, in_maps, core_ids=list(range(n_cores)))
    return np.concatenate([np.asarray(r["y"]) for r in res.results], axis=0).astype(np.float32)
```

```python
import contextlib
import math
import numpy as np
import concourse.bass as bass
import concourse.mybir as mybir
from concourse.bass_utils import run_bass_kernel_spmd

F32 = mybir.dt.float32
BF16 = mybir.dt.bfloat16
AF = mybir.ActivationFunctionType
ALU = mybir.AluOpType

D = 1024
DFF = 2816
NFC = DFF // 128
EPS = 1e-6
NEG = -30000.0
POOL_W = (2, 4, 8, 16)
N_DMA_SEMS = 24


class Buf:
    __slots__ = ("name", "w", "r", "excl")

    def __init__(self, name="", excl=False):
        self.name = name
        self.w = None
        self.r = {}
        self.excl = excl


class Op:
    __slots__ = ("eng", "pos", "fn", "deps", "needed", "sem", "value", "is_dma")

    def __init__(self, eng, pos, fn, is_dma):
        self.eng = eng
        self.pos = pos
        self.fn = fn
        self.deps = []
        self.needed = False
        self.sem = None
        self.value = None
        self.is_dma = is_dma


class Sched:
    ENGS = ("pe", "act", "dve", "pool", "sp")

    def __init__(self, nc):
        self.nc = nc
        self.ops = {e: [] for e in self.ENGS}
        self.waited = {e: {} for e in self.ENGS}
        self.n_dma = 0
        self.dma_ops = []
        self.barrier_deps = {e: [] for e in self.ENGS}

    def barrier(self):
        last = []
        for e in self.ENGS:
            for op in reversed(self.ops[e]):
                if not op.is_dma:
                    last.append(op)
                    break
        last += self.dma_ops[-N_DMA_SEMS:]
        for e in self.ENGS:
            self.barrier_deps[e] = list(last)

    def _add(self, eng, fn, reads, writes, is_dma=False):
        lst = self.ops[eng]
        op = Op(eng, len(lst), fn, is_dma)
        deps = {}

        def need(d, same_ok):
            if d is None:
                return
            if d.eng == eng and (not d.is_dma) and same_ok:
                return
            deps[id(d)] = d

        for b in reads:
            need(b.w, False)
            if b.excl:
                for d in b.r.values():
                    need(d, True)
        for b in writes:
            need(b.w, True)
            for d in b.r.values():
                need(d, True)
        if self.barrier_deps[eng]:
            for d in self.barrier_deps[eng]:
                if not (d.eng == eng and not d.is_dma):
                    deps[id(d)] = d
            self.barrier_deps[eng] = []
        wt = self.waited[eng]
        if is_dma:
            idx = self.n_dma
            self.n_dma += 1
            if idx >= N_DMA_SEMS:
                prev = self.dma_ops[idx - N_DMA_SEMS]
                deps[id(prev)] = prev
        for d in deps.values():
            if d.is_dma:
                key = ("dma", d.pos % N_DMA_SEMS)
            else:
                key = d.eng
            if wt.get(key, -1) >= d.pos:
                continue
            wt[key] = d.pos
            d.needed = True
            op.deps.append(d)
        if is_dma:
            op.pos = idx
            op.needed = True
            self.dma_ops.append(op)
        for b in reads:
            b.r[("dma", op.pos) if is_dma else eng] = op
        for b in writes:
            b.w = op
            b.r = {}
        lst.append(op)
        return op

    def pe(self, fn, reads=(), writes=()):
        return self._add("pe", fn, reads, writes)

    def act(self, fn, reads=(), writes=()):
        return self._add("act", fn, reads, writes)

    def dve(self, fn, reads=(), writes=()):
        return self._add("dve", fn, reads, writes)

    def pool(self, fn, reads=(), writes=()):
        return self._add("pool", fn, reads, writes)

    def dma(self, fn, reads=(), writes=(), q="sp"):
        return self._add(q, fn, reads, writes, is_dma=True)

    def emit(self, final_wait_ops=()):
        nc = self.nc
        with contextlib.ExitStack() as es:
            esem = {e: es.enter_context(nc.semaphore("s_" + e)) for e in self.ENGS}
            dsem = [es.enter_context(nc.semaphore("d%d" % i)) for i in range(N_DMA_SEMS)]
            for e in self.ENGS:
                cnt = 0
                for op in self.ops[e]:
                    if op.is_dma:
                        op.sem = dsem[op.pos % N_DMA_SEMS]
                        op.value = 16 * (op.pos // N_DMA_SEMS + 1)
                    elif op.needed:
                        cnt += 1
                        op.sem = esem[e]
                        op.value = cnt
            block = es.enter_context(nc.Block())
            decos = {"pe": block.tensor, "act": block.scalar, "dve": block.vector,
                     "pool": block.gpsimd, "sp": block.sync}

            def mk(e, extra):
                def body(eng):
                    for op in self.ops[e]:
                        for d in op.deps:
                            eng.wait_ge(d.sem, d.value)
                        ins = op.fn(eng)
                        if op.is_dma:
                            ins.then_inc(op.sem, 16)
                        elif op.needed:
                            ins.then_inc(op.sem, 1)
                    for d in extra:
                        eng.wait_ge(d.sem, d.value)
                return body

            for e in self.ENGS:
                extra = list(final_wait_ops) if e == "sp" else []
                if not self.ops[e] and not extra:
                    continue
                decos[e](mk(e, extra))


def _ap(t, off, dims):
    return bass.AP(t.tensor if hasattr(t, "tensor") else t, off, [list(d) for d in dims])


def build(T, NSEQ, L, debug=False, stop_after_M=False):
    NT = T // 128
    NST = T // 512
    NSLOT = T // 16
    NCT = NSLOT // 128
    nc = bass.Bass("TRN2", target_bir_lowering=False)

    def din(name, shape):
        return nc.dram_tensor(name, list(shape), F32, kind="ExternalInput").ap()

    x_in = din("x", [NSEQ, T, D])
    w_in = din("w_in", [L, D, 1816])
    w_out = din("w_out", [L, D, D])
    w_gate = din("w_gate", [L, D, DFF])
    w_up = din("w_up", [L, D, DFF])
    w_down = din("w_down", [L, DFF, D])
    w_pool = din("w_pool", [L, 128, 4 * 128])
    w1 = din("cmp_w1", [L, 2, 64, 32 * 128])
    w2 = din("cmp_w2", [L, 2, 128, 64])
    peT = din("cmp_peT", [L, 2, 64, 32])
    vecs = din("vecs", [L, 128, 112])
    nao = din("nao", [L, 1, 512])
    normf = din("normf", [1, D])
    c_ident = din("c_ident", [128, 128])
    c_tri = din("c_tri", [128, 256])
    c_dcmp = din("c_dcmp", [128, 256])
    c_ovl = din("c_ovl", [128, 128])
    c_F = din("c_F", [128, 128])
    c_E = din("c_E", [128, 32 * 128])
    c_cinv = din("c_cinv", [128, 64])
    y_out = nc.dram_tensor("y", [NSEQ, T, D], F32, kind="ExternalOutput").ap()
    xbuf = nc.dram_tensor("xbuf", [NSEQ, T, D], F32).ap()
    dbg_out = {}
    if debug:
        dbg_attn = nc.dram_tensor("dbg_attn", [T, 512], F32, kind="ExternalOutput").ap()
        dbg_imp = nc.dram_tensor("dbg_imp", [T, 2, 64], F32, kind="ExternalOutput").ap()
        dbg_selb = nc.dram_tensor("dbg_selb", [T, 2, 64], BF16, kind="ExternalOutput").ap()

    S = Sched(nc)
    out_dmas = []

    with contextlib.ExitStack() as top:
        def sb(name, shape, dt=F32, es=top):
            return es.enter_context(nc.sbuf_tensor(name, list(shape), dt))

        banks = [top.enter_context(nc.psum_tensor("bank%d" % i, [128, 512], F32)) for i in range(8)]
        bbuf = [Buf("bank%d" % i, excl=True) for i in range(8)]
        rr = {"mm": [0, 1], "s": [2, 3], "acc": [4, 5]}
        rr_i = {"mm": 0, "s": 0, "acc": 0}

        def bank(kind):
            i = rr[kind][rr_i[kind] % len(rr[kind])]
            rr_i[kind] += 1
            return banks[i], bbuf[i]

        ident = sb("ident", [128, 128], BF16); Bconst = Buf("const")
        ones = sb("ones", [128, 128], BF16)
        epsc = sb("epsc", [128, 1], F32)
        S.dma(lambda e: e.dma_start(out=ident[:], in_=c_ident), writes=[Bconst], q="pool")
        S.dve(lambda e: e.memset(ones[:], 1.0), writes=[Bconst])
        S.dve(lambda e: e.memset(epsc[:], EPS), writes=[Bconst])

        xd = [[Buf("xd%d_%d" % (s, i)) for i in range(NST)] for s in range(NSEQ)]

        def x_src(l, seq):
            return x_in if l == 0 else xbuf

        def dbg(name, ap_sb, shape, reads):
            if not debug:
                return
            t = nc.dram_tensor("dbg_" + name, list(shape), ap_sb.dtype, kind="ExternalOutput").ap()
            dbg_out[name] = t
            out_dmas.append(S.dma(lambda e: e.dma_start(out=t, in_=ap_sb), reads=reads))

        def rstd_from_ss(ss_ap, out_ap, n, Bss, Bout):
            S.act(lambda e: e.activation(out_ap, ss_ap, AF.Ln, scale=1.0 / n, bias=epsc[0:ss_ap.shape[0], 0:1]),
                  reads=[Bss, Bconst], writes=[Bout])
            S.act(lambda e: e.activation(out_ap, out_ap, AF.Exp, scale=-0.5), reads=[Bout], writes=[Bout])

        def phase_M(l):
            with contextlib.ExitStack() as es:
                def t(name, shape, dt=F32):
                    return sb("M%d_" % l + name, shape, dt, es)
                wfm = t("wfm", [128, 8, 1536], BF16); wtm = t("wtm", [128, 8, 280], BF16)
                wo = t("wo", [128, 8, D], BF16); wpl = t("wpl", [128, 512], BF16)
                w1k = t("w1k", [128, 32 * 128], BF16); w1v = t("w1v", [128, 32 * 128], BF16)
                w2k = t("w2k", [128, 128], BF16); w2v = t("w2v", [128, 64], BF16)
                pet = t("pet", [64, 64], BF16); peb = t("peb", [128, 2], F32)
                vc_ = t("vecs", [128, 112], F32); naob = t("naob", [128, 512], F32)
                Bw = Buf("Mweights")
                tri = t("tri", [128, 256], BF16); dcmp = t("dcmp", [128, 256], F32); ovl = t("ovl", [128, 128], BF16)
                Fc = t("Fc", [128, 128], F32); Ec = t("Ec", [128, 32 * 128], BF16); cinv = t("cinv", [128, 64], F32)
                for dst, src in ((tri, c_tri), (ovl, c_ovl), (Ec, c_E)):
                    S.dma(lambda e, dst=dst, src=src: e.dma_start(out=dst[:], in_=src), writes=[Bconst], q="pool")
                for dst, src in ((dcmp, c_dcmp), (Fc, c_F), (cinv, c_cinv)):
                    S.dma(lambda e, dst=dst, src=src: e.dma_start(out=dst[:], in_=src), writes=[Bconst])
                kselT = t("kselT", [128, T], BF16); kwinT = t("kwinT", [128, T], BF16)
                vselA = t("vselA", [128, NT, 2, 65], BF16); vwinA = t("vwinA", [128, NT, 2, 65], BF16)
                kcmpT = t("kcmpT", [128, NSLOT], BF16); vcmpA = t("vcmpA", [128, NCT, 2, 65], BF16)
                glv = t("glv", [128, 2, NSLOT], BF16)
                Bksel, Bkwin, Bvsel, Bvwin = Buf("ksel"), Buf("kwin"), Buf("vsel"), Buf("vwin")
                Bkcmp, Bvcmp, Bglv = Buf("kcmp"), Buf("vcmp"), Buf("glv")
                xt = t("xt", [128, 4, D], F32); Bxt = Buf("xt")
                xn = [t("xn%d" % i, [128, D], BF16) for i in range(2)]; Bxn = [Buf("xn0"), Buf("xn1")]
                ss = t("ss", [128, 8], F32); Bss = Buf("ss")
                rs = t("rs", [128, 8], F32); Brs = Buf("rs")
                junk = t("junk", [128, D], BF16); Bjunk = Buf("junk")
                hT = t("hT", [128, 8, 512], BF16); BhT = Buf("hT")
                uT = t("uT", [128, 4, 528], F32); BuT = Buf("uT")
                qT = t("qT", [128, 4, 4, 128], BF16); BqT = Buf("qT")
                kcT = t("kcT", [128, 528], BF16); vcT = t("vcT", [128, 528], BF16); BkcT = Buf("kcT"); BvcT = Buf("vcT")
                gat = t("gat", [128, 4, 24], F32); Bgat = Buf("gat")
                ptA = t("ptA", [128, 528], F32); ptB = t("ptB", [128, 528], F32); BptA = Buf("ptA"); BptB = Buf("ptB")
                pooled = [t("pooled%d" % i, [128, 512], BF16) for i in range(2)]; Bpooled = [Buf(), Buf()]
                po = t("po", [128, 4, 512], F32); Bpo = Buf("po")
                sq = [t("sq%d" % i, [128, 512], BF16) for i in range(2)]; Bsq = [Buf(), Buf()]
                rbc = t("rbc", [128, 512], F32); Brbc = Buf("rbc")
                mixT = t("mixT", [128, 8, 512], BF16); BmixT = Buf("mixT")
                PT = [t("PT%d" % i, [128, 512], BF16) for i in range(3)]; BPT = [Buf(), Buf(), Buf()]
                pt_i = [0]
                o2 = [t("o%d" % i, [128, 512], F32) for i in range(2)]; Bo2 = [Buf(), Buf()]
                on2 = [t("on%d" % i, [128, 512], BF16) for i in range(2)]; Bon2 = [Buf(), Buf()]
                otmps = [t("otmp%d" % i, [128, 256], F32) for i in range(2)]; Botmps = [Buf(), Buf()]
                hb = t("hb", [128, 128], F32); Bhb = Buf("hb")
                glt = t("glt", [128, 128], BF16); Bglt = Buf("glt")
                cbs = [t("cb%d" % i, [128, 128], BF16) for i in range(2)]; Bcbs = [Buf(), Buf()]
                sm = t("sm", [128, 64], F32); Bsm = Buf("sm")
                smx = [t("smx%d" % i, [128, 16], F32) for i in range(6)]; Bsmx = [Buf() for i in range(6)]
                imps = [t("imp%d" % i, [128, 64], F32) for i in range(2)]; Bimps = [Buf(), Buf()]
                imp2s = [t("impb%d" % i, [128, 64], F32) for i in range(2)]; Bimp2s = [Buf(), Buf()]
                m8s = [t("m8%d" % i, [128, 16], F32) for i in range(2)]; Bm8s = [Buf(), Buf()]
                selbs = [t("selb%d" % i, [128, 64], BF16) for i in range(2)]; Bselbs = [Buf(), Buf()]
                selbTs = [t("selbT%d" % i, [128, 128], BF16) for i in range(2)]; BselbTs = [Buf(), Buf()]
                ssa2 = [t("ssa%d" % i, [128, 2], F32) for i in range(2)]; Bssa2 = [Buf(), Buf()]

                def wdma(dst_ap, src_ap):
                    S.dma(lambda e: e.dma_start(out=dst_ap, in_=src_ap), writes=[Bw], q="pool")
                wv = w_in[l].rearrange("(k p) c -> p k c", p=128)
                for k in range(8):
                    wdma(wfm[:, k, :], wv[:, k, 0:1536])
                wdma(wtm[:, :, :], wv[:, :, 1536:1816])
                wov = w_out[l].rearrange("(k p) c -> p k c", p=128)
                for k in range(8):
                    wdma(wo[:, k, :], wov[:, k, :])
                wdma(wpl[:], w_pool[l])
                for h in range(2):
                    wdma(w1k[64 * h:64 * h + 64, :], w1[l, 0])
                    wdma(w1v[64 * h:64 * h + 64, :], w1[l, 1])
                    wdma(w2k[:, 64 * h:64 * h + 64], w2[l, 0])
                wdma(w2v[:], w2[l, 1])
                wdma(pet[:, 0:32], peT[l, 0]); wdma(pet[:, 32:64], peT[l, 1])
                S.dma(lambda e: e.dma_start(out=vc_[:], in_=vecs[l]), writes=[Bw])
                S.dma(lambda e: e.dma_start(out=naob[:], in_=nao[l].partition_broadcast(128)), writes=[Bw])
                g1col = vc_[:, 0:8]; spool = vc_[:, 8:12]; npo = vc_[:, 12:16]

                S.pool(lambda e: e.memset(vselA[:], 1.0), writes=[Bvsel])
                S.pool(lambda e: e.memset(vwinA[:], 1.0), writes=[Bvwin])
                S.pool(lambda e: e.memset(vcmpA[:], 1.0), writes=[Bvcmp])
                S.pool(lambda e: e.memset(kcmpT[:], 0.0), writes=[Bkcmp])
                S.pool(lambda e: e.memset(glv[:], 0.0), writes=[Bglv])
                S.pool(lambda e: e.memset(kselT[:], 0.0), writes=[Bksel])
                S.pool(lambda e: e.memset(kwinT[:], 0.0), writes=[Bkwin])
                for g_ in range(2):
                    S.pool(lambda e, g_=g_: e.memset(selbTs[g_][:], 0.0), writes=[BselbTs[g_]])
                bk, Bb = banks[7], bbuf[7]
                for kv, wt_ in ((0, w1k), (1, w1v)):
                    for li in range(32):
                        S.pe(lambda e, kv=kv, wt_=wt_, li=li: e.matmul(bk[:, kv:kv + 1], wt_[0:64, li * 128:(li + 1) * 128],
                                                                      pet[:, kv * 32 + li:kv * 32 + li + 1], start=(li == 0), stop=(li == 31)),
                             reads=[Bw], writes=[Bb])
                S.dve(lambda e: e.tensor_copy(peb[:], bk[:, 0:2]), reads=[Bb], writes=[Bw])

                for seq in range(NSEQ):
                    xs = x_src(l, seq)
                    S.pool(lambda e: e.memset(uT[:, :, 0:16], 0.0), writes=[BuT])
                    S.pool(lambda e: e.memset(kcT[:, 0:16], 0.0), writes=[BkcT])
                    S.pool(lambda e: e.memset(vcT[:, 0:16], 0.0), writes=[BvcT])
                    for st in range(NST):
                        T0 = st * 512
                        S.dma(lambda e, xs=xs, seq=seq, T0=T0: e.dma_start(
                            out=xt[:], in_=xs[seq, T0:T0 + 512, :].rearrange("(s p) d -> p s d", p=128)),
                            reads=[xd[seq][st]], writes=[Bxt])
                        if st > 0:
                            S.pool(lambda e: e.tensor_copy(uT[:, :, 0:16], uT[:, :, 512:528]), reads=[BuT], writes=[BuT])
                            S.pool(lambda e: e.tensor_copy(kcT[:, 0:16], kcT[:, 512:528]), reads=[BkcT], writes=[BkcT])
                            S.pool(lambda e: e.tensor_copy(vcT[:, 0:16], vcT[:, 512:528]), reads=[BvcT], writes=[BvcT])
                        S.dve(lambda e: e.memset(ss[:], 0.0), writes=[Bss])
                        for s in range(4):
                            S.act(lambda e, s=s: e.activation(junk[:], xt[:, s, :], AF.Square, accum_out=ss[:, s:s + 1]),
                                  reads=[Bxt], writes=[Bjunk, Bss])
                        rstd_from_ss(ss[:, 0:4], rs[:, 0:4], D, Bss, Brs)
                        for s in range(4):
                            S.dve(lambda e, s=s: e.tensor_scalar(xn[s % 2][:], xt[:, s, :], rs[:, s:s + 1], None, ALU.mult),
                                  reads=[Bxt, Brs], writes=[Bxn[s % 2]])
                            for kh in range(2):
                                bk, Bb = bank("mm")
                                for kk in range(4):
                                    k = kh * 4 + kk
                                    S.pe(lambda e, s=s, k=k, kk=kk, bk=bk: e.matmul(bk[:, kk * 128:(kk + 1) * 128], xn[s % 2][:, k * 128:(k + 1) * 128],
                                                                                   ident[:], start=True, stop=True),
                                         reads=[Bxn[s % 2], Bconst], writes=[Bb])
                                for kk in range(4):
                                    k = kh * 4 + kk
                                    eng = S.act if kk % 2 == 0 else S.dve
                                    if kk % 2 == 0:
                                        S.act(lambda e, s=s, k=k, kk=kk, bk=bk: e.activation(hT[:, k, s * 128:(s + 1) * 128], bk[:, kk * 128:(kk + 1) * 128],
                                                                                          AF.Copy, scale=g1col[:, k:k + 1]),
                                              reads=[Bb, Bw], writes=[BhT])
                                    else:
                                        S.dve(lambda e, s=s, k=k, kk=kk, bk=bk: e.tensor_scalar(hT[:, k, s * 128:(s + 1) * 128], bk[:, kk * 128:(kk + 1) * 128],
                                                                                              g1col[:, k:k + 1], None, ALU.mult),
                                              reads=[Bb, Bw], writes=[BhT])
                        for c in range(12):
                            bk, Bb = bank("mm")
                            for k in range(8):
                                S.pe(lambda e, c=c, k=k, bk=bk: e.matmul(bk[:], wfm[:, k, c * 128:(c + 1) * 128], hT[:, k, :], start=(k == 0), stop=(k == 7)),
                                     reads=[Bw, BhT], writes=[Bb])
                            if c < 4:
                                S.act(lambda e, c=c, bk=bk: e.activation(uT[:, c, 16:528], bk[:], AF.Copy), reads=[Bb], writes=[BuT])
                            elif c < 8:
                                r = c - 4
                                dst = _ap(qT, r * 128, [[2048, 128], [512, 4], [1, 128]])
                                src = _ap(bk, 0, [[512, 128], [128, 4], [1, 128]])
                                S.dve(lambda e, dst=dst, src=src: e.tensor_copy(dst, src), reads=[Bb], writes=[BqT])
                            elif c == 8:
                                S.act(lambda e, bk=bk: e.activation(kcT[:, 16:528], bk[:], AF.Copy), reads=[Bb], writes=[BkcT])
                            elif c == 9:
                                S.dve(lambda e, bk=bk: e.tensor_copy(vcT[:, 16:528], bk[:]), reads=[Bb], writes=[BvcT])
                            elif c == 10:
                                S.act(lambda e, bk=bk, T0=T0: e.activation(kselT[:, T0:T0 + 512], bk[:], AF.Copy), reads=[Bb], writes=[Bksel])
                            else:
                                S.dve(lambda e, bk=bk, T0=T0: e.tensor_copy(kwinT[:, T0:T0 + 512], bk[:]), reads=[Bb], writes=[Bkwin])
                        for s in range(4):
                            ti = st * 4 + s
                            bk, Bb = bank("mm")
                            for k in range(8):
                                S.pe(lambda e, s=s, k=k, bk=bk: e.matmul(bk[:, 0:280], hT[:, k, s * 128:(s + 1) * 128], wtm[:, k, :], start=(k == 0), stop=(k == 7)),
                                     reads=[Bw, BhT], writes=[Bb])
                            src = _ap(bk, 0, [[512, 128], [64, 2], [1, 64]])
                            S.act(lambda e, ti=ti, src=src: e.activation(vselA[:, ti, :, 0:64], src, AF.Copy), reads=[Bb], writes=[Bvsel])
                            src2 = _ap(bk, 128, [[512, 128], [64, 2], [1, 64]])
                            S.dve(lambda e, ti=ti, src2=src2: e.tensor_copy(vwinA[:, ti, :, 0:64], src2), reads=[Bb], writes=[Bvwin])
                            S.act(lambda e, s=s, bk=bk: e.activation(gat[:, s, :], bk[:, 256:280], AF.Exp, scale=-1.0), reads=[Bb], writes=[Bgat])
                            S.dve(lambda e, s=s: e.tensor_scalar(gat[:, s, :], gat[:, s, :], 1.0, None, ALU.add), reads=[Bgat], writes=[Bgat])
                            S.dve(lambda e, s=s: e.reciprocal(gat[:, s, :], gat[:, s, :]), reads=[Bgat], writes=[Bgat])
                        bst, Bbst = banks[6], bbuf[6]
                        for p in range(4):
                            w = POOL_W[p]
                            cur, Bcur = uT[:, p, :], BuT
                            tmp = [(ptA, BptA), (ptB, BptB)]
                            step = 1
                            i = 0
                            while step < w:
                                dstt, Bd = tmp[i % 2]
                                lo = 2 * step - 1
                                S.pool(lambda e, dstt=dstt, cur=cur, lo=lo, step=step: e.tensor_tensor(dstt[:, lo:528], cur[:, lo:528], cur[:, lo - step:528 - step], ALU.add),
                                       reads=[Bcur], writes=[Bd])
                                cur, Bcur = dstt[:, :], Bd
                                step *= 2
                                i += 1
                            pl, Bpl = pooled[p % 2], Bpooled[p % 2]
                            S.dve(lambda e, pl=pl, cur=cur, p=p, w=w: e.scalar_tensor_tensor(pl[:], cur[:, 16:528], 1.0 / w, uT[:, p, 16:528], ALU.mult, ALU.subtract),
                                   reads=[Bcur, BuT], writes=[Bpl])
                            if st == 0:
                                S.pool(lambda e, cur=cur, p=p: e.tensor_tensor(sm[:, 0:16], cur[:, 16:32], cinv[:, p * 16:(p + 1) * 16], ALU.mult),
                                       reads=[Bcur, Bconst], writes=[Bsm])
                                S.pool(lambda e, pl=pl, p=p: e.tensor_tensor(pl[:, 0:16], sm[:, 0:16], uT[:, p, 16:32], ALU.subtract),
                                       reads=[Bsm, BuT], writes=[Bpl])
                            bk, Bb = bank("mm")
                            S.pe(lambda e, p=p, pl=pl, bk=bk: e.matmul(bk[:], wpl[:, p * 128:(p + 1) * 128], pl[:], start=True, stop=True),
                                 reads=[Bw, Bpl], writes=[Bb])
                            S.act(lambda e, p=p, bk=bk: e.activation(po[:, p, :], bk[:], AF.Copy, scale=spool[:, p:p + 1]), reads=[Bb, Bw], writes=[Bpo])
                            S.dve(lambda e, p=p: e.tensor_tensor(sq[p % 2][:], po[:, p, :], po[:, p, :], ALU.mult), reads=[Bpo], writes=[Bsq[p % 2]])
                            S.pe(lambda e, p=p: e.matmul(bst[:], ones[:], sq[p % 2][:], start=(p == 0), stop=(p == 3)),
                                 reads=[Bconst, Bsq[p % 2]], writes=[Bbst])
                        rstd_from_ss(bst[:], rbc[:], 512, Bbst, Brbc)
                        for p in range(4):
                            S.dve(lambda e, p=p: e.scalar_tensor_tensor(mixT[:, p, :], po[:, p, :], npo[:, p:p + 1], rbc[:], ALU.mult, ALU.mult),
                                  reads=[Bpo, Bw, Brbc], writes=[BmixT])
                        s0 = 32 * st
                        for g in range(2):
                            bk, Bb = banks[6 + g], bbuf[6 + g]
                            R0 = 64 * g
                            for kv, (src_t, Bsrc, wt_) in enumerate(((kcT, BkcT, w1k), (vcT, BvcT, w1v))):
                                for li in range(32):
                                    S.pe(lambda e, kv=kv, src_t=src_t, wt_=wt_, li=li, R0=R0, bk=bk: e.matmul(
                                        bk[:, kv * 32:(kv + 1) * 32], wt_[R0:R0 + 64, li * 128:(li + 1) * 128],
                                        src_t[R0:R0 + 64, li:li + 16 * 31 + 1:16], start=(li == 0), stop=(li == 31)),
                                        reads=[Bw, Bsrc], writes=[Bb])
                            for kv in range(2):
                                S.act(lambda e, kv=kv, g=g, bk=bk: e.activation(glt[:, g * 64 + kv * 32:g * 64 + kv * 32 + 32], bk[:, kv * 32:(kv + 1) * 32],
                                                                            AF.Gelu_apprx_tanh, bias=peb[:, kv:kv + 1]),
                                      reads=[Bb, Bw], writes=[Bglt])
                            S.dve(lambda e, g=g, s0=s0: e.tensor_copy(glv[:, g, s0:s0 + 32], glt[:, g * 64 + 32:g * 64 + 64]), reads=[Bglt], writes=[Bglv])
                        for g in range(2):
                            bk, Bb = bank("mm")
                            S.pe(lambda e, g=g, bk=bk: e.matmul(bk[:, 0:32], w2k[:], glt[:, g * 64:g * 64 + 32], start=True, stop=True),
                                 reads=[Bw, Bglt], writes=[Bb])
                            S.act(lambda e, g=g, bk=bk, s0=s0: e.activation(kcmpT[64 * g:64 * g + 64, s0:s0 + 32], bk[64 * g:64 * g + 64, 0:32], AF.Copy),
                                  reads=[Bb], writes=[Bkcmp])
                            jt = st // 4
                            S.pe(lambda e, g=g, bk=bk, jt=jt: e.matmul(bk[:, 64:128], glv[:, g, jt * 128:(jt + 1) * 128], w2v[:], start=True, stop=True),
                                 reads=[Bw, Bglv], writes=[Bb])
                            S.dve(lambda e, g=g, bk=bk, jt=jt: e.tensor_copy(vcmpA[:, jt, g, 0:64], bk[:, 64:128]), reads=[Bb], writes=[Bvcmp])
                        pipe = {"pending": None, "delayed": []}

                        def step_delayed():
                            nd = []
                            for cnt, fn in pipe["delayed"]:
                                if cnt <= 0:
                                    fn()
                                else:
                                    nd.append((cnt - 1, fn))
                            pipe["delayed"] = nd

                        def finish_prev():
                            prev = pipe["pending"]
                            if prev is not None:
                                prev["pv_fn"](prev["P"])
                                if prev["post"] is not None:
                                    prev["post"]()
                            pipe["pending"] = None

                        def run_task(task):
                            for f in task["pre"]:
                                f()
                            P = task["s_fn"]()
                            finish_prev()
                            task["P"] = P
                            pipe["pending"] = task
                            step_delayed()

                        for s in range(4):
                            for task in attn_tasks(l, seq, st, s, pipe, locals()):
                                run_task(task)
                        finish_prev()
                        while pipe["delayed"]:
                            step_delayed()
                        for s in range(4):
                            for nh in range(2):
                                bk, Bb = bank("mm")
                                for k in range(8):
                                    S.pe(lambda e, s=s, nh=nh, k=k, bk=bk: e.matmul(bk[:], mixT[:, k, s * 128:(s + 1) * 128], wo[:, k, nh * 512:(nh + 1) * 512],
                                                                                 start=(k == 0), stop=(k == 7)),
                                         reads=[BmixT, Bw], writes=[Bb])
                                S.dve(lambda e, s=s, nh=nh, bk=bk: e.tensor_tensor(xt[:, s, nh * 512:(nh + 1) * 512], xt[:, s, nh * 512:(nh + 1) * 512], bk[:], ALU.add),
                                      reads=[Bb, Bxt], writes=[Bxt])
                        xdst = y_out if stop_after_M else xbuf
                        od = S.dma(lambda e, seq=seq, T0=T0, xdst=xdst: e.dma_start(out=xdst[seq, T0:T0 + 512, :].rearrange("(s p) d -> p s d", p=128), in_=xt[:]),
                                   reads=[Bxt], writes=[xd[seq][st]])
                        if stop_after_M:
                            out_dmas.append(od)

        def attn_tasks(l, seq, st, s, pipe, L_):
            qT = L_["qT"]; BqT = L_["BqT"]; PT = L_["PT"]; BPT = L_["BPT"]; pt_i = L_["pt_i"]
            kcmpT, Bkcmp, vcmpA, Bvcmp = L_["kcmpT"], L_["Bkcmp"], L_["vcmpA"], L_["Bvcmp"]
            kselT, Bksel, vselA, Bvsel = L_["kselT"], L_["Bksel"], L_["vselA"], L_["Bvsel"]
            kwinT, Bkwin, vwinA, Bvwin = L_["kwinT"], L_["Bkwin"], L_["vwinA"], L_["Bvwin"]
            cbs, Bcbs, smx, Bsmx = L_["cbs"], L_["Bcbs"], L_["smx"], L_["Bsmx"]
            imps, Bimps, imp2s, Bimp2s, m8s, Bm8s = L_["imps"], L_["Bimps"], L_["imp2s"], L_["Bimp2s"], L_["m8s"], L_["Bm8s"]
            selbs, Bselbs, selbTs, BselbTs = L_["selbs"], L_["Bselbs"], L_["selbTs"], L_["BselbTs"]
            gat, Bgat, o2, Bo2, otmps, Botmps = L_["gat"], L_["Bgat"], L_["o2"], L_["Bo2"], L_["otmps"], L_["Botmps"]
            tri, dcmp, ovl, Fc, Ec = L_["tri"], L_["dcmp"], L_["ovl"], L_["Fc"], L_["Ec"]
            ssa2, Bssa2, on2, Bon2, junk, Bjunk = L_["ssa2"], L_["Bssa2"], L_["on2"], L_["Bon2"], L_["junk"], L_["Bjunk"]
            naob, Bw, mixT, BmixT = L_["naob"], L_["Bw"], L_["mixT"], L_["BmixT"]
            qi = st * 4 + s
            t0 = qi * 128
            ncmp = 1 if (NCT == 1 or qi < 16) else 2
            ob = s % 2
            o_t, Bo = o2[ob], Bo2[ob]
            tasks = []

            def mk_s(kT_ap, Bk, bias_fns, qrhs):
                def s_fn():
                    bk, Bb = bank("s")
                    n = len(bias_fns)
                    for i, (fn, rd) in enumerate(bias_fns):
                        S.pe(lambda e, fn=fn, i=i, bk=bk: fn(e, bk, i == 0), reads=rd, writes=[Bb])
                    S.pe(lambda e, bk=bk, n=n: e.matmul(bk[:], kT_ap, qrhs, start=(n == 0), stop=True), reads=[Bk, BqT], writes=[Bb])
                    i = pt_i[0] % 3
                    pt_i[0] += 1
                    S.act(lambda e, bk=bk, i=i: e.activation(PT[i][:], bk[:], AF.Exp, scale=0.125), reads=[Bb], writes=[BPT[i]])
                    return (PT[i], BPT[i])
                return s_fn

            def mk_pv(acc, Bacc, v_ap, Bv, first, last, impj=None, bimp=None, Bbimp=None, nj=1):
                def pv_fn(PP):
                    P, BP = PP
                    for r in range(4):
                        S.pe(lambda e, r=r: e.matmul(acc[:, r * 65:(r + 1) * 65], P[:, r * 128:(r + 1) * 128], v_ap, start=(first and r == 0), stop=last),
                             reads=[BP, Bv], writes=[Bacc])
                    if impj is not None:
                        for r in range(4):
                            S.pe(lambda e, r=r: e.matmul(bimp[:, r * 64:(r + 1) * 64], P[:, r * 128:(r + 1) * 128], ovl[:, impj * 64:(impj + 1) * 64],
                                                        start=(impj == 0 and r == 0), stop=(impj == nj - 1)),
                                 reads=[BP, Bconst], writes=[Bbimp])
                return pv_fn

            def combine(acc, Bacc, g, b, bi, first):
                sm, Bsm = smx[bi], Bsmx[bi]
                sums = _ap(acc, 64, [[512, 128], [65, 4]])
                S.dve(lambda e: e.tensor_scalar(sm[:, 0:4], sums, 1e-30, None, ALU.max), reads=[Bacc], writes=[Bsm])
                S.dve(lambda e: e.reciprocal(sm[:, 4:8], sm[:, 0:4]), reads=[Bsm], writes=[Bsm])
                gsl = _ap(gat, s * 24 + g * 12 + b, [[96, 128], [3, 4]])
                S.dve(lambda e: e.tensor_tensor(sm[:, 8:12], sm[:, 4:8], gsl, ALU.mult), reads=[Bsm, Bgat], writes=[Bsm])
                in0 = _ap(acc, 0, [[512, 128], [65, 4], [1, 64]])
                in1 = _ap(sm, 8, [[16, 128], [1, 4], [0, 64]])
                if first:
                    S.dve(lambda e: e.tensor_tensor(_ap(o_t, g * 256, [[512, 128], [64, 4], [1, 64]]), in0, in1, ALU.mult), reads=[Bacc, Bsm], writes=[Bo])
                else:
                    ot_, Bot_ = otmps[g], Botmps[g]
                    S.dve(lambda e: e.tensor_tensor(_ap(ot_, 0, [[256, 128], [64, 4], [1, 64]]), in0, in1, ALU.mult), reads=[Bacc, Bsm], writes=[Bot_])
                    S.pool(lambda e: e.tensor_tensor(o_t[:, g * 256:(g + 1) * 256], o_t[:, g * 256:(g + 1) * 256], ot_[:], ALU.add),
                           reads=[Bot_, Bo], writes=[Bo])

            def rep4(t_, off, pstride):
                return _ap(t_, off, [[pstride, 128], [0, 4], [1, 128]])

            pre_cb = []
            for j in range(ncmp):
                cval = 30000.0 * (t0 - 2048 * j - 15)
                pre_cb.append(lambda j=j, cval=cval: S.dve(lambda e: e.tensor_scalar(cbs[j][:], dcmp[:, j * 128:(j + 1) * 128], cval, 0.0, ALU.add, ALU.min),
                                                           reads=[Bconst], writes=[Bcbs[j]]))
            for g in range(2):
                R0 = 64 * g
                qrhs = qT[R0:R0 + 64, s, :, :]
                acc, Bacc = bank("acc")
                bimp, Bbimp = banks[6 + g], bbuf[6 + g]

                def post_cmp(acc=acc, Bacc=Bacc, g=g, bimp=bimp, Bbimp=Bbimp):
                    combine(acc, Bacc, g, 0, g, True)
                    sm, Bsm = smx[g], Bsmx[g]
                    imp, Bimp, imp2, Bimp2, m8, Bm8, selb, Bselb = imps[g], Bimps[g], imp2s[g], Bimp2s[g], m8s[g], Bm8s[g], selbs[g], Bselbs[g]
                    for r in range(4):
                        if r == 0:
                            S.dve(lambda e: e.tensor_scalar(imp[:], bimp[:, 0:64], sm[:, 4:5], None, ALU.mult), reads=[Bbimp, Bsm], writes=[Bimp])
                        else:
                            S.dve(lambda e, r=r: e.scalar_tensor_tensor(imp[:], bimp[:, r * 64:(r + 1) * 64], sm[:, 4 + r:5 + r], imp[:], ALU.mult, ALU.add),
                                  reads=[Bbimp, Bsm, Bimp], writes=[Bimp])
                    S.dve(lambda e: e.tensor_tensor(imp[:], imp[:], Fc[:, 62 - 2 * qi:126 - 2 * qi], ALU.add), reads=[Bimp, Bconst], writes=[Bimp])
                    S.dve(lambda e: e.max(out=m8[:, 0:8], in_=imp[:]), reads=[Bimp], writes=[Bm8])
                    S.dve(lambda e: e.match_replace(out=imp2[:], in_to_replace=m8[:, 0:8], in_values=imp[:], imm_value=-3e9), reads=[Bimp, Bm8], writes=[Bimp2])
                    S.dve(lambda e: e.max(out=m8[:, 8:16], in_=imp2[:]), reads=[Bimp2], writes=[Bm8])
                    S.dve(lambda e: e.tensor_scalar(selb[:], imp[:], m8[:, 15:16], None, ALU.is_lt), reads=[Bimp, Bm8], writes=[Bselb])

                for j in range(ncmp):
                    bias = [(lambda e, bk, st_, j=j: e.matmul(bk[:], ident[:], rep4(cbs[j], 0, 128), start=st_, stop=False), [Bconst, Bcbs[j]])]
                    tasks.append(dict(pre=(pre_cb if (g == 0 and j == 0) else []),
                                      s_fn=mk_s(kcmpT[R0:R0 + 64, j * 128:(j + 1) * 128], Bkcmp, bias, qrhs),
                                      pv_fn=mk_pv(acc, Bacc, vcmpA[:, j, g, :], Bvcmp, j == 0, j == ncmp - 1, impj=j, bimp=bimp, Bbimp=Bbimp, nj=ncmp),
                                      post=(post_cmp if j == ncmp - 1 else None)))

            def selb_transpose(g):
                def f():
                    bk, Bb = bank("mm")
                    S.pe(lambda e: e.matmul(bk[0:64, 0:128], selbs[g][:], ident[:], start=True, stop=True), reads=[Bselbs[g], Bconst], writes=[Bb])
                    S.act(lambda e: e.activation(selbTs[g][0:64, :], bk[0:64, 0:128], AF.Copy, scale=NEG), reads=[Bb], writes=[BselbTs[g]])
                return f
            kts = [kt for kt in range(qi - 4, qi + 1) if kt >= 0]
            for g in range(2):
                R0 = 64 * g
                qrhs = qT[R0:R0 + 64, s, :, :]
                acc, Bacc = bank("acc")
                for i, kt in enumerate(kts):
                    bias = []
                    if kt == qi:
                        bias.append((lambda e, bk, st_: e.matmul(bk[:], ident[:], rep4(tri, 0, 256), start=st_, stop=False), [Bconst]))
                    if kt == qi - 4:
                        bias.append((lambda e, bk, st_: e.matmul(bk[:], ident[:], rep4(tri, 128, 256), start=st_, stop=False), [Bconst]))
                    pre = []
                    if i == 0:
                        pre = [selb_transpose(0)] if g == 1 else []
                    tasks.append(dict(pre=pre, s_fn=mk_s(kwinT[R0:R0 + 64, kt * 128:(kt + 1) * 128], Bkwin, bias, qrhs),
                                      pv_fn=mk_pv(acc, Bacc, vwinA[:, kt, g, :], Bvwin, i == 0, i == len(kts) - 1),
                                      post=((lambda acc=acc, Bacc=Bacc, g=g: combine(acc, Bacc, g, 2, 2 + g, False)) if i == len(kts) - 1 else None)))
            def finalize():
                if debug and seq == 0 and l == 0:
                    out_dmas.append(S.dma(lambda e: e.dma_start(out=dbg_attn[t0:t0 + 128, :], in_=o_t[:]), reads=[Bo]))
                ssa, Bssa, on, Bon = ssa2[ob], Bssa2[ob], on2[ob], Bon2[ob]
                S.dve(lambda e: e.memset(ssa[:], 0.0), writes=[Bssa])
                S.act(lambda e: e.activation(junk[:, 0:512], o_t[:], AF.Square, accum_out=ssa[:, 0:1]), reads=[Bo], writes=[Bjunk, Bssa])
                rstd_from_ss(ssa[:, 0:1], ssa[:, 1:2], 512, Bssa, Bssa)
                S.dve(lambda e: e.scalar_tensor_tensor(on[:], o_t[:], ssa[:, 1:2], naob[:], ALU.mult, ALU.mult), reads=[Bo, Bssa, Bw], writes=[Bon])

                def tr():
                    bk, Bb = bank("mm")
                    for c in range(4):
                        S.pe(lambda e, c=c: e.matmul(bk[:, c * 128:(c + 1) * 128], on[:, c * 128:(c + 1) * 128], ident[:], start=True, stop=True),
                             reads=[Bon, Bconst], writes=[Bb])
                    dst = _ap(mixT, 4 * 512 + s * 128, [[4096, 128], [512, 4], [1, 128]])
                    src = _ap(bk, 0, [[512, 128], [128, 4], [1, 128]])
                    S.act(lambda e: e.activation(dst, src, AF.Copy), reads=[Bb], writes=[BmixT])
                pipe["delayed"].append((3, tr))

            for g in range(2):
                R0 = 64 * g
                qrhs = qT[R0:R0 + 64, s, :, :]
                acc, Bacc = bank("acc")
                for kt in range(qi + 1):
                    bias = [(lambda e, bk, st_, kt=kt, g=g: e.matmul(bk[:], Ec[:, kt * 128:(kt + 1) * 128], rep4(selbTs[g], 0, 128), start=st_, stop=False),
                             [Bconst, BselbTs[g]])]
                    if kt == qi:
                        bias.append((lambda e, bk, st_: e.matmul(bk[:], ident[:], rep4(tri, 0, 256), start=st_, stop=False), [Bconst]))
                    pre = []
                    if kt == 0 and g == 0:
                        pre = [selb_transpose(1)]

                    def post_sel(acc=acc, Bacc=Bacc, g=g):
                        combine(acc, Bacc, g, 1, 4 + g, False)
                        if g == 1:
                            finalize()
                    tasks.append(dict(pre=pre, s_fn=mk_s(kselT[R0:R0 + 64, kt * 128:(kt + 1) * 128], Bksel, bias, qrhs),
                                      pv_fn=mk_pv(acc, Bacc, vselA[:, kt, g, :], Bvsel, kt == 0, kt == qi),
                                      post=(post_sel if kt == qi else None)))
            return tasks

        def phase_F(l, last):
            with contextlib.ExitStack() as es:
                def t(name, shape, dt=F32):
                    return sb("F%d_" % l + name, shape, dt, es)
                wg = t("wg", [128, 8, DFF], BF16); wu = t("wu", [128, 8, DFF], BF16); wd = t("wd", [128, NFC, D], BF16)
                vc_ = t("vecs", [128, 112], F32); Bw = Buf("Fweights")
                nfb = t("nfb", [128, D], F32) if last else None
                xt = t("xt", [128, 4, D], F32); Bxs = [Buf("xs%d" % i) for i in range(4)]
                xn = [t("xn%d" % i, [128, D], BF16) for i in range(2)]; Bxn = [Buf(), Buf()]
                junk = t("junk", [128, D], BF16); Bjunk = Buf()
                ss = t("ss", [128, 8], F32); Bss = Buf(); rs = t("rs", [128, 8], F32); Brs = Buf()
                hT = t("hT", [128, 8, 512], BF16); BhT = Buf()
                aT = t("aT", [128, NFC, 512], BF16); BaT = Buf()
                gb = [t("gb%d" % i, [128, 514], F32) for i in range(2)]; Bgb = [Buf(), Buf()]
                tA = [t("tA%d" % i, [128, 512], F32) for i in range(2)]; BtA = [Buf(), Buf()]
                tB = [t("tB%d" % i, [128, 512], F32) for i in range(2)]; BtB = [Buf(), Buf()]
                gh = t("gh", [128, 2, NFC, 2], F32); Bgh = Buf()

                def wdma(dst_ap, src_ap):
                    S.dma(lambda e: e.dma_start(out=dst_ap, in_=src_ap), writes=[Bw], q="pool")
                gv = w_gate[l].rearrange("(k p) c -> p k c", p=128)
                uv = w_up[l].rearrange("(k p) c -> p k c", p=128)
                dv = w_down[l].rearrange("(c p) d -> p c d", p=128)
                for k in range(8):
                    wdma(wg[:, k, :], gv[:, k, :])
                for k in range(8):
                    wdma(wu[:, k, :], uv[:, k, :])
                for c in range(NFC):
                    wdma(wd[:, c, :], dv[:, c, :])
                S.dma(lambda e: e.dma_start(out=vc_[:], in_=vecs[l]), writes=[Bw])
                if last:
                    S.dma(lambda e: e.dma_start(out=nfb[:], in_=normf.partition_broadcast(128)), writes=[Bw])
                n2col = vc_[:, 16:24]
                cw = vc_[:, 24:90]
                cbias = vc_[:, 90:112]

                for seq in range(NSEQ):
                    S.pool(lambda e: e.memset(gh[:], 0.0), writes=[Bgh])
                    for st in range(NST):
                        T0 = st * 512
                        for s in range(4):
                            S.dma(lambda e, seq=seq, T0=T0, s=s: e.dma_start(out=xt[:, s, :], in_=xbuf[seq, T0 + s * 128:T0 + (s + 1) * 128, :]),
                                  reads=[xd[seq][st]], writes=[Bxs[s]])
                        S.dve(lambda e: e.memset(ss[:], 0.0), writes=[Bss])
                        for s in range(4):
                            S.act(lambda e, s=s: e.activation(junk[:], xt[:, s, :], AF.Square, accum_out=ss[:, s:s + 1]), reads=[Bxs[s]], writes=[Bjunk, Bss])
                        rstd_from_ss(ss[:, 0:4], rs[:, 0:4], D, Bss, Brs)
                        for s in range(4):
                            S.dve(lambda e, s=s: e.tensor_scalar(xn[s % 2][:], xt[:, s, :], rs[:, s:s + 1], None, ALU.mult),
                                  reads=[Bxs[s], Brs], writes=[Bxn[s % 2]])
                            for kh in range(2):
                                bk, Bb = bank("mm")
                                for kk in range(4):
                                    k = kh * 4 + kk
                                    S.pe(lambda e, s=s, k=k, kk=kk, bk=bk: e.matmul(bk[:, kk * 128:(kk + 1) * 128], xn[s % 2][:, k * 128:(k + 1) * 128],
                                                                                   ident[:], start=True, stop=True),
                                         reads=[Bxn[s % 2], Bconst], writes=[Bb])
                                for kk in range(4):
                                    k = kh * 4 + kk
                                    if kk % 2 == 0:
                                        S.act(lambda e, s=s, k=k, kk=kk, bk=bk: e.activation(hT[:, k, s * 128:(s + 1) * 128], bk[:, kk * 128:(kk + 1) * 128],
                                                                                          AF.Copy, scale=n2col[:, k:k + 1]), reads=[Bb, Bw], writes=[BhT])
                                    else:
                                        S.dve(lambda e, s=s, k=k, kk=kk, bk=bk: e.tensor_scalar(hT[:, k, s * 128:(s + 1) * 128], bk[:, kk * 128:(kk + 1) * 128],
                                                                                              n2col[:, k:k + 1], None, ALU.mult), reads=[Bb, Bw], writes=[BhT])
                        hi, ho = st % 2, (st + 1) % 2
                        for c in range(NFC):
                            bg, Bbg = bank("mm")
                            for k in range(8):
                                S.pe(lambda e, c=c, k=k, bg=bg: e.matmul(bg[:], wg[:, k, c * 128:(c + 1) * 128], hT[:, k, :], start=(k == 0), stop=(k == 7)),
                                     reads=[Bw, BhT], writes=[Bbg])
                            bu, Bbu = bank("s")
                            for k in range(8):
                                S.pe(lambda e, c=c, k=k, bu=bu: e.matmul(bu[:], wu[:, k, c * 128:(c + 1) * 128], hT[:, k, :], start=(k == 0), stop=(k == 7)),
                                     reads=[Bw, BhT], writes=[Bbu])
                            i = c % 2
                            S.act(lambda e, i=i, bg=bg: e.activation(gb[i][:, 2:514], bg[:], AF.Copy), reads=[Bbg], writes=[Bgb[i]])
                            S.pool(lambda e, i=i, c=c, hi=hi: e.tensor_copy(gb[i][:, 0:2], gh[:, hi, c, :]), reads=[Bgh], writes=[Bgb[i]])
                            S.pool(lambda e, i=i, c=c, ho=ho: e.tensor_copy(gh[:, ho, c, :], gb[i][:, 512:514]), reads=[Bgb[i]], writes=[Bgh])
                            S.act(lambda e, i=i, c=c: e.activation(tA[i][:], gb[i][:, 2:514], AF.Identity, scale=cw[:, 3 * c + 2:3 * c + 3], bias=cbias[:, c:c + 1]),
                                  reads=[Bgb[i], Bw], writes=[BtA[i]])
                            S.dve(lambda e, i=i, c=c: e.scalar_tensor_tensor(tB[i][:], gb[i][:, 1:513], cw[:, 3 * c + 1:3 * c + 2], tA[i][:], ALU.mult, ALU.add),
                                  reads=[Bgb[i], Bw, BtA[i]], writes=[BtB[i]])
                            S.dve(lambda e, i=i, c=c: e.scalar_tensor_tensor(tA[i][:], gb[i][:, 0:512], cw[:, 3 * c:3 * c + 1], tB[i][:], ALU.mult, ALU.add),
                                   reads=[Bgb[i], Bw, BtB[i]], writes=[BtA[i]])
                            S.act(lambda e, i=i: e.activation(tB[i][:], tA[i][:], AF.Silu), reads=[BtA[i]], writes=[BtB[i]])
                            S.dve(lambda e, i=i, c=c, bu=bu: e.tensor_tensor(aT[:, c, :], tB[i][:], bu[:], ALU.mult), reads=[BtB[i], Bbu], writes=[BaT])
                        for s in range(4):
                            for nh in range(2):
                                bk, Bb = bank("acc")
                                for c in range(NFC):
                                    S.pe(lambda e, s=s, nh=nh, c=c, bk=bk: e.matmul(bk[:], aT[:, c, s * 128:(s + 1) * 128], wd[:, c, nh * 512:(nh + 1) * 512],
                                                                                 start=(c == 0), stop=(c == NFC - 1)),
                                         reads=[BaT, Bw], writes=[Bb])
                                S.dve(lambda e, s=s, nh=nh, bk=bk: e.tensor_tensor(xt[:, s, nh * 512:(nh + 1) * 512], xt[:, s, nh * 512:(nh + 1) * 512], bk[:], ALU.add),
                                      reads=[Bb, Bxs[s]], writes=[Bxs[s]])
                            if not last:
                                S.dma(lambda e, seq=seq, T0=T0, s=s: e.dma_start(out=xbuf[seq, T0 + s * 128:T0 + (s + 1) * 128, :], in_=xt[:, s, :]),
                                      reads=[Bxs[s]], writes=[xd[seq][st]])
                            else:
                                S.act(lambda e, s=s: e.activation(junk[:], xt[:, s, :], AF.Square, accum_out=ss[:, 4 + s:5 + s]), reads=[Bxs[s]], writes=[Bjunk, Bss])
                                rstd_from_ss(ss[:, 4 + s:5 + s], rs[:, 4 + s:5 + s], D, Bss, Brs)
                                S.dve(lambda e, s=s: e.scalar_tensor_tensor(xt[:, s, :], xt[:, s, :], rs[:, 4 + s:5 + s], nfb[:], ALU.mult, ALU.mult),
                                      reads=[Bxs[s], Brs, Bw], writes=[Bxs[s]])
                                out_dmas.append(S.dma(lambda e, seq=seq, T0=T0, s=s: e.dma_start(out=y_out[seq, T0 + s * 128:T0 + (s + 1) * 128, :], in_=xt[:, s, :]),
                                                      reads=[Bxs[s]], writes=[xd[seq][st]]))

        for l in range(L):
            S.barrier()
            phase_M(l)
            if stop_after_M:
                break
            S.barrier()
            phase_F(l, l == L - 1)
        S.emit(final_wait_ops=out_dmas)
    return nc, dbg_out


def _consts():
    p = np.arange(128)[:, None].astype(np.float64)
    q = np.arange(128)[None, :].astype(np.float64)
    c = {}
    c["c_ident"] = np.eye(128, dtype=np.float32)
    tri = np.where(p <= q, 0.0, NEG)
    tri2 = np.where(p > q, 0.0, NEG)
    c["c_tri"] = np.concatenate([tri, tri2], axis=1).astype(np.float32)
    d1 = 30000.0 * (q - 16.0 * p)
    d0 = d1.copy()
    d0[0, :] = -1e9
    c["c_dcmp"] = np.concatenate([d0, d1], axis=1).astype(np.float32)
    ovl = np.zeros((128, 2, 64), np.float32)
    for j in range(2):
        for pp in range(128):
            s = 128 * j + pp
            if s == 0:
                continue
            n = s - 1
            for b in range(64):
                if (16 * n < 64 * b + 64) and (16 * n + 31 >= 64 * b):
                    ovl[pp, j, b] = 1.0
            ovl[pp, j, 0] = 1e6
    c["c_ovl"] = ovl.reshape(128, 128)
    F = np.zeros((128, 128), np.float32)
    for qq in range(128):
        cur = qq // 64
        for j in range(128):
            br = j - 62
            if br == cur:
                F[qq, j] = 3e6
            elif br == cur - 1:
                F[qq, j] = 2e6
            elif br > cur:
                F[qq, j] = -1e9
    c["c_F"] = F
    E = np.zeros((128, 32, 128), np.float32)
    for kt in range(32):
        for pp in range(128):
            E[2 * kt + pp // 64, kt, pp] = 1.0
    c["c_E"] = E.reshape(128, 32 * 128)
    cinv = np.zeros((128, 4, 16), np.float32)
    for gi, w in enumerate(POOL_W):
        for j in range(16):
            cinv[:, gi, j] = 1.0 / min(j + 1, w)
    c["c_cinv"] = cinv.reshape(128, 64)
    return c


def _prep_weights(inp, L):
    perm = list(range(0, 512))
    for r in range(4):
        perm += list(range(512 + r * 64, 512 + r * 64 + 64)) + list(range(512 + (4 + r) * 64, 512 + (4 + r) * 64 + 64))
    perm += list(range(1024, 1152)) + list(range(1152, 1280)) + list(range(1280, 1408)) + list(range(1536, 1664))
    perm += list(range(1408, 1536)) + list(range(1664, 1792)) + list(range(1792, 1816))
    perm = np.asarray(perm)
    out = {}
    out["w_in"] = np.ascontiguousarray(np.asarray(inp["w_in"])[:, :, perm])
    for k in ("w_out", "w_gate", "w_up", "w_down"):
        out[k] = np.ascontiguousarray(np.asarray(inp[k]))
    out["w_pool"] = np.ascontiguousarray(np.asarray(inp["w_pool"]).transpose(0, 2, 1, 3).reshape(L, 128, 512))
    w1 = np.stack([np.asarray(inp["cmp_w1_k"]), np.asarray(inp["cmp_w1_v"])], axis=1)
    out["cmp_w1"] = np.ascontiguousarray(w1.reshape(L, 2, 32, 64, 128).transpose(0, 1, 3, 2, 4).reshape(L, 2, 64, 32 * 128))
    out["cmp_w2"] = np.ascontiguousarray(np.stack([np.asarray(inp["cmp_w2_k"]), np.asarray(inp["cmp_w2_v"])], axis=1))
    pe = np.stack([np.asarray(inp["cmp_pe_k"]), np.asarray(inp["cmp_pe_v"])], axis=1)
    out["cmp_peT"] = np.ascontiguousarray(pe.transpose(0, 1, 3, 2))
    vec = np.zeros((L, 128, 112), np.float32)
    vec[:, :, 0:8] = np.asarray(inp["norm1"]).reshape(L, 8, 128).transpose(0, 2, 1)
    vec[:, :, 8:12] = np.asarray(inp["s_pool"]).reshape(L, 4, 128).transpose(0, 2, 1)
    vec[:, :, 12:16] = np.asarray(inp["norm_pool_out"]).reshape(L, 4, 128).transpose(0, 2, 1)
    vec[:, :, 16:24] = np.asarray(inp["norm2"]).reshape(L, 8, 128).transpose(0, 2, 1)
    cwv = np.asarray(inp["conv_w"]).reshape(L, 3, NFC, 128).transpose(0, 3, 2, 1)
    vec[:, :, 24:90] = cwv.reshape(L, 128, 66)
    vec[:, :, 90:112] = np.asarray(inp["conv_b"]).reshape(L, NFC, 128).transpose(0, 2, 1)
    out["vecs"] = vec
    out["nao"] = np.ascontiguousarray(np.asarray(inp["norm_attn_out"]).reshape(L, 1, 512))
    out["normf"] = np.ascontiguousarray(np.asarray(inp["norm_f"]).reshape(1, D))
    out.update(_consts())
    return {k: np.ascontiguousarray(v, dtype=np.float32) for k, v in out.items()}


_CACHE = {}


def kernel(**inputs):
    x = np.asarray(inputs["x"], dtype=np.float32)
    B, T, _ = x.shape
    L = np.asarray(inputs["w_in"]).shape[0]
    n_cores = 8
    nseq = B // n_cores
    key = (T, nseq, L)
    if key not in _CACHE:
        _CACHE[key] = build(T, nseq, L)[0]
    nc = _CACHE[key]
    shared = _prep_weights(inputs, L)
    in_maps = []
    for c in range(n_cores):
        m = dict(shared)
        m["x"] = np.ascontiguousarray(x[c * nseq:(c + 1) * nseq])
        in_maps.append(m)
    res = run_bass_kernel_spmd(nc, in_maps, core_ids=list(range(n_cores)))
    return np.concatenate([np.asarray(r["y"]) for r in res.results], axis=0).astype(np.float32)
```

```python
import contextlib
import math
import numpy as np
import concourse.bass as bass
import concourse.mybir as mybir
from concourse.bass_utils import run_bass_kernel_spmd

F32 = mybir.dt.float32
BF16 = mybir.dt.bfloat16
AF = mybir.ActivationFunctionType
ALU = mybir.AluOpType

D = 1024
DFF = 2816
NFC = DFF // 128
EPS = 1e-6
NEG = -30000.0
POOL_W = (2, 4, 8, 16)
N_DMA_SEMS = 24


class Buf:
    __slots__ = ("name", "w", "r", "excl")

    def __init__(self, name="", excl=False):
        self.name = name
        self.w = None
        self.r = {}
        self.excl = excl


class Op:
    __slots__ = ("eng", "pos", "fn", "deps", "needed", "sem", "value", "is_dma")

    def __init__(self, eng, pos, fn, is_dma):
        self.eng = eng
        self.pos = pos
        self.fn = fn
        self.deps = []
        self.needed = False
        self.sem = None
        self.value = None
        self.is_dma = is_dma


class Sched:
    ENGS = ("pe", "act", "dve", "pool", "sp")

    def __init__(self, nc):
        self.nc = nc
        self.ops = {e: [] for e in self.ENGS}
        self.waited = {e: {} for e in self.ENGS}
        self.n_dma = 0
        self.dma_ops = []
        self.barrier_deps = {e: [] for e in self.ENGS}

    def barrier(self):
        last = []
        for e in self.ENGS:
            for op in reversed(self.ops[e]):
                if not op.is_dma:
                    last.append(op)
                    break
        last += self.dma_ops[-N_DMA_SEMS:]
        for e in self.ENGS:
            self.barrier_deps[e] = list(last)

    def _add(self, eng, fn, reads, writes, is_dma=False):
        lst = self.ops[eng]
        op = Op(eng, len(lst), fn, is_dma)
        deps = {}

        def need(d, same_ok):
            if d is None:
                return
            if d.eng == eng and (not d.is_dma) and same_ok:
                return
            deps[id(d)] = d

        for b in reads:
            need(b.w, False)
            if b.excl:
                for d in b.r.values():
                    need(d, True)
        for b in writes:
            need(b.w, True)
            for d in b.r.values():
                need(d, True)
        if self.barrier_deps[eng]:
            for d in self.barrier_deps[eng]:
                if not (d.eng == eng and not d.is_dma):
                    deps[id(d)] = d
            self.barrier_deps[eng] = []
        wt = self.waited[eng]
        if is_dma:
            idx = self.n_dma
            self.n_dma += 1
            if idx >= N_DMA_SEMS:
                prev = self.dma_ops[idx - N_DMA_SEMS]
                deps[id(prev)] = prev
        for d in deps.values():
            if d.is_dma:
                key = ("dma", d.pos % N_DMA_SEMS)
            else:
                key = d.eng
            if wt.get(key, -1) >= d.pos:
                continue
            wt[key] = d.pos
            d.needed = True
            op.deps.append(d)
        if is_dma:
            op.pos = idx
            op.needed = True
            self.dma_ops.append(op)
        for b in reads:
            b.r[("dma", op.pos) if is_dma else eng] = op
        for b in writes:
            b.w = op
            b.r = {}
        lst.append(op)
        return op

    def pe(self, fn, reads=(), writes=()):
        return self._add("pe", fn, reads, writes)

    def act(self, fn, reads=(), writes=()):
        return self._add("act", fn, reads, writes)

    def dve(self, fn, reads=(), writes=()):
        return self._add("dve", fn, reads, writes)

    def pool(self, fn, reads=(), writes=()):
        return self._add("pool", fn, reads, writes)

    def dma(self, fn, reads=(), writes=(), q="sp"):
        return self._add(q, fn, reads, writes, is_dma=True)

    def emit(self, final_wait_ops=()):
        nc = self.nc
        with contextlib.ExitStack() as es:
            esem = {e: es.enter_context(nc.semaphore("s_" + e)) for e in self.ENGS}
            dsem = [es.enter_context(nc.semaphore("d%d" % i)) for i in range(N_DMA_SEMS)]
            for e in self.ENGS:
                cnt = 0
                for op in self.ops[e]:
                    if op.is_dma:
                        op.sem = dsem[op.pos % N_DMA_SEMS]
                        op.value = 16 * (op.pos // N_DMA_SEMS + 1)
                    elif op.needed:
                        cnt += 1
                        op.sem = esem[e]
                        op.value = cnt
            block = es.enter_context(nc.Block())
            decos = {"pe": block.tensor, "act": block.scalar, "dve": block.vector,
                     "pool": block.gpsimd, "sp": block.sync}

            def mk(e, extra):
                def body(eng):
                    for op in self.ops[e]:
                        for d in op.deps:
                            eng.wait_ge(d.sem, d.value)
                        ins = op.fn(eng)
                        if op.is_dma:
                            ins.then_inc(op.sem, 16)
                        elif op.needed:
                            ins.then_inc(op.sem, 1)
                    for d in extra:
                        eng.wait_ge(d.sem, d.value)
                return body

            for e in self.ENGS:
                extra = list(final_wait_ops) if e == "sp" else []
                if not self.ops[e] and not extra:
                    continue
                decos[e](mk(e, extra))


def _ap(t, off, dims):
    return bass.AP(t.tensor if hasattr(t, "tensor") else t, off, [list(d) for d in dims])


def build(T, NSEQ, L, debug=False, stop_after_M=False):
    NT = T // 128
    NST = T // 512
    NSLOT = T // 16
    NCT = NSLOT // 128
    nc = bass.Bass("TRN2", target_bir_lowering=False)

    def din(name, shape):
        return nc.dram_tensor(name, list(shape), F32, kind="ExternalInput").ap()

    x_in = din("x", [NSEQ, T, D])
    w_in = din("w_in", [L, D, 1816])
    w_out = din("w_out", [L, D, D])
    w_gate = din("w_gate", [L, D, DFF])
    w_up = din("w_up", [L, D, DFF])
    w_down = din("w_down", [L, DFF, D])
    w_pool = din("w_pool", [L, 128, 4 * 128])
    w1 = din("cmp_w1", [L, 2, 64, 32 * 128])
    w2 = din("cmp_w2", [L, 2, 128, 64])
    peT = din("cmp_peT", [L, 2, 64, 32])
    vecs = din("vecs", [L, 128, 112])
    nao = din("nao", [L, 1, 512])
    normf = din("normf", [1, D])
    c_ident = din("c_ident", [128, 128])
    c_tri = din("c_tri", [128, 256])
    c_dcmp = din("c_dcmp", [128, 256])
    c_ovl = din("c_ovl", [128, 128])
    c_F = din("c_F", [128, 128])
    c_E = din("c_E", [64, 4096])
    c_cinv = din("c_cinv", [128, 64])
    y_out = nc.dram_tensor("y", [NSEQ, T, D], F32, kind="ExternalOutput").ap()
    xbuf = nc.dram_tensor("xbuf", [NSEQ, T, D], F32).ap()
    dbg_out = {}
    if debug:
        dbg_attn = nc.dram_tensor("dbg_attn", [T, 512], F32, kind="ExternalOutput").ap()
        dbg_imp = nc.dram_tensor("dbg_imp", [T, 2, 64], F32, kind="ExternalOutput").ap()
        dbg_selb = nc.dram_tensor("dbg_selb", [T, 2, 64], BF16, kind="ExternalOutput").ap()

    S = Sched(nc)
    out_dmas = []

    with contextlib.ExitStack() as top:
        def sb(name, shape, dt=F32, es=top):
            return es.enter_context(nc.sbuf_tensor(name, list(shape), dt))

        banks = [top.enter_context(nc.psum_tensor("bank%d" % i, [128, 512], F32)) for i in range(8)]
        bbuf = [Buf("bank%d" % i, excl=True) for i in range(8)]
        rr = {"mm": [0, 1], "s": [2, 3], "acc": [4, 5]}
        rr_i = {"mm": 0, "s": 0, "acc": 0}

        def bank(kind):
            i = rr[kind][rr_i[kind] % len(rr[kind])]
            rr_i[kind] += 1
            return banks[i], bbuf[i]

        ident = sb("ident", [128, 128], BF16); Bconst = Buf("const")
        ones = sb("ones", [128, 128], BF16)
        epsc = sb("epsc", [128, 1], F32)
        S.dma(lambda e: e.dma_start(out=ident[:], in_=c_ident), writes=[Bconst], q="pool")
        S.dve(lambda e: e.memset(ones[:], 1.0), writes=[Bconst])
        S.dve(lambda e: e.memset(epsc[:], EPS), writes=[Bconst])

        xd = [[Buf("xd%d_%d" % (s, i)) for i in range(NST)] for s in range(NSEQ)]

        def x_src(l, seq):
            return x_in if l == 0 else xbuf

        def dbg(name, ap_sb, shape, reads):
            if not debug:
                return
            t = nc.dram_tensor("dbg_" + name, list(shape), ap_sb.dtype, kind="ExternalOutput").ap()
            dbg_out[name] = t
            out_dmas.append(S.dma(lambda e: e.dma_start(out=t, in_=ap_sb), reads=reads))

        def rstd_from_ss(ss_ap, out_ap, n, Bss, Bout):
            S.act(lambda e: e.activation(out_ap, ss_ap, AF.Ln, scale=1.0 / n, bias=epsc[0:ss_ap.shape[0], 0:1]),
                  reads=[Bss, Bconst], writes=[Bout])
            S.act(lambda e: e.activation(out_ap, out_ap, AF.Exp, scale=-0.5), reads=[Bout], writes=[Bout])

        def phase_M(l):
            with contextlib.ExitStack() as es:
                def t(name, shape, dt=F32):
                    return sb("M%d_" % l + name, shape, dt, es)
                wfm = t("wfm", [128, 8, 1536], BF16); wtm = t("wtm", [128, 8, 280], BF16)
                wo = t("wo", [128, 8, D], BF16); wpl = t("wpl", [128, 512], BF16)
                w1k = t("w1k", [128, 32 * 128], BF16); w1v = t("w1v", [128, 32 * 128], BF16)
                w2k = t("w2k", [128, 128], BF16); w2v = t("w2v", [128, 64], BF16)
                pet = t("pet", [64, 64], BF16); peb = t("peb", [128, 2], F32)
                vc_ = t("vecs", [128, 112], F32); naob = t("naob", [128, 512], F32)
                Bw = Buf("Mweights")
                tri = t("tri", [128, 256], BF16); dcmp = t("dcmp", [128, 256], F32); ovl = t("ovl", [128, 128], BF16)
                Fc = t("Fc", [128, 128], F32); cinv = t("cinv", [128, 64], F32)
                for dst, src in ((tri, c_tri), (ovl, c_ovl)):
                    S.dma(lambda e, dst=dst, src=src: e.dma_start(out=dst[:], in_=src), writes=[Bconst], q="pool")
                for dst, src in ((dcmp, c_dcmp), (Fc, c_F), (cinv, c_cinv)):
                    S.dma(lambda e, dst=dst, src=src: e.dma_start(out=dst[:], in_=src), writes=[Bconst])
                KA = t("KA", [128, T], BF16); KB = t("KB", [128, T], BF16)
                KWX = [t("KWA", [128, T], BF16), t("KWB", [128, T], BF16)]; BKW = [Buf("KWA"), Buf("KWB")]
                KX = [KA, KB]; BKX = [Buf("KA"), Buf("KB")]
                vselA = t("vselA", [128, NT, 2, 65], BF16); vwinA = t("vwinA", [128, NT, 2, 65], BF16)
                KCX = [t("KCA", [128, NSLOT], BF16), t("KCB", [128, NSLOT], BF16)]; BKC = [Buf("KCA"), Buf("KCB")]; vcmpA = t("vcmpA", [128, NCT, 2, 65], BF16)
                glv = t("glv", [128, 2, NSLOT], BF16)
                Bksel, Bkwin, Bvsel, Bvwin = Buf("ksel"), Buf("kwin"), Buf("vsel"), Buf("vwin")
                Bkcmp, Bvcmp, Bglv = Buf("kcmp"), Buf("vcmp"), Buf("glv")
                xt = t("xt", [128, 4, D], F32); Bxt = Buf("xt")
                xn = [t("xn%d" % i, [128, D], BF16) for i in range(2)]; Bxn = [Buf("xn0"), Buf("xn1")]
                ss = t("ss", [128, 8], F32); Bss = Buf("ss")
                rs = t("rs", [128, 8], F32); Brs = Buf("rs")
                junk = t("junk", [128, D], BF16); Bjunk = Buf("junk")
                hT = t("hT", [128, 8, 512], BF16); BhT = Buf("hT")
                uT = t("uT", [128, 4, 528], F32); BuT = Buf("uT")
                qT = t("qT", [128, 4, 2, 512], BF16); BqT = Buf("qT")
                BqS = [[Buf() for _ in range(2)] for _ in range(4)]
                kcT = t("kcT", [128, 528], BF16); vcT = t("vcT", [128, 528], BF16); BkcT = Buf("kcT"); BvcT = Buf("vcT")
                gat = t("gat", [128, 4, 24], F32); Bgat = Buf("gat")
                ptA = t("ptA", [128, 528], F32); ptB = t("ptB", [128, 528], F32); BptA = Buf("ptA"); BptB = Buf("ptB")
                pooled = [t("pooled%d" % i, [128, 512], BF16) for i in range(2)]; Bpooled = [Buf(), Buf()]
                po = t("po", [128, 4, 512], F32); Bpo = Buf("po")
                sq = [t("sq%d" % i, [128, 512], BF16) for i in range(2)]; Bsq = [Buf(), Buf()]
                rbc = t("rbc", [128, 512], F32); Brbc = Buf("rbc")
                mixT = t("mixT", [128, 8, 512], BF16); BmixT = Buf("mixT")
                PT = [t("PT%d" % i, [128, 512], BF16) for i in range(3)]; BPT = [Buf(), Buf(), Buf()]
                pt_i = [0]
                o2 = [t("o%d" % i, [128, 512], F32) for i in range(2)]; Bo2 = [Buf(), Buf()]
                on2 = [t("on%d" % i, [128, 512], BF16) for i in range(2)]; Bon2 = [Buf(), Buf()]
                otmps = [t("otmp%d" % i, [128, 256], F32) for i in range(2)]; Botmps = [Buf(), Buf()]
                hb = t("hb", [128, 128], F32); Bhb = Buf("hb")
                glt = t("glt", [128, 128], BF16); Bglt = Buf("glt")
                cbs = [t("cb%d" % i, [128, 128], BF16) for i in range(2)]; Bcbs = [Buf(), Buf()]
                sm = t("sm", [128, 64], F32); Bsm = Buf("sm")
                smx = [t("smx%d" % i, [128, 16], F32) for i in range(6)]; Bsmx = [Buf() for i in range(6)]
                imps = [t("imp%d" % i, [128, 64], F32) for i in range(2)]; Bimps = [Buf(), Buf()]
                imp2s = [t("impb%d" % i, [128, 64], F32) for i in range(2)]; Bimp2s = [Buf(), Buf()]
                m8s = [t("m8%d" % i, [128, 16], F32) for i in range(2)]; Bm8s = [Buf(), Buf()]
                selbs = [t("selb%d" % i, [128, 128], BF16) for i in range(2)]; Bselbs = [Buf(), Buf()]
                ssa2 = [t("ssa%d" % i, [128, 2], F32) for i in range(2)]; Bssa2 = [Buf(), Buf()]

                Bwfm, Bwtm, Bwo, Bwc = Buf("wfm"), Buf("wtm"), Buf("wo"), Buf("wc")

                def wdma(dst_ap, src_ap, B_):
                    S.dma(lambda e: e.dma_start(out=dst_ap, in_=src_ap), writes=[B_], q="pool")
                S.dma(lambda e: e.dma_start(out=vc_[:], in_=vecs[l]), writes=[Bw])
                S.dma(lambda e: e.dma_start(out=naob[:], in_=nao[l].partition_broadcast(128)), writes=[Bw])
                wdma(wpl[:], w_pool[l], Bw)
                wv = w_in[l].rearrange("(k p) c -> p k c", p=128)
                for k in range(8):
                    wdma(wfm[:, k, :], wv[:, k, 0:1536], Bwfm)
                wdma(wtm[:, :, :], wv[:, :, 1536:1816], Bwtm)
                for h in range(2):
                    wdma(w1k[64 * h:64 * h + 64, :], w1[l, 0], Bwc)
                    wdma(w1v[64 * h:64 * h + 64, :], w1[l, 1], Bwc)
                    wdma(w2k[:, 64 * h:64 * h + 64], w2[l, 0], Bwc)
                wdma(w2v[:], w2[l, 1], Bwc)
                wdma(pet[:, 0:32], peT[l, 0], Bwc); wdma(pet[:, 32:64], peT[l, 1], Bwc)
                wov = w_out[l].rearrange("(k p) c -> p k c", p=128)
                for k in range(8):
                    wdma(wo[:, k, :], wov[:, k, :], Bwo)
                g1col = vc_[:, 0:8]; spool = vc_[:, 8:12]; npo = vc_[:, 12:16]

                S.pool(lambda e: e.memset(vselA[:], 1.0), writes=[Bvsel])
                S.pool(lambda e: e.memset(vwinA[:], 1.0), writes=[Bvwin])
                S.pool(lambda e: e.memset(vcmpA[:], 1.0), writes=[Bvcmp])
                for g_ in range(2):
                    S.pool(lambda e, g_=g_: e.memset(KCX[g_][:], 0.0), writes=[BKC[g_]])
                    S.pool(lambda e, g_=g_: e.memset(KWX[g_][:], 0.0), writes=[BKW[g_]])
                S.pool(lambda e: e.memset(glv[:], 0.0), writes=[Bglv])
                S.dma(lambda e: e.dma_start(out=KA[64:128, :], in_=c_E[:, 0:T]), writes=[BKX[0]], q="pool")
                S.dma(lambda e: e.dma_start(out=KB[0:64, :], in_=c_E[:, 0:T]), writes=[BKX[1]], q="pool")
                S.pool(lambda e: e.memset(KA[0:64, :], 0.0), writes=[BKX[0]])
                S.pool(lambda e: e.memset(KB[64:128, :], 0.0), writes=[BKX[1]])
                S.pool(lambda e: e.memset(qT[:], 0.0), writes=[BqT])
                for g_ in range(2):
                    S.pool(lambda e, g_=g_: e.memset(selbs[g_][:], 0.0), writes=[Bselbs[g_]])
                bk, Bb = banks[7], bbuf[7]
                for kv, wt_ in ((0, w1k), (1, w1v)):
                    for li in range(32):
                        S.pe(lambda e, kv=kv, wt_=wt_, li=li: e.matmul(bk[:, kv:kv + 1], wt_[0:64, li * 128:(li + 1) * 128],
                                                                      pet[:, kv * 32 + li:kv * 32 + li + 1], start=(li == 0), stop=(li == 31)),
                             reads=[Bwc], writes=[Bb])
                S.dve(lambda e: e.tensor_copy(peb[:], bk[:, 0:2]), reads=[Bb], writes=[Bwc])

                for seq in range(NSEQ):
                    xs = x_src(l, seq)
                    S.pool(lambda e: e.memset(uT[:, :, 0:16], 0.0), writes=[BuT])
                    S.pool(lambda e: e.memset(kcT[:, 0:16], 0.0), writes=[BkcT])
                    S.pool(lambda e: e.memset(vcT[:, 0:16], 0.0), writes=[BvcT])
                    for st in range(NST):
                        T0 = st * 512
                        S.dma(lambda e, xs=xs, seq=seq, T0=T0: e.dma_start(
                            out=xt[:], in_=xs[seq, T0:T0 + 512, :].rearrange("(s p) d -> p s d", p=128)),
                            reads=[xd[seq][st]], writes=[Bxt])
                        if st > 0:
                            S.pool(lambda e: e.tensor_copy(uT[:, :, 0:16], uT[:, :, 512:528]), reads=[BuT], writes=[BuT])
                            S.pool(lambda e: e.tensor_copy(kcT[:, 0:16], kcT[:, 512:528]), reads=[BkcT], writes=[BkcT])
                            S.pool(lambda e: e.tensor_copy(vcT[:, 0:16], vcT[:, 512:528]), reads=[BvcT], writes=[BvcT])
                        S.dve(lambda e: e.memset(ss[:], 0.0), writes=[Bss])
                        for s in range(4):
                            S.act(lambda e, s=s: e.activation(junk[:], xt[:, s, :], AF.Square, accum_out=ss[:, s:s + 1]),
                                  reads=[Bxt], writes=[Bjunk, Bss])
                        for s in range(4):
                            rstd_from_ss(ss[:, s:s + 1], rs[:, s:s + 1], D, Bss, Brs)
                            S.dve(lambda e, s=s: e.tensor_scalar(xn[s % 2][:], xt[:, s, :], rs[:, s:s + 1], None, ALU.mult),
                                  reads=[Bxt, Brs], writes=[Bxn[s % 2]])
                            for kh in range(2):
                                bk, Bb = bank("mm")
                                for kk in range(4):
                                    k = kh * 4 + kk
                                    S.pe(lambda e, s=s, k=k, kk=kk, bk=bk: e.matmul(bk[:, kk * 128:(kk + 1) * 128], xn[s % 2][:, k * 128:(k + 1) * 128],
                                                                                   ident[:], start=True, stop=True),
                                         reads=[Bxn[s % 2], Bconst], writes=[Bb])
                                for kk in range(4):
                                    k = kh * 4 + kk
                                    eng = S.act if kk % 2 == 0 else S.dve
                                    if kk % 2 == 0:
                                        S.act(lambda e, s=s, k=k, kk=kk, bk=bk: e.activation(hT[:, k, s * 128:(s + 1) * 128], bk[:, kk * 128:(kk + 1) * 128],
                                                                                          AF.Copy, scale=g1col[:, k:k + 1]),
                                              reads=[Bb, Bw], writes=[BhT])
                                    else:
                                        S.dve(lambda e, s=s, k=k, kk=kk, bk=bk: e.tensor_scalar(hT[:, k, s * 128:(s + 1) * 128], bk[:, kk * 128:(kk + 1) * 128],
                                                                                              g1col[:, k:k + 1], None, ALU.mult),
                                              reads=[Bb, Bw], writes=[BhT])
                        for c in range(12):
                            bk, Bb = bank("mm")
                            for k in range(8):
                                S.pe(lambda e, c=c, k=k, bk=bk: e.matmul(bk[:], wfm[:, k, c * 128:(c + 1) * 128], hT[:, k, :], start=(k == 0), stop=(k == 7)),
                                     reads=[Bwfm, BhT], writes=[Bb])
                            if c < 4:
                                S.act(lambda e, c=c, bk=bk: e.activation(uT[:, c, 16:528], bk[:], AF.Copy), reads=[Bb], writes=[BuT])
                            elif c < 8:
                                r = c - 4
                                for g_ in range(2):
                                    dst = _ap(qT, g_ * 64 * 4096 + g_ * 512 + r * 128, [[4096, 64], [1024, 4], [1, 128]])
                                    src = _ap(bk, g_ * 64 * 512, [[512, 64], [128, 4], [1, 128]])
                                    if g_ == 0:
                                        S.dve(lambda e, dst=dst, src=src: e.tensor_copy(dst, src), reads=[Bb], writes=[BqT])
                                    else:
                                        S.act(lambda e, dst=dst, src=src: e.activation(dst, src, AF.Copy), reads=[Bb], writes=[BqT])
                            elif c == 8:
                                S.act(lambda e, bk=bk: e.activation(kcT[:, 16:528], bk[:], AF.Copy), reads=[Bb], writes=[BkcT])
                            elif c == 9:
                                S.dve(lambda e, bk=bk: e.tensor_copy(vcT[:, 16:528], bk[:]), reads=[Bb], writes=[BvcT])
                            elif c == 10:
                                S.act(lambda e, bk=bk, T0=T0: e.activation(KA[0:64, T0:T0 + 512], bk[0:64, :], AF.Copy), reads=[Bb], writes=[BKX[0]])
                                S.dve(lambda e, bk=bk, T0=T0: e.tensor_copy(KB[64:128, T0:T0 + 512], bk[64:128, :]), reads=[Bb], writes=[BKX[1]])
                            else:
                                S.dve(lambda e, bk=bk, T0=T0: e.tensor_copy(KWX[0][0:64, T0:T0 + 512], bk[0:64, :]), reads=[Bb], writes=[BKW[0]])
                                S.act(lambda e, bk=bk, T0=T0: e.activation(KWX[1][64:128, T0:T0 + 512], bk[64:128, :], AF.Copy), reads=[Bb], writes=[BKW[1]])
                        for s in range(4):
                            ti = st * 4 + s
                            bk, Bb = bank("mm")
                            for k in range(8):
                                S.pe(lambda e, s=s, k=k, bk=bk: e.matmul(bk[:, 0:280], hT[:, k, s * 128:(s + 1) * 128], wtm[:, k, :], start=(k == 0), stop=(k == 7)),
                                     reads=[Bwtm, BhT], writes=[Bb])
                            src = _ap(bk, 0, [[512, 128], [64, 2], [1, 64]])
                            S.act(lambda e, ti=ti, src=src: e.activation(vselA[:, ti, :, 0:64], src, AF.Copy), reads=[Bb], writes=[Bvsel])
                            src2 = _ap(bk, 128, [[512, 128], [64, 2], [1, 64]])
                            S.dve(lambda e, ti=ti, src2=src2: e.tensor_copy(vwinA[:, ti, :, 0:64], src2), reads=[Bb], writes=[Bvwin])
                            S.act(lambda e, s=s, bk=bk: e.activation(gat[:, s, :], bk[:, 256:280], AF.Exp, scale=-1.0), reads=[Bb], writes=[Bgat])
                            S.dve(lambda e, s=s: e.tensor_scalar(gat[:, s, :], gat[:, s, :], 1.0, None, ALU.add), reads=[Bgat], writes=[Bgat])
                            S.dve(lambda e, s=s: e.reciprocal(gat[:, s, :], gat[:, s, :]), reads=[Bgat], writes=[Bgat])
                        s0 = 32 * st
                        for g in range(2):
                            bk, Bb = banks[6 + g], bbuf[6 + g]
                            R0 = 64 * g
                            for kv, (src_t, Bsrc, wt_) in enumerate(((kcT, BkcT, w1k), (vcT, BvcT, w1v))):
                                for li in range(32):
                                    S.pe(lambda e, kv=kv, src_t=src_t, wt_=wt_, li=li, R0=R0, bk=bk: e.matmul(
                                        bk[:, kv * 32:(kv + 1) * 32], wt_[R0:R0 + 64, li * 128:(li + 1) * 128],
                                        src_t[R0:R0 + 64, li:li + 16 * 31 + 1:16], start=(li == 0), stop=(li == 31)),
                                        reads=[Bwc, Bsrc], writes=[Bb])
                            for kv in range(2):
                                S.act(lambda e, kv=kv, g=g, bk=bk: e.activation(glt[:, g * 64 + kv * 32:g * 64 + kv * 32 + 32], bk[:, kv * 32:(kv + 1) * 32],
                                                                            AF.Gelu_apprx_tanh, bias=peb[:, kv:kv + 1]),
                                      reads=[Bb, Bwc], writes=[Bglt])
                            S.dve(lambda e, g=g, s0=s0: e.tensor_copy(glv[:, g, s0:s0 + 32], glt[:, g * 64 + 32:g * 64 + 64]), reads=[Bglt], writes=[Bglv])
                        for g in range(2):
                            bk, Bb = bank("mm")
                            S.pe(lambda e, g=g, bk=bk: e.matmul(bk[:, 0:32], w2k[:], glt[:, g * 64:g * 64 + 32], start=True, stop=True),
                                 reads=[Bwc, Bglt], writes=[Bb])
                            S.act(lambda e, g=g, bk=bk, s0=s0: e.activation(KCX[g][64 * g:64 * g + 64, s0:s0 + 32], bk[64 * g:64 * g + 64, 0:32], AF.Copy),
                                  reads=[Bb], writes=[BKC[g]])
                            jt = st // 4
                            S.pe(lambda e, g=g, bk=bk, jt=jt: e.matmul(bk[:, 64:128], glv[:, g, jt * 128:(jt + 1) * 128], w2v[:], start=True, stop=True),
                                 reads=[Bwc, Bglv], writes=[Bb])
                            S.dve(lambda e, g=g, bk=bk, jt=jt: e.tensor_copy(vcmpA[:, jt, g, 0:64], bk[:, 64:128]), reads=[Bb], writes=[Bvcmp])
                        pipe = {"pending": None, "delayed": []}

                        def step_delayed():
                            nd = []
                            for cnt, fn in pipe["delayed"]:
                                if cnt <= 0:
                                    fn()
                                else:
                                    nd.append((cnt - 1, fn))
                            pipe["delayed"] = nd

                        def finish_prev():
                            prev = pipe["pending"]
                            if prev is not None:
                                prev["pv_fn"](prev["P"])
                                if prev["post"] is not None:
                                    prev["post"]()
                            pipe["pending"] = None

                        def run_task(task):
                            for f in task["pre"]:
                                f()
                            P = task["s_fn"]()
                            finish_prev()
                            task["P"] = P
                            pipe["pending"] = task
                            step_delayed()

                        for s in range(4):
                            for task in attn_tasks(l, seq, st, s, pipe, locals()):
                                run_task(task)
                        finish_prev()
                        while pipe["delayed"]:
                            step_delayed()
                        bst, Bbst = banks[6], bbuf[6]
                        for p in range(4):
                            w = POOL_W[p]
                            cur, Bcur = uT[:, p, :], BuT
                            tmp = [(ptA, BptA), (ptB, BptB)]
                            step = 1
                            i = 0
                            while step < w:
                                dstt, Bd = tmp[i % 2]
                                lo = 2 * step - 1
                                S.pool(lambda e, dstt=dstt, cur=cur, lo=lo, step=step: e.tensor_tensor(dstt[:, lo:528], cur[:, lo:528], cur[:, lo - step:528 - step], ALU.add),
                                       reads=[Bcur], writes=[Bd])
                                cur, Bcur = dstt[:, :], Bd
                                step *= 2
                                i += 1
                            pl, Bpl = pooled[p % 2], Bpooled[p % 2]
                            S.dve(lambda e, pl=pl, cur=cur, p=p, w=w: e.scalar_tensor_tensor(pl[:], cur[:, 16:528], 1.0 / w, uT[:, p, 16:528], ALU.mult, ALU.subtract),
                                   reads=[Bcur, BuT], writes=[Bpl])
                            if st == 0:
                                S.pool(lambda e, cur=cur, p=p: e.tensor_tensor(sm[:, 0:16], cur[:, 16:32], cinv[:, p * 16:(p + 1) * 16], ALU.mult),
                                       reads=[Bcur, Bconst], writes=[Bsm])
                                S.pool(lambda e, pl=pl, p=p: e.tensor_tensor(pl[:, 0:16], sm[:, 0:16], uT[:, p, 16:32], ALU.subtract),
                                       reads=[Bsm, BuT], writes=[Bpl])
                            bk, Bb = bank("mm")
                            S.pe(lambda e, p=p, pl=pl, bk=bk: e.matmul(bk[:], wpl[:, p * 128:(p + 1) * 128], pl[:], start=True, stop=True),
                                 reads=[Bw, Bpl], writes=[Bb])
                            S.act(lambda e, p=p, bk=bk: e.activation(po[:, p, :], bk[:], AF.Copy, scale=spool[:, p:p + 1]), reads=[Bb, Bw], writes=[Bpo])
                            S.dve(lambda e, p=p: e.tensor_tensor(sq[p % 2][:], po[:, p, :], po[:, p, :], ALU.mult), reads=[Bpo], writes=[Bsq[p % 2]])
                            S.pe(lambda e, p=p: e.matmul(bst[:], ones[:], sq[p % 2][:], start=(p == 0), stop=(p == 3)),
                                 reads=[Bconst, Bsq[p % 2]], writes=[Bbst])
                        rstd_from_ss(bst[:], rbc[:], 512, Bbst, Brbc)
                        for p in range(4):
                            S.dve(lambda e, p=p: e.scalar_tensor_tensor(mixT[:, p, :], po[:, p, :], npo[:, p:p + 1], rbc[:], ALU.mult, ALU.mult),
                                  reads=[Bpo, Bw, Brbc], writes=[BmixT])
                        for s in range(4):
                            for nh in range(2):
                                bk, Bb = bank("mm")
                                for k in range(8):
                                    S.pe(lambda e, s=s, nh=nh, k=k, bk=bk: e.matmul(bk[:], mixT[:, k, s * 128:(s + 1) * 128], wo[:, k, nh * 512:(nh + 1) * 512],
                                                                                 start=(k == 0), stop=(k == 7)),
                                         reads=[BmixT, Bwo], writes=[Bb])
                                S.dve(lambda e, s=s, nh=nh, bk=bk: e.tensor_tensor(xt[:, s, nh * 512:(nh + 1) * 512], xt[:, s, nh * 512:(nh + 1) * 512], bk[:], ALU.add),
                                      reads=[Bb, Bxt], writes=[Bxt])
                        xdst = y_out if stop_after_M else xbuf
                        od = S.dma(lambda e, seq=seq, T0=T0, xdst=xdst: e.dma_start(out=xdst[seq, T0:T0 + 512, :].rearrange("(s p) d -> p s d", p=128), in_=xt[:]),
                                   reads=[Bxt], writes=[xd[seq][st]])
                        if stop_after_M:
                            out_dmas.append(od)

        def attn_tasks(l, seq, st, s, pipe, L_):
            qT = L_["qT"]; BqT = L_["BqT"]; PT = L_["PT"]; BPT = L_["BPT"]; pt_i = L_["pt_i"]
            KCX, BKC, vcmpA, Bvcmp = L_["KCX"], L_["BKC"], L_["vcmpA"], L_["Bvcmp"]
            KX, BKX, BqS, vselA, Bvsel = L_["KX"], L_["BKX"], L_["BqS"], L_["vselA"], L_["Bvsel"]
            KWX, BKW, vwinA, Bvwin = L_["KWX"], L_["BKW"], L_["vwinA"], L_["Bvwin"]
            cbs, Bcbs, smx, Bsmx = L_["cbs"], L_["Bcbs"], L_["smx"], L_["Bsmx"]
            imps, Bimps, imp2s, Bimp2s, m8s, Bm8s = L_["imps"], L_["Bimps"], L_["imp2s"], L_["Bimp2s"], L_["m8s"], L_["Bm8s"]
            selbs, Bselbs = L_["selbs"], L_["Bselbs"]
            gat, Bgat, o2, Bo2, otmps, Botmps = L_["gat"], L_["Bgat"], L_["o2"], L_["Bo2"], L_["otmps"], L_["Botmps"]
            tri, dcmp, ovl, Fc = L_["tri"], L_["dcmp"], L_["ovl"], L_["Fc"]
            ssa2, Bssa2, on2, Bon2, junk, Bjunk = L_["ssa2"], L_["Bssa2"], L_["on2"], L_["Bon2"], L_["junk"], L_["Bjunk"]
            naob, Bw, mixT, BmixT = L_["naob"], L_["Bw"], L_["mixT"], L_["BmixT"]
            qi = st * 4 + s
            t0 = qi * 128
            ncmp = 1 if (NCT == 1 or qi < 16) else 2
            ob = s % 2
            o_t, Bo = o2[ob], Bo2[ob]
            tasks = []

            def mk_s(kT_ap, Bk, bias_fns, qrhs, extra_reads=()):
                def s_fn():
                    bk, Bb = bank("s")
                    n = len(bias_fns)
                    for i, (fn, rd) in enumerate(bias_fns):
                        S.pe(lambda e, fn=fn, i=i, bk=bk: fn(e, bk, i == 0), reads=rd, writes=[Bb])
                    S.pe(lambda e, bk=bk, n=n: e.matmul(bk[:], kT_ap, qrhs, start=(n == 0), stop=True), reads=[Bk, BqT] + list(extra_reads), writes=[Bb])
                    i = pt_i[0] % 3
                    pt_i[0] += 1
                    S.act(lambda e, bk=bk, i=i: e.activation(PT[i][:], bk[:], AF.Exp, scale=0.125), reads=[Bb], writes=[BPT[i]])
                    return (PT[i], BPT[i])
                return s_fn

            def mk_pv(acc, Bacc, v_ap, Bv, first, last, impj=None, bimp=None, Bbimp=None, nj=1):
                def pv_fn(PP):
                    P, BP = PP
                    for r in range(4):
                        S.pe(lambda e, r=r: e.matmul(acc[:, r * 65:(r + 1) * 65], P[:, r * 128:(r + 1) * 128], v_ap, start=(first and r == 0), stop=last),
                             reads=[BP, Bv], writes=[Bacc])
                    if impj is not None:
                        for r in range(4):
                            S.pe(lambda e, r=r: e.matmul(bimp[:, r * 64:(r + 1) * 64], P[:, r * 128:(r + 1) * 128], ovl[:, impj * 64:(impj + 1) * 64],
                                                        start=(impj == 0 and r == 0), stop=(impj == nj - 1)),
                                 reads=[BP, Bconst], writes=[Bbimp])
                return pv_fn

            def combine(acc, Bacc, g, b, bi, first):
                sm, Bsm = smx[bi], Bsmx[bi]
                sums = _ap(acc, 64, [[512, 128], [65, 4]])
                S.dve(lambda e: e.tensor_scalar(sm[:, 0:4], sums, 1e-30, None, ALU.max), reads=[Bacc], writes=[Bsm])
                S.dve(lambda e: e.reciprocal(sm[:, 4:8], sm[:, 0:4]), reads=[Bsm], writes=[Bsm])
                gsl = _ap(gat, s * 24 + g * 12 + b, [[96, 128], [3, 4]])
                S.dve(lambda e: e.tensor_tensor(sm[:, 8:12], sm[:, 4:8], gsl, ALU.mult), reads=[Bsm, Bgat], writes=[Bsm])
                in0 = _ap(acc, 0, [[512, 128], [65, 4], [1, 64]])
                in1 = _ap(sm, 8, [[16, 128], [1, 4], [0, 64]])
                if first:
                    S.dve(lambda e: e.tensor_tensor(_ap(o_t, g * 256, [[512, 128], [64, 4], [1, 64]]), in0, in1, ALU.mult), reads=[Bacc, Bsm], writes=[Bo])
                else:
                    ot_, Bot_ = otmps[g], Botmps[g]
                    S.dve(lambda e: e.tensor_tensor(_ap(ot_, 0, [[256, 128], [64, 4], [1, 64]]), in0, in1, ALU.mult), reads=[Bacc, Bsm], writes=[Bot_])
                    S.pool(lambda e: e.tensor_tensor(o_t[:, g * 256:(g + 1) * 256], o_t[:, g * 256:(g + 1) * 256], ot_[:], ALU.add),
                           reads=[Bot_, Bo], writes=[Bo])

            def rep4(t_, off, pstride):
                return _ap(t_, off, [[pstride, 128], [0, 4], [1, 128]])

            pre_cb = []
            for j in range(ncmp):
                cval = 30000.0 * (t0 - 2048 * j - 15)
                pre_cb.append(lambda j=j, cval=cval: S.dve(lambda e: e.tensor_scalar(cbs[j][:], dcmp[:, j * 128:(j + 1) * 128], cval, 0.0, ALU.add, ALU.min),
                                                           reads=[Bconst], writes=[Bcbs[j]]))
            for g in range(2):
                R0 = 64 * g
                qrhs = qT[R0:R0 + 64, s, g, :]
                acc, Bacc = bank("acc")
                bimp, Bbimp = banks[6 + g], bbuf[6 + g]

                def post_cmp(acc=acc, Bacc=Bacc, g=g, bimp=bimp, Bbimp=Bbimp):
                    combine(acc, Bacc, g, 0, g, True)
                    sm, Bsm = smx[g], Bsmx[g]
                    imp, Bimp, imp2, Bimp2, m8, Bm8, selb, Bselb = imps[g], Bimps[g], imp2s[g], Bimp2s[g], m8s[g], Bm8s[g], selbs[g], Bselbs[g]
                    for r in range(4):
                        if r == 0:
                            S.dve(lambda e: e.tensor_scalar(imp[:], bimp[:, 0:64], sm[:, 4:5], None, ALU.mult), reads=[Bbimp, Bsm], writes=[Bimp])
                        else:
                            S.dve(lambda e, r=r: e.scalar_tensor_tensor(imp[:], bimp[:, r * 64:(r + 1) * 64], sm[:, 4 + r:5 + r], imp[:], ALU.mult, ALU.add),
                                  reads=[Bbimp, Bsm, Bimp], writes=[Bimp])
                    S.dve(lambda e: e.tensor_tensor(imp[:], imp[:], Fc[:, 62 - 2 * qi:126 - 2 * qi], ALU.add), reads=[Bimp, Bconst], writes=[Bimp])
                    S.dve(lambda e: e.max(out=m8[:, 0:8], in_=imp[:]), reads=[Bimp], writes=[Bm8])
                    S.dve(lambda e: e.match_replace(out=imp2[:], in_to_replace=m8[:, 0:8], in_values=imp[:], imm_value=-3e9), reads=[Bimp, Bm8], writes=[Bimp2])
                    S.dve(lambda e: e.max(out=m8[:, 8:16], in_=imp2[:]), reads=[Bimp2], writes=[Bm8])
                    S.dve(lambda e: e.tensor_scalar(selb[:, 64 * (1 - g):64 * (1 - g) + 64], imp[:], m8[:, 15:16], None, ALU.is_lt), reads=[Bimp, Bm8], writes=[Bselb])

                for j in range(ncmp):
                    bias = [(lambda e, bk, st_, j=j: e.matmul(bk[:], ident[:], rep4(cbs[j], 0, 128), start=st_, stop=False), [Bconst, Bcbs[j]])]
                    tasks.append(dict(pre=(pre_cb if (g == 0 and j == 0) else []),
                                      s_fn=mk_s(KCX[g][:, j * 128:(j + 1) * 128], BKC[g], bias, qT[:, s, g, :]),
                                      pv_fn=mk_pv(acc, Bacc, vcmpA[:, j, g, :], Bvcmp, j == 0, j == ncmp - 1, impj=j, bimp=bimp, Bbimp=Bbimp, nj=ncmp),
                                      post=(post_cmp if j == ncmp - 1 else None)))

            def selb_transpose(g):
                def f():
                    bk, Bb = bank("mm")
                    S.pe(lambda e: e.matmul(bk[:, 0:128], selbs[g][:], ident[:], start=True, stop=True), reads=[Bselbs[g], Bconst], writes=[Bb])
                    h0 = 64 * (1 - g)
                    dst = _ap(qT, h0 * 4096 + s * 1024 + g * 512, [[4096, 64], [128, 4], [1, 128]])
                    src = _ap(bk, h0 * 512, [[512, 64], [0, 4], [1, 128]])
                    S.act(lambda e: e.activation(dst, src, AF.Copy, scale=NEG), reads=[Bb], writes=[BqS[s][g]])
                return f
            kts = [kt for kt in range(qi - 4, qi + 1) if kt >= 0]
            for g in range(2):
                R0 = 64 * g
                qrhs = qT[R0:R0 + 64, s, g, :]
                acc, Bacc = bank("acc")
                for i, kt in enumerate(kts):
                    bias = []
                    if kt == qi:
                        bias.append((lambda e, bk, st_: e.matmul(bk[:], ident[:], rep4(tri, 0, 256), start=st_, stop=False), [Bconst]))
                    if kt == qi - 4:
                        bias.append((lambda e, bk, st_: e.matmul(bk[:], ident[:], rep4(tri, 128, 256), start=st_, stop=False), [Bconst]))
                    pre = []
                    if i == 0:
                        pre = [selb_transpose(0)] if g == 1 else []
                    tasks.append(dict(pre=pre, s_fn=mk_s(KWX[g][:, kt * 128:(kt + 1) * 128], BKW[g], bias, qT[:, s, g, :]),
                                      pv_fn=mk_pv(acc, Bacc, vwinA[:, kt, g, :], Bvwin, i == 0, i == len(kts) - 1),
                                      post=((lambda acc=acc, Bacc=Bacc, g=g: combine(acc, Bacc, g, 2, 2 + g, False)) if i == len(kts) - 1 else None)))
            def finalize():
                if debug and seq == 0 and l == 0:
                    out_dmas.append(S.dma(lambda e: e.dma_start(out=dbg_attn[t0:t0 + 128, :], in_=o_t[:]), reads=[Bo]))
                ssa, Bssa, on, Bon = ssa2[ob], Bssa2[ob], on2[ob], Bon2[ob]
                S.dve(lambda e: e.memset(ssa[:], 0.0), writes=[Bssa])
                S.act(lambda e: e.activation(junk[:, 0:512], o_t[:], AF.Square, accum_out=ssa[:, 0:1]), reads=[Bo], writes=[Bjunk, Bssa])
                rstd_from_ss(ssa[:, 0:1], ssa[:, 1:2], 512, Bssa, Bssa)
                S.dve(lambda e: e.scalar_tensor_tensor(on[:], o_t[:], ssa[:, 1:2], naob[:], ALU.mult, ALU.mult), reads=[Bo, Bssa, Bw], writes=[Bon])

                def tr():
                    bk, Bb = bank("mm")
                    for c in range(4):
                        S.pe(lambda e, c=c: e.matmul(bk[:, c * 128:(c + 1) * 128], on[:, c * 128:(c + 1) * 128], ident[:], start=True, stop=True),
                             reads=[Bon, Bconst], writes=[Bb])
                    dst = _ap(mixT, 4 * 512 + s * 128, [[4096, 128], [512, 4], [1, 128]])
                    src = _ap(bk, 0, [[512, 128], [128, 4], [1, 128]])
                    S.act(lambda e: e.activation(dst, src, AF.Copy), reads=[Bb], writes=[BmixT])
                pipe["delayed"].append((3, tr))

            for g in range(2):
                R0 = 64 * g
                qrhs = qT[R0:R0 + 64, s, g, :]
                acc, Bacc = bank("acc")
                for kt in range(qi + 1):
                    bias = []
                    if kt == qi:
                        bias.append((lambda e, bk, st_: e.matmul(bk[:], ident[:], rep4(tri, 0, 256), start=st_, stop=False), [Bconst]))
                    pre = []
                    if kt == 0 and g == 0:
                        pre = [selb_transpose(1)]

                    def post_sel(acc=acc, Bacc=Bacc, g=g):
                        combine(acc, Bacc, g, 1, 4 + g, False)
                        if g == 1:
                            finalize()
                    tasks.append(dict(pre=pre, s_fn=mk_s(KX[g][:, kt * 128:(kt + 1) * 128], BKX[g], bias, qT[:, s, g, :], extra_reads=[BqS[s][g]]),
                                      pv_fn=mk_pv(acc, Bacc, vselA[:, kt, g, :], Bvsel, kt == 0, kt == qi),
                                      post=(post_sel if kt == qi else None)))
            return tasks

        def phase_F(l, last):
            with contextlib.ExitStack() as es:
                def t(name, shape, dt=F32):
                    return sb("F%d_" % l + name, shape, dt, es)
                wg = t("wg", [128, 8, DFF], BF16); wu = t("wu", [128, 8, DFF], BF16); wd = t("wd", [128, NFC, D], BF16)
                vc_ = t("vecs", [128, 112], F32); Bw = Buf("Fweights")
                nfb = t("nfb", [128, D], F32) if last else None
                xt = t("xt", [128, 4, D], F32); Bxs = [Buf("xs%d" % i) for i in range(4)]
                xn = [t("xn%d" % i, [128, D], BF16) for i in range(2)]; Bxn = [Buf(), Buf()]
                junk = t("junk", [128, D], BF16); Bjunk = Buf()
                ss = t("ss", [128, 8], F32); Bss = Buf(); rs = t("rs", [128, 8], F32); Brs = Buf()
                hT = t("hT", [128, 8, 512], BF16); BhT = Buf()
                aT = t("aT", [128, NFC, 512], BF16); BaT = Buf()
                gb = [t("gb%d" % i, [128, 514], F32) for i in range(2)]; Bgb = [Buf(), Buf()]
                tA = [t("tA%d" % i, [128, 512], F32) for i in range(2)]; BtA = [Buf(), Buf()]
                tB = [t("tB%d" % i, [128, 512], F32) for i in range(2)]; BtB = [Buf(), Buf()]
                gh = t("gh", [128, 2, NFC, 2], F32); Bgh = Buf()

                Bwg, Bwu, Bwd = Buf("wg"), Buf("wu"), Buf("wd")

                def wdma(dst_ap, src_ap, B_):
                    S.dma(lambda e: e.dma_start(out=dst_ap, in_=src_ap), writes=[B_], q="pool")
                S.dma(lambda e: e.dma_start(out=vc_[:], in_=vecs[l]), writes=[Bw])
                if last:
                    S.dma(lambda e: e.dma_start(out=nfb[:], in_=normf.partition_broadcast(128)), writes=[Bw])
                gv = w_gate[l].rearrange("(k p) c -> p k c", p=128)
                uv = w_up[l].rearrange("(k p) c -> p k c", p=128)
                dv = w_down[l].rearrange("(c p) d -> p c d", p=128)
                for k in range(8):
                    wdma(wg[:, k, :], gv[:, k, :], Bwg)
                for k in range(8):
                    wdma(wu[:, k, :], uv[:, k, :], Bwu)
                for c in range(NFC):
                    wdma(wd[:, c, :], dv[:, c, :], Bwd)
                n2col = vc_[:, 16:24]
                cw = vc_[:, 24:90]
                cbias = vc_[:, 90:112]

                for seq in range(NSEQ):
                    S.pool(lambda e: e.memset(gh[:], 0.0), writes=[Bgh])
                    for st in range(NST):
                        T0 = st * 512
                        for s in range(4):
                            S.dma(lambda e, seq=seq, T0=T0, s=s: e.dma_start(out=xt[:, s, :], in_=xbuf[seq, T0 + s * 128:T0 + (s + 1) * 128, :]),
                                  reads=[xd[seq][st]], writes=[Bxs[s]])
                        S.dve(lambda e: e.memset(ss[:], 0.0), writes=[Bss])
                        for s in range(4):
                            S.act(lambda e, s=s: e.activation(junk[:], xt[:, s, :], AF.Square, accum_out=ss[:, s:s + 1]), reads=[Bxs[s]], writes=[Bjunk, Bss])
                        for s in range(4):
                            rstd_from_ss(ss[:, s:s + 1], rs[:, s:s + 1], D, Bss, Brs)
                            S.dve(lambda e, s=s: e.tensor_scalar(xn[s % 2][:], xt[:, s, :], rs[:, s:s + 1], None, ALU.mult),
                                  reads=[Bxs[s], Brs], writes=[Bxn[s % 2]])
                            for kh in range(2):
                                bk, Bb = bank("mm")
                                for kk in range(4):
                                    k = kh * 4 + kk
                                    S.pe(lambda e, s=s, k=k, kk=kk, bk=bk: e.matmul(bk[:, kk * 128:(kk + 1) * 128], xn[s % 2][:, k * 128:(k + 1) * 128],
                                                                                   ident[:], start=True, stop=True),
                                         reads=[Bxn[s % 2], Bconst], writes=[Bb])
                                for kk in range(4):
                                    k = kh * 4 + kk
                                    if kk % 2 == 0:
                                        S.act(lambda e, s=s, k=k, kk=kk, bk=bk: e.activation(hT[:, k, s * 128:(s + 1) * 128], bk[:, kk * 128:(kk + 1) * 128],
                                                                                          AF.Copy, scale=n2col[:, k:k + 1]), reads=[Bb, Bw], writes=[BhT])
                                    else:
                                        S.dve(lambda e, s=s, k=k, kk=kk, bk=bk: e.tensor_scalar(hT[:, k, s * 128:(s + 1) * 128], bk[:, kk * 128:(kk + 1) * 128],
                                                                                              n2col[:, k:k + 1], None, ALU.mult), reads=[Bb, Bw], writes=[BhT])
                        hi, ho = st % 2, (st + 1) % 2
                        for c in range(NFC):
                            bg, Bbg = bank("mm")
                            for k in range(8):
                                S.pe(lambda e, c=c, k=k, bg=bg: e.matmul(bg[:], wg[:, k, c * 128:(c + 1) * 128], hT[:, k, :], start=(k == 0), stop=(k == 7)),
                                     reads=[Bwg, BhT], writes=[Bbg])
                            bu, Bbu = bank("s")
                            for k in range(8):
                                S.pe(lambda e, c=c, k=k, bu=bu: e.matmul(bu[:], wu[:, k, c * 128:(c + 1) * 128], hT[:, k, :], start=(k == 0), stop=(k == 7)),
                                     reads=[Bwu, BhT], writes=[Bbu])
                            i = c % 2
                            S.act(lambda e, i=i, bg=bg: e.activation(gb[i][:, 2:514], bg[:], AF.Copy), reads=[Bbg], writes=[Bgb[i]])
                            S.pool(lambda e, i=i, c=c, hi=hi: e.tensor_copy(gb[i][:, 0:2], gh[:, hi, c, :]), reads=[Bgh], writes=[Bgb[i]])
                            S.pool(lambda e, i=i, c=c, ho=ho: e.tensor_copy(gh[:, ho, c, :], gb[i][:, 512:514]), reads=[Bgb[i]], writes=[Bgh])
                            S.act(lambda e, i=i, c=c: e.activation(tA[i][:], gb[i][:, 2:514], AF.Identity, scale=cw[:, 3 * c + 2:3 * c + 3], bias=cbias[:, c:c + 1]),
                                  reads=[Bgb[i], Bw], writes=[BtA[i]])
                            S.dve(lambda e, i=i, c=c: e.scalar_tensor_tensor(tB[i][:], gb[i][:, 1:513], cw[:, 3 * c + 1:3 * c + 2], tA[i][:], ALU.mult, ALU.add),
                                  reads=[Bgb[i], Bw, BtA[i]], writes=[BtB[i]])
                            S.dve(lambda e, i=i, c=c: e.scalar_tensor_tensor(tA[i][:], gb[i][:, 0:512], cw[:, 3 * c:3 * c + 1], tB[i][:], ALU.mult, ALU.add),
                                   reads=[Bgb[i], Bw, BtB[i]], writes=[BtA[i]])
                            S.act(lambda e, i=i: e.activation(tB[i][:], tA[i][:], AF.Silu), reads=[BtA[i]], writes=[BtB[i]])
                            S.dve(lambda e, i=i, c=c, bu=bu: e.tensor_tensor(aT[:, c, :], tB[i][:], bu[:], ALU.mult), reads=[BtB[i], Bbu], writes=[BaT])
                        for s in range(4):
                            for nh in range(2):
                                bk, Bb = bank("acc")
                                for c in range(NFC):
                                    S.pe(lambda e, s=s, nh=nh, c=c, bk=bk: e.matmul(bk[:], aT[:, c, s * 128:(s + 1) * 128], wd[:, c, nh * 512:(nh + 1) * 512],
                                                                                 start=(c == 0), stop=(c == NFC - 1)),
                                         reads=[BaT, Bwd], writes=[Bb])
                                S.dve(lambda e, s=s, nh=nh, bk=bk: e.tensor_tensor(xt[:, s, nh * 512:(nh + 1) * 512], xt[:, s, nh * 512:(nh + 1) * 512], bk[:], ALU.add),
                                      reads=[Bb, Bxs[s]], writes=[Bxs[s]])
                            if not last:
                                S.dma(lambda e, seq=seq, T0=T0, s=s: e.dma_start(out=xbuf[seq, T0 + s * 128:T0 + (s + 1) * 128, :], in_=xt[:, s, :]),
                                      reads=[Bxs[s]], writes=[xd[seq][st]])
                            else:
                                S.act(lambda e, s=s: e.activation(junk[:], xt[:, s, :], AF.Square, accum_out=ss[:, 4 + s:5 + s]), reads=[Bxs[s]], writes=[Bjunk, Bss])
                                rstd_from_ss(ss[:, 4 + s:5 + s], rs[:, 4 + s:5 + s], D, Bss, Brs)
                                S.dve(lambda e, s=s: e.scalar_tensor_tensor(xt[:, s, :], xt[:, s, :], rs[:, 4 + s:5 + s], nfb[:], ALU.mult, ALU.mult),
                                      reads=[Bxs[s], Brs, Bw], writes=[Bxs[s]])
                                out_dmas.append(S.dma(lambda e, seq=seq, T0=T0, s=s: e.dma_start(out=y_out[seq, T0 + s * 128:T0 + (s + 1) * 128, :], in_=xt[:, s, :]),
                                                      reads=[Bxs[s]], writes=[xd[seq][st]]))

        for l in range(L):
            S.barrier()
            phase_M(l)
            if stop_after_M:
                break
            S.barrier()
            phase_F(l, l == L - 1)
        S.emit(final_wait_ops=out_dmas)
    return nc, dbg_out


def _consts():
    p = np.arange(128)[:, None].astype(np.float64)
    q = np.arange(128)[None, :].astype(np.float64)
    c = {}
    c["c_ident"] = np.eye(128, dtype=np.float32)
    tri = np.where(p <= q, 0.0, NEG)
    tri2 = np.where(p > q, 0.0, NEG)
    c["c_tri"] = np.concatenate([tri, tri2], axis=1).astype(np.float32)
    d1 = 30000.0 * (q - 16.0 * p)
    d0 = d1.copy()
    d0[0, :] = -1e9
    c["c_dcmp"] = np.concatenate([d0, d1], axis=1).astype(np.float32)
    ovl = np.zeros((128, 2, 64), np.float32)
    for j in range(2):
        for pp in range(128):
            s = 128 * j + pp
            if s == 0:
                continue
            n = s - 1
            for b in range(64):
                if (16 * n < 64 * b + 64) and (16 * n + 31 >= 64 * b):
                    ovl[pp, j, b] = 1.0
            ovl[pp, j, 0] = 1e6
    c["c_ovl"] = ovl.reshape(128, 128)
    F = np.zeros((128, 128), np.float32)
    for qq in range(128):
        cur = qq // 64
        for j in range(128):
            br = j - 62
            if br == cur:
                F[qq, j] = 3e6
            elif br == cur - 1:
                F[qq, j] = 2e6
            elif br > cur:
                F[qq, j] = -1e9
    c["c_F"] = F
    c["c_E"] = (np.arange(64)[:, None] == (np.arange(4096)[None, :] // 64)).astype(np.float32)
    cinv = np.zeros((128, 4, 16), np.float32)
    for gi, w in enumerate(POOL_W):
        for j in range(16):
            cinv[:, gi, j] = 1.0 / min(j + 1, w)
    c["c_cinv"] = cinv.reshape(128, 64)
    return c


def _prep_weights(inp, L):
    perm = list(range(0, 512))
    for r in range(4):
        perm += list(range(512 + r * 64, 512 + r * 64 + 64)) + list(range(512 + (4 + r) * 64, 512 + (4 + r) * 64 + 64))
    perm += list(range(1024, 1152)) + list(range(1152, 1280)) + list(range(1280, 1408)) + list(range(1536, 1664))
    perm += list(range(1408, 1536)) + list(range(1664, 1792)) + list(range(1792, 1816))
    perm = np.asarray(perm)
    out = {}
    out["w_in"] = np.ascontiguousarray(np.asarray(inp["w_in"])[:, :, perm])
    for k in ("w_out", "w_gate", "w_up", "w_down"):
        out[k] = np.ascontiguousarray(np.asarray(inp[k]))
    out["w_pool"] = np.ascontiguousarray(np.asarray(inp["w_pool"]).transpose(0, 2, 1, 3).reshape(L, 128, 512))
    w1 = np.stack([np.asarray(inp["cmp_w1_k"]), np.asarray(inp["cmp_w1_v"])], axis=1)
    out["cmp_w1"] = np.ascontiguousarray(w1.reshape(L, 2, 32, 64, 128).transpose(0, 1, 3, 2, 4).reshape(L, 2, 64, 32 * 128))
    out["cmp_w2"] = np.ascontiguousarray(np.stack([np.asarray(inp["cmp_w2_k"]), np.asarray(inp["cmp_w2_v"])], axis=1))
    pe = np.stack([np.asarray(inp["cmp_pe_k"]), np.asarray(inp["cmp_pe_v"])], axis=1)
    out["cmp_peT"] = np.ascontiguousarray(pe.transpose(0, 1, 3, 2))
    vec = np.zeros((L, 128, 112), np.float32)
    vec[:, :, 0:8] = np.asarray(inp["norm1"]).reshape(L, 8, 128).transpose(0, 2, 1)
    vec[:, :, 8:12] = np.asarray(inp["s_pool"]).reshape(L, 4, 128).transpose(0, 2, 1)
    vec[:, :, 12:16] = np.asarray(inp["norm_pool_out"]).reshape(L, 4, 128).transpose(0, 2, 1)
    vec[:, :, 16:24] = np.asarray(inp["norm2"]).reshape(L, 8, 128).transpose(0, 2, 1)
    cwv = np.asarray(inp["conv_w"]).reshape(L, 3, NFC, 128).transpose(0, 3, 2, 1)
    vec[:, :, 24:90] = cwv.reshape(L, 128, 66)
    vec[:, :, 90:112] = np.asarray(inp["conv_b"]).reshape(L, NFC, 128).transpose(0, 2, 1)
    out["vecs"] = vec
    out["nao"] = np.ascontiguousarray(np.asarray(inp["norm_attn_out"]).reshape(L, 1, 512))
    out["normf"] = np.ascontiguousarray(np.asarray(inp["norm_f"]).reshape(1, D))
    out.update(_consts())
    return {k: np.ascontiguousarray(v, dtype=np.float32) for k, v in out.items()}


_CACHE = {}


def kernel(**inputs):
    x = np.asarray(inputs["x"], dtype=np.float32)
    B, T, _ = x.shape
    L = np.asarray(inputs["w_in"]).shape[0]
    n_cores = 8
    nseq = B // n_cores
    key = (T, nseq, L)
    if key not in _CACHE:
        _CACHE[key] = build(T, nseq, L)[0]
    nc = _CACHE[key]
    shared = _prep_weights(inputs, L)
    in_maps = []
    for c in range(n_cores):
        m = dict(shared)
        m["x"] = np.ascontiguousarray(x[c * nseq:(c + 1) * nseq])
        in_maps.append(m)
    res = run_bass_kernel_spmd(nc, in_maps, core_ids=list(range(n_cores)))
    return np.concatenate([np.asarray(r["y"]) for r in res.results], axis=0).astype(np.float32)
```

```python
import contextlib
import math
import numpy as np
import concourse.bass as bass
import concourse.mybir as mybir
from concourse.bass_utils import run_bass_kernel_spmd

F32 = mybir.dt.float32
BF16 = mybir.dt.bfloat16
AF = mybir.ActivationFunctionType
ALU = mybir.AluOpType

D = 1024
DFF = 2816
NFC = DFF // 128
EPS = 1e-6
NEG = -30000.0
POOL_W = (2, 4, 8, 16)
N_DMA_SEMS = 24


class Buf:
    __slots__ = ("name", "w", "r", "excl")

    def __init__(self, name="", excl=False):
        self.name = name
        self.w = None
        self.r = {}
        self.excl = excl


class Op:
    __slots__ = ("eng", "pos", "fn", "deps", "needed", "sem", "value", "is_dma")

    def __init__(self, eng, pos, fn, is_dma):
        self.eng = eng
        self.pos = pos
        self.fn = fn
        self.deps = []
        self.needed = False
        self.sem = None
        self.value = None
        self.is_dma = is_dma


class Sched:
    ENGS = ("pe", "act", "dve", "pool", "sp")

    def __init__(self, nc):
        self.nc = nc
        self.ops = {e: [] for e in self.ENGS}
        self.waited = {e: {} for e in self.ENGS}
        self.n_dma = 0
        self.dma_ops = []
        self.barrier_deps = {e: [] for e in self.ENGS}

    def barrier(self):
        last = []
        for e in self.ENGS:
            for op in reversed(self.ops[e]):
                if not op.is_dma:
                    last.append(op)
                    break
        last += self.dma_ops[-N_DMA_SEMS:]
        for e in self.ENGS:
            self.barrier_deps[e] = list(last)

    def _add(self, eng, fn, reads, writes, is_dma=False):
        lst = self.ops[eng]
        op = Op(eng, len(lst), fn, is_dma)
        deps = {}

        def need(d, same_ok):
            if d is None:
                return
            if d.eng == eng and (not d.is_dma) and same_ok:
                return
            deps[id(d)] = d

        for b in reads:
            need(b.w, False)
            if b.excl:
                for d in b.r.values():
                    need(d, True)
        for b in writes:
            need(b.w, True)
            for d in b.r.values():
                need(d, True)
        if self.barrier_deps[eng]:
            for d in self.barrier_deps[eng]:
                if not (d.eng == eng and not d.is_dma):
                    deps[id(d)] = d
            self.barrier_deps[eng] = []
        wt = self.waited[eng]
        if is_dma:
            idx = self.n_dma
            self.n_dma += 1
            if idx >= N_DMA_SEMS:
                prev = self.dma_ops[idx - N_DMA_SEMS]
                deps[id(prev)] = prev
        for d in deps.values():
            if d.is_dma:
                key = ("dma", d.pos % N_DMA_SEMS)
            else:
                key = d.eng
            if wt.get(key, -1) >= d.pos:
                continue
            wt[key] = d.pos
            d.needed = True
            op.deps.append(d)
        if is_dma:
            op.pos = idx
            op.needed = True
            self.dma_ops.append(op)
        for b in reads:
            b.r[("dma", op.pos) if is_dma else eng] = op
        for b in writes:
            b.w = op
            b.r = {}
        lst.append(op)
        return op

    def pe(self, fn, reads=(), writes=()):
        return self._add("pe", fn, reads, writes)

    def act(self, fn, reads=(), writes=()):
        return self._add("act", fn, reads, writes)

    def dve(self, fn, reads=(), writes=()):
        return self._add("dve", fn, reads, writes)

    def pool(self, fn, reads=(), writes=()):
        return self._add("pool", fn, reads, writes)

    def dma(self, fn, reads=(), writes=(), q="sp"):
        return self._add(q, fn, reads, writes, is_dma=True)

    def emit(self, final_wait_ops=()):
        nc = self.nc
        with contextlib.ExitStack() as es:
            esem = {e: es.enter_context(nc.semaphore("s_" + e)) for e in self.ENGS}
            dsem = [es.enter_context(nc.semaphore("d%d" % i)) for i in range(N_DMA_SEMS)]
            for e in self.ENGS:
                cnt = 0
                for op in self.ops[e]:
                    if op.is_dma:
                        op.sem = dsem[op.pos % N_DMA_SEMS]
                        op.value = 16 * (op.pos // N_DMA_SEMS + 1)
                    elif op.needed:
                        cnt += 1
                        op.sem = esem[e]
                        op.value = cnt
            block = es.enter_context(nc.Block())
            decos = {"pe": block.tensor, "act": block.scalar, "dve": block.vector,
                     "pool": block.gpsimd, "sp": block.sync}

            def mk(e, extra):
                def body(eng):
                    for op in self.ops[e]:
                        for d in op.deps:
                            eng.wait_ge(d.sem, d.value)
                        ins = op.fn(eng)
                        if op.is_dma:
                            ins.then_inc(op.sem, 16)
                        elif op.needed:
                            ins.then_inc(op.sem, 1)
                    for d in extra:
                        eng.wait_ge(d.sem, d.value)
                return body

            for e in self.ENGS:
                extra = list(final_wait_ops) if e == "sp" else []
                if not self.ops[e] and not extra:
                    continue
                decos[e](mk(e, extra))


def _ap(t, off, dims):
    return bass.AP(t.tensor if hasattr(t, "tensor") else t, off, [list(d) for d in dims])


def build(T, NSEQ, L, debug=False, stop_after_M=False):
    NT = T // 128
    NST = T // 512
    NSLOT = T // 16
    NCT = NSLOT // 128
    nc = bass.Bass("TRN2", target_bir_lowering=False)

    def din(name, shape):
        return nc.dram_tensor(name, list(shape), F32, kind="ExternalInput").ap()

    x_in = din("x", [NSEQ, T, D])
    w_in = din("w_in", [L, D, 1816])
    w_out = din("w_out", [L, D, D])
    w_gate = din("w_gate", [L, D, DFF])
    w_up = din("w_up", [L, D, DFF])
    w_down = din("w_down", [L, DFF, D])
    w_pool = din("w_pool", [L, 128, 4 * 128])
    w1 = din("cmp_w1", [L, 2, 64, 32 * 128])
    w2 = din("cmp_w2", [L, 2, 128, 64])
    peT = din("cmp_peT", [L, 2, 64, 32])
    vecs = din("vecs", [L, 128, 112])
    nao = din("nao", [L, 1, 512])
    normf = din("normf", [1, D])
    c_ident = din("c_ident", [128, 128])
    c_tri = din("c_tri", [128, 256])
    c_dcmp = din("c_dcmp", [128, 256])
    c_ovl = din("c_ovl", [128, 128])
    c_F = din("c_F", [128, 128])
    c_E = din("c_E", [64, 4096])
    c_cinv = din("c_cinv", [128, 64])
    y_out = nc.dram_tensor("y", [NSEQ, T, D], F32, kind="ExternalOutput").ap()
    xbuf = nc.dram_tensor("xbuf", [NSEQ, T, D], F32).ap()
    dbg_out = {}
    if debug:
        dbg_attn = nc.dram_tensor("dbg_attn", [T, 512], F32, kind="ExternalOutput").ap()
        dbg_imp = nc.dram_tensor("dbg_imp", [T, 2, 64], F32, kind="ExternalOutput").ap()
        dbg_selb = nc.dram_tensor("dbg_selb", [T, 2, 64], BF16, kind="ExternalOutput").ap()

    S = Sched(nc)
    out_dmas = []

    with contextlib.ExitStack() as top:
        def sb(name, shape, dt=F32, es=top):
            return es.enter_context(nc.sbuf_tensor(name, list(shape), dt))

        banks = [top.enter_context(nc.psum_tensor("bank%d" % i, [128, 512], F32)) for i in range(8)]
        bbuf = [Buf("bank%d" % i, excl=True) for i in range(8)]
        rr = {"mm": [0, 1], "s": [2, 3], "acc": [4, 5], "s3": [2, 3, 1], "mma": [0]}
        rr_i = {"mm": 0, "s": 0, "acc": 0, "s3": 0, "mma": 0}

        def bank(kind):
            i = rr[kind][rr_i[kind] % len(rr[kind])]
            rr_i[kind] += 1
            return banks[i], bbuf[i]

        ident = sb("ident", [128, 128], BF16); Bconst = Buf("const")
        ones = sb("ones", [128, 128], BF16)
        epsc = sb("epsc", [128, 1], F32)
        S.dma(lambda e: e.dma_start(out=ident[:], in_=c_ident), writes=[Bconst], q="pool")
        S.dve(lambda e: e.memset(ones[:], 1.0), writes=[Bconst])
        S.dve(lambda e: e.memset(epsc[:], EPS), writes=[Bconst])

        xd = [[Buf("xd%d_%d" % (s, i)) for i in range(NST)] for s in range(NSEQ)]

        def x_src(l, seq):
            return x_in if l == 0 else xbuf

        def dbg(name, ap_sb, shape, reads):
            if not debug:
                return
            t = nc.dram_tensor("dbg_" + name, list(shape), ap_sb.dtype, kind="ExternalOutput").ap()
            dbg_out[name] = t
            out_dmas.append(S.dma(lambda e: e.dma_start(out=t, in_=ap_sb), reads=reads))

        def rstd_from_ss(ss_ap, out_ap, n, Bss, Bout):
            S.act(lambda e: e.activation(out_ap, ss_ap, AF.Ln, scale=1.0 / n, bias=epsc[0:ss_ap.shape[0], 0:1]),
                  reads=[Bss, Bconst], writes=[Bout])
            S.act(lambda e: e.activation(out_ap, out_ap, AF.Exp, scale=-0.5), reads=[Bout], writes=[Bout])

        def phase_M(l):
            with contextlib.ExitStack() as es:
                def t(name, shape, dt=F32):
                    return sb("M%d_" % l + name, shape, dt, es)
                wfm = t("wfm", [128, 8, 1536], BF16); wtm = t("wtm", [128, 8, 280], BF16)
                wo = t("wo", [128, 8, D], BF16); wpl = t("wpl", [128, 512], BF16)
                w1k = t("w1k", [128, 32 * 128], BF16); w1v = t("w1v", [128, 32 * 128], BF16)
                w2k = t("w2k", [128, 128], BF16); w2v = t("w2v", [128, 64], BF16)
                pet = t("pet", [64, 64], BF16); peb = t("peb", [128, 2], F32)
                vc_ = t("vecs", [128, 112], F32); naob = t("naob", [128, 512], F32)
                Bw = Buf("Mweights")
                tri = t("tri", [128, 256], BF16); dcmp = t("dcmp", [128, 256], F32); ovl = t("ovl", [128, 128], BF16)
                Fc = t("Fc", [128, 128], F32); cinv = t("cinv", [128, 64], F32)
                for dst, src in ((tri, c_tri), (ovl, c_ovl)):
                    S.dma(lambda e, dst=dst, src=src: e.dma_start(out=dst[:], in_=src), writes=[Bconst], q="pool")
                for dst, src in ((dcmp, c_dcmp), (Fc, c_F), (cinv, c_cinv)):
                    S.dma(lambda e, dst=dst, src=src: e.dma_start(out=dst[:], in_=src), writes=[Bconst])
                KA = t("KA", [128, T], BF16); KB = t("KB", [128, T], BF16)
                KWX = [t("KWA", [128, T], BF16), t("KWB", [128, T], BF16)]; BKW = [Buf("KWA"), Buf("KWB")]
                KX = [KA, KB]; BKX = [Buf("KA"), Buf("KB")]
                vselA = t("vselA", [128, NT, 2, 65], BF16); vwinA = t("vwinA", [128, NT, 2, 65], BF16)
                KCX = [t("KCA", [128, NSLOT], BF16), t("KCB", [128, NSLOT], BF16)]; BKC = [Buf("KCA"), Buf("KCB")]; vcmpA = t("vcmpA", [128, NCT, 2, 65], BF16)
                glv = t("glv", [128, 2, NSLOT], BF16)
                Bksel, Bkwin, Bvsel, Bvwin = Buf("ksel"), Buf("kwin"), Buf("vsel"), Buf("vwin")
                Bkcmp, Bvcmp, Bglv = Buf("kcmp"), Buf("vcmp"), Buf("glv")
                xt = t("xt", [128, 4, D], F32); Bxt = Buf("xt")
                xn = [t("xn%d" % i, [128, D], BF16) for i in range(2)]; Bxn = [Buf("xn0"), Buf("xn1")]
                ss = t("ss", [128, 8], F32); Bss = Buf("ss")
                rs = t("rs", [128, 8], F32); Brs = Buf("rs")
                hT = t("hT", [128, 8, 512], BF16); BhT = Buf("hT")
                uT = t("uT", [128, 4, 528], F32); BuT = Buf("uT")
                qT = t("qT", [128, 4, 2, 512], BF16); BqT = Buf("qT")
                BqS = [[Buf() for _ in range(2)] for _ in range(4)]
                kcT = t("kcT", [128, 528], BF16); vcT = t("vcT", [128, 528], BF16); BkcT = Buf("kcT"); BvcT = Buf("vcT")
                gat = t("gat", [128, 4, 24], F32); Bgat = Buf("gat")
                ptA = t("ptA", [128, 528], F32); ptB = t("ptB", [128, 528], F32); BptA = Buf("ptA"); BptB = Buf("ptB")
                pooled = [t("pooled%d" % i, [128, 512], BF16) for i in range(2)]; Bpooled = [Buf(), Buf()]
                po = t("po", [128, 4, 512], BF16); Bpo = Buf("po")
                sq = [t("sq%d" % i, [128, 512], BF16) for i in range(2)]; Bsq = [Buf(), Buf()]
                rbc = t("rbc", [128, 512], F32); Brbc = Buf("rbc")
                mixT = t("mixT", [128, 8, 512], BF16); BmixT = Buf("mixT")
                PT = [t("PT%d" % i, [128, 512], BF16) for i in range(4)]; BPT = [Buf(), Buf(), Buf(), Buf()]
                pt_i = [0]
                o2 = [t("o%d" % i, [128, 512], F32) for i in range(2)]; Bo2 = [Buf(), Buf()]
                on2 = [t("on%d" % i, [128, 512], BF16) for i in range(2)]; Bon2 = [Buf(), Buf()]
                otmps = [t("otmp%d" % i, [128, 256], F32) for i in range(2)]; Botmps = [Buf(), Buf()]
                glt = t("glt", [128, 128], BF16); Bglt = Buf("glt")
                cbs = [t("cb%d" % i, [128, 128], BF16) for i in range(2)]; Bcbs = [Buf(), Buf()]
                sm = t("sm", [128, 64], F32); Bsm = Buf("sm")
                smx = [t("smx%d" % i, [128, 16], F32) for i in range(6)]; Bsmx = [Buf() for i in range(6)]
                imps = [t("imp%d" % i, [128, 64], F32) for i in range(2)]; Bimps = [Buf(), Buf()]
                imp2s = [t("impb%d" % i, [128, 64], F32) for i in range(2)]; Bimp2s = [Buf(), Buf()]
                m8s = [t("m8%d" % i, [128, 16], F32) for i in range(2)]; Bm8s = [Buf(), Buf()]
                selbs = [t("selb%d" % i, [128, 128], BF16) for i in range(2)]; Bselbs = [Buf(), Buf()]
                ssa2 = [t("ssa%d" % i, [128, 2], F32) for i in range(2)]; Bssa2 = [Buf(), Buf()]

                Bwfm, Bwtm, Bwo, Bwc = Buf("wfm"), Buf("wtm"), Buf("wo"), Buf("wc")

                def wdma(dst_ap, src_ap, B_):
                    S.dma(lambda e: e.dma_start(out=dst_ap, in_=src_ap), writes=[B_], q="pool")
                S.dma(lambda e: e.dma_start(out=vc_[:], in_=vecs[l]), writes=[Bw])
                S.dma(lambda e: e.dma_start(out=naob[:], in_=nao[l].partition_broadcast(128)), writes=[Bw])
                wdma(wpl[:], w_pool[l], Bw)
                wv = w_in[l].rearrange("(k p) c -> p k c", p=128)
                for k in range(8):
                    wdma(wfm[:, k, :], wv[:, k, 0:1536], Bwfm)
                wdma(wtm[:, :, :], wv[:, :, 1536:1816], Bwtm)
                for h in range(2):
                    wdma(w1k[64 * h:64 * h + 64, :], w1[l, 0], Bwc)
                    wdma(w1v[64 * h:64 * h + 64, :], w1[l, 1], Bwc)
                    wdma(w2k[:, 64 * h:64 * h + 64], w2[l, 0], Bwc)
                wdma(w2v[:], w2[l, 1], Bwc)
                wdma(pet[:, 0:32], peT[l, 0], Bwc); wdma(pet[:, 32:64], peT[l, 1], Bwc)
                wov = w_out[l].rearrange("(k p) c -> p k c", p=128)
                for k in range(8):
                    wdma(wo[:, k, :], wov[:, k, :], Bwo)
                g1col = vc_[:, 0:8]; spool = vc_[:, 8:12]; npo = vc_[:, 12:16]

                S.pool(lambda e: e.memset(vselA[:], 1.0), writes=[Bvsel])
                S.pool(lambda e: e.memset(vwinA[:], 1.0), writes=[Bvwin])
                S.pool(lambda e: e.memset(vcmpA[:], 1.0), writes=[Bvcmp])
                for g_ in range(2):
                    S.pool(lambda e, g_=g_: e.memset(KCX[g_][:], 0.0), writes=[BKC[g_]])
                    S.pool(lambda e, g_=g_: e.memset(KWX[g_][:], 0.0), writes=[BKW[g_]])
                S.pool(lambda e: e.memset(glv[:], 0.0), writes=[Bglv])
                S.dma(lambda e: e.dma_start(out=KA[64:128, :], in_=c_E[:, 0:T]), writes=[BKX[0]], q="pool")
                S.dma(lambda e: e.dma_start(out=KB[0:64, :], in_=c_E[:, 0:T]), writes=[BKX[1]], q="pool")
                S.pool(lambda e: e.memset(KA[0:64, :], 0.0), writes=[BKX[0]])
                S.pool(lambda e: e.memset(KB[64:128, :], 0.0), writes=[BKX[1]])
                S.pool(lambda e: e.memset(qT[:], 0.0), writes=[BqT])
                for g_ in range(2):
                    S.pool(lambda e, g_=g_: e.memset(selbs[g_][:], 0.0), writes=[Bselbs[g_]])
                bk, Bb = banks[7], bbuf[7]
                for kv, wt_ in ((0, w1k), (1, w1v)):
                    for li in range(32):
                        S.pe(lambda e, kv=kv, wt_=wt_, li=li: e.matmul(bk[:, kv:kv + 1], wt_[0:64, li * 128:(li + 1) * 128],
                                                                      pet[:, kv * 32 + li:kv * 32 + li + 1], start=(li == 0), stop=(li == 31)),
                             reads=[Bwc], writes=[Bb])
                S.dve(lambda e: e.tensor_copy(peb[:], bk[:, 0:2]), reads=[Bb], writes=[Bwc])

                def load_x(seq_, st_):
                    xs_ = x_src(l, seq_)
                    S.dma(lambda e: e.dma_start(out=xt[:], in_=xs_[seq_, st_ * 512:st_ * 512 + 512, :].rearrange("(s p) d -> p s d", p=128)),
                          reads=[xd[seq_][st_]], writes=[Bxt])
                load_x(0, 0)
                xr = [t("xr%d" % i, [128, 512], F32) for i in range(2)]; Bxr = [Buf(), Buf()]
                for seq in range(NSEQ):
                    xs = x_src(l, seq)
                    S.pool(lambda e: e.memset(uT[:, :, 0:16], 0.0), writes=[BuT])
                    S.pool(lambda e: e.memset(kcT[:, 0:16], 0.0), writes=[BkcT])
                    S.pool(lambda e: e.memset(vcT[:, 0:16], 0.0), writes=[BvcT])
                    for st in range(NST):
                        T0 = st * 512
                        if st > 0:
                            S.pool(lambda e: e.tensor_copy(uT[:, :, 0:16], uT[:, :, 512:528]), reads=[BuT], writes=[BuT])
                            S.pool(lambda e: e.tensor_copy(kcT[:, 0:16], kcT[:, 512:528]), reads=[BkcT], writes=[BkcT])
                            S.pool(lambda e: e.tensor_copy(vcT[:, 0:16], vcT[:, 512:528]), reads=[BvcT], writes=[BvcT])
                        S.pool(lambda e: e.memset(ss[:], 0.0), writes=[Bss])
                        for s in range(4):
                            S.act(lambda e, s=s: e.activation(xn[s % 2][:], xt[:, s, :], AF.Square, accum_out=ss[:, s:s + 1]),
                                  reads=[Bxt], writes=[Bxn[s % 2], Bss])
                        for s in range(4):
                            rstd_from_ss(ss[:, s:s + 1], rs[:, s:s + 1], D, Bss, Brs)
                            S.act(lambda e, s=s: e.activation(xn[s % 2][:], xt[:, s, :], AF.Copy, scale=rs[:, s:s + 1]),
                                  reads=[Bxt, Brs], writes=[Bxn[s % 2]])
                            for kh in range(2):
                                bk, Bb = bank("mm")
                                for kk in range(4):
                                    k = kh * 4 + kk
                                    S.pe(lambda e, s=s, k=k, kk=kk, bk=bk: e.matmul(bk[:, kk * 128:(kk + 1) * 128], xn[s % 2][:, k * 128:(k + 1) * 128],
                                                                                   ident[:], start=True, stop=True),
                                         reads=[Bxn[s % 2], Bconst], writes=[Bb])
                                for kk in range(4):
                                    k = kh * 4 + kk
                                    eng = S.act if kk % 2 == 0 else S.dve
                                    if kk % 2 == 0:
                                        S.act(lambda e, s=s, k=k, kk=kk, bk=bk: e.activation(hT[:, k, s * 128:(s + 1) * 128], bk[:, kk * 128:(kk + 1) * 128],
                                                                                          AF.Copy, scale=g1col[:, k:k + 1]),
                                              reads=[Bb, Bw], writes=[BhT])
                                    else:
                                        S.dve(lambda e, s=s, k=k, kk=kk, bk=bk: e.tensor_scalar(hT[:, k, s * 128:(s + 1) * 128], bk[:, kk * 128:(kk + 1) * 128],
                                                                                              g1col[:, k:k + 1], None, ALU.mult),
                                              reads=[Bb, Bw], writes=[BhT])
                        if st + 1 < NST:
                            load_x(seq, st + 1)
                        elif seq + 1 < NSEQ:
                            load_x(seq + 1, 0)
                        for c in range(12):
                            bk, Bb = bank("mm")
                            for k in range(8):
                                S.pe(lambda e, c=c, k=k, bk=bk: e.matmul(bk[:], wfm[:, k, c * 128:(c + 1) * 128], hT[:, k, :], start=(k == 0), stop=(k == 7)),
                                     reads=[Bwfm, BhT], writes=[Bb])
                            if c < 4:
                                S.act(lambda e, c=c, bk=bk: e.activation(uT[:, c, 16:528], bk[:], AF.Copy), reads=[Bb], writes=[BuT])
                            elif c < 8:
                                r = c - 4
                                for g_ in range(2):
                                    dst = _ap(qT, g_ * 64 * 4096 + g_ * 512 + r * 128, [[4096, 64], [1024, 4], [1, 128]])
                                    src = _ap(bk, g_ * 64 * 512, [[512, 64], [128, 4], [1, 128]])
                                    if g_ == 0:
                                        S.dve(lambda e, dst=dst, src=src: e.tensor_copy(dst, src), reads=[Bb], writes=[BqT])
                                    else:
                                        S.act(lambda e, dst=dst, src=src: e.activation(dst, src, AF.Copy), reads=[Bb], writes=[BqT])
                            elif c == 8:
                                S.act(lambda e, bk=bk: e.activation(kcT[:, 16:528], bk[:], AF.Copy), reads=[Bb], writes=[BkcT])
                            elif c == 9:
                                S.dve(lambda e, bk=bk: e.tensor_copy(vcT[:, 16:528], bk[:]), reads=[Bb], writes=[BvcT])
                            elif c == 10:
                                S.act(lambda e, bk=bk, T0=T0: e.activation(KA[0:64, T0:T0 + 512], bk[0:64, :], AF.Copy), reads=[Bb], writes=[BKX[0]])
                                S.dve(lambda e, bk=bk, T0=T0: e.tensor_copy(KB[64:128, T0:T0 + 512], bk[64:128, :]), reads=[Bb], writes=[BKX[1]])
                            else:
                                S.dve(lambda e, bk=bk, T0=T0: e.tensor_copy(KWX[0][0:64, T0:T0 + 512], bk[0:64, :]), reads=[Bb], writes=[BKW[0]])
                                S.act(lambda e, bk=bk, T0=T0: e.activation(KWX[1][64:128, T0:T0 + 512], bk[64:128, :], AF.Copy), reads=[Bb], writes=[BKW[1]])
                        for s in range(4):
                            ti = st * 4 + s
                            bk, Bb = bank("mm")
                            for k in range(8):
                                S.pe(lambda e, s=s, k=k, bk=bk: e.matmul(bk[:, 0:280], hT[:, k, s * 128:(s + 1) * 128], wtm[:, k, :], start=(k == 0), stop=(k == 7)),
                                     reads=[Bwtm, BhT], writes=[Bb])
                            src = _ap(bk, 0, [[512, 128], [64, 2], [1, 64]])
                            S.act(lambda e, ti=ti, src=src: e.activation(vselA[:, ti, :, 0:64], src, AF.Copy), reads=[Bb], writes=[Bvsel])
                            src2 = _ap(bk, 128, [[512, 128], [64, 2], [1, 64]])
                            S.dve(lambda e, ti=ti, src2=src2: e.tensor_copy(vwinA[:, ti, :, 0:64], src2), reads=[Bb], writes=[Bvwin])
                            S.act(lambda e, s=s, bk=bk: e.activation(gat[:, s, :], bk[:, 256:280], AF.Exp, scale=-1.0), reads=[Bb], writes=[Bgat])
                            S.dve(lambda e, s=s: e.tensor_scalar(gat[:, s, :], gat[:, s, :], 1.0, None, ALU.add), reads=[Bgat], writes=[Bgat])
                            S.dve(lambda e, s=s: e.reciprocal(gat[:, s, :], gat[:, s, :]), reads=[Bgat], writes=[Bgat])
                        s0 = 32 * st
                        for g in range(2):
                            bk, Bb = banks[6 + g], bbuf[6 + g]
                            R0 = 64 * g
                            for kv, (src_t, Bsrc, wt_) in enumerate(((kcT, BkcT, w1k), (vcT, BvcT, w1v))):
                                for li in range(32):
                                    S.pe(lambda e, kv=kv, src_t=src_t, wt_=wt_, li=li, R0=R0, bk=bk: e.matmul(
                                        bk[:, kv * 32:(kv + 1) * 32], wt_[R0:R0 + 64, li * 128:(li + 1) * 128],
                                        src_t[R0:R0 + 64, li:li + 16 * 31 + 1:16], start=(li == 0), stop=(li == 31)),
                                        reads=[Bwc, Bsrc], writes=[Bb])
                            for kv in range(2):
                                S.act(lambda e, kv=kv, g=g, bk=bk: e.activation(glt[:, g * 64 + kv * 32:g * 64 + kv * 32 + 32], bk[:, kv * 32:(kv + 1) * 32],
                                                                            AF.Gelu_apprx_tanh, bias=peb[:, kv:kv + 1]),
                                      reads=[Bb, Bwc], writes=[Bglt])
                            S.dve(lambda e, g=g, s0=s0: e.tensor_copy(glv[:, g, s0:s0 + 32], glt[:, g * 64 + 32:g * 64 + 64]), reads=[Bglt], writes=[Bglv])
                        for g in range(2):
                            bk, Bb = bank("mm")
                            S.pe(lambda e, g=g, bk=bk: e.matmul(bk[:, 0:32], w2k[:], glt[:, g * 64:g * 64 + 32], start=True, stop=True),
                                 reads=[Bwc, Bglt], writes=[Bb])
                            S.act(lambda e, g=g, bk=bk, s0=s0: e.activation(KCX[g][64 * g:64 * g + 64, s0:s0 + 32], bk[64 * g:64 * g + 64, 0:32], AF.Copy),
                                  reads=[Bb], writes=[BKC[g]])
                            jt = st // 4
                            S.pe(lambda e, g=g, bk=bk, jt=jt: e.matmul(bk[:, 64:128], glv[:, g, jt * 128:(jt + 1) * 128], w2v[:], start=True, stop=True),
                                 reads=[Bwc, Bglv], writes=[Bb])
                            S.dve(lambda e, g=g, bk=bk, jt=jt: e.tensor_copy(vcmpA[:, jt, g, 0:64], bk[:, 64:128]), reads=[Bb], writes=[Bvcmp])
                        pipe = {"pend": [], "delayed": []}

                        def step_delayed():
                            nd = []
                            for cnt, fn in pipe["delayed"]:
                                if cnt <= 0:
                                    fn()
                                else:
                                    nd.append((cnt - 1, fn))
                            pipe["delayed"] = nd

                        def finish_one():
                            prev = pipe["pend"].pop(0)
                            prev["pv_fn"](prev["P"])
                            if prev["post"] is not None:
                                prev["post"]()

                        def run_task(task):
                            for f in task["pre"]:
                                f()
                            task["P"] = task["s_fn"]()
                            pipe["pend"].append(task)
                            if len(pipe["pend"]) > 2:
                                finish_one()
                            step_delayed()

                        for s in range(4):
                            for task in attn_tasks(l, seq, st, s, pipe, locals()):
                                run_task(task)
                        while pipe["pend"]:
                            finish_one()
                        while pipe["delayed"]:
                            step_delayed()
                        bst, Bbst = banks[6], bbuf[6]
                        for p in range(4):
                            w = POOL_W[p]
                            cur, Bcur = uT[:, p, :], BuT
                            tmp = [(ptA, BptA), (ptB, BptB)]
                            step = 1
                            i = 0
                            while step < w:
                                dstt, Bd = tmp[i % 2]
                                lo = 2 * step - 1
                                S.pool(lambda e, dstt=dstt, cur=cur, lo=lo, step=step: e.tensor_tensor(dstt[:, lo:528], cur[:, lo:528], cur[:, lo - step:528 - step], ALU.add),
                                       reads=[Bcur], writes=[Bd])
                                cur, Bcur = dstt[:, :], Bd
                                step *= 2
                                i += 1
                            pl, Bpl = pooled[p % 2], Bpooled[p % 2]
                            S.dve(lambda e, pl=pl, cur=cur, p=p, w=w: e.scalar_tensor_tensor(pl[:], cur[:, 16:528], 1.0 / w, uT[:, p, 16:528], ALU.mult, ALU.subtract),
                                   reads=[Bcur, BuT], writes=[Bpl])
                            if st == 0:
                                S.pool(lambda e, cur=cur, p=p: e.tensor_tensor(sm[:, 0:16], cur[:, 16:32], cinv[:, p * 16:(p + 1) * 16], ALU.mult),
                                       reads=[Bcur, Bconst], writes=[Bsm])
                                S.pool(lambda e, pl=pl, p=p: e.tensor_tensor(pl[:, 0:16], sm[:, 0:16], uT[:, p, 16:32], ALU.subtract),
                                       reads=[Bsm, BuT], writes=[Bpl])
                            bk, Bb = bank("mm")
                            S.pe(lambda e, p=p, pl=pl, bk=bk: e.matmul(bk[:], wpl[:, p * 128:(p + 1) * 128], pl[:], start=True, stop=True),
                                 reads=[Bw, Bpl], writes=[Bb])
                            S.act(lambda e, p=p, bk=bk: e.activation(po[:, p, :], bk[:], AF.Copy, scale=spool[:, p:p + 1]), reads=[Bb, Bw], writes=[Bpo])
                            S.dve(lambda e, p=p: e.tensor_tensor(sq[p % 2][:], po[:, p, :], po[:, p, :], ALU.mult), reads=[Bpo], writes=[Bsq[p % 2]])
                            S.pe(lambda e, p=p: e.matmul(bst[:], ones[:], sq[p % 2][:], start=(p == 0), stop=(p == 3)),
                                 reads=[Bconst, Bsq[p % 2]], writes=[Bbst])
                        rstd_from_ss(bst[:], rbc[:], 512, Bbst, Brbc)
                        for p in range(4):
                            S.dve(lambda e, p=p: e.scalar_tensor_tensor(mixT[:, p, :], po[:, p, :], npo[:, p:p + 1], rbc[:], ALU.mult, ALU.mult),
                                  reads=[Bpo, Bw, Brbc], writes=[BmixT])
                        xdst = y_out if stop_after_M else xbuf
                        for s in range(4):
                            for nh in range(2):
                                i = (s * 2 + nh) % 2
                                r0 = T0 + s * 128
                                S.dma(lambda e, i=i, r0=r0, nh=nh, xs=xs, seq=seq: e.dma_start(out=xr[i][:], in_=xs[seq, r0:r0 + 128, nh * 512:(nh + 1) * 512]),
                                      reads=([xd[seq][st]] if l > 0 else []), writes=[Bxr[i]])
                                bk, Bb = bank("mm")
                                for k in range(8):
                                    S.pe(lambda e, s=s, nh=nh, k=k, bk=bk: e.matmul(bk[:], mixT[:, k, s * 128:(s + 1) * 128], wo[:, k, nh * 512:(nh + 1) * 512],
                                                                                 start=(k == 0), stop=(k == 7)),
                                         reads=[BmixT, Bwo], writes=[Bb])
                                S.dve(lambda e, i=i, bk=bk: e.tensor_tensor(xr[i][:], xr[i][:], bk[:], ALU.add), reads=[Bb, Bxr[i]], writes=[Bxr[i]])
                                od = S.dma(lambda e, i=i, r0=r0, nh=nh, seq=seq, xdst=xdst: e.dma_start(out=xdst[seq, r0:r0 + 128, nh * 512:(nh + 1) * 512], in_=xr[i][:]),
                                           reads=[Bxr[i]], writes=[xd[seq][st]])
                                if stop_after_M:
                                    out_dmas.append(od)

        def attn_tasks(l, seq, st, s, pipe, L_):
            qT = L_["qT"]; BqT = L_["BqT"]; PT = L_["PT"]; BPT = L_["BPT"]; pt_i = L_["pt_i"]
            KCX, BKC, vcmpA, Bvcmp = L_["KCX"], L_["BKC"], L_["vcmpA"], L_["Bvcmp"]
            KX, BKX, BqS, vselA, Bvsel = L_["KX"], L_["BKX"], L_["BqS"], L_["vselA"], L_["Bvsel"]
            KWX, BKW, vwinA, Bvwin = L_["KWX"], L_["BKW"], L_["vwinA"], L_["Bvwin"]
            cbs, Bcbs, smx, Bsmx = L_["cbs"], L_["Bcbs"], L_["smx"], L_["Bsmx"]
            imps, Bimps, imp2s, Bimp2s, m8s, Bm8s = L_["imps"], L_["Bimps"], L_["imp2s"], L_["Bimp2s"], L_["m8s"], L_["Bm8s"]
            selbs, Bselbs = L_["selbs"], L_["Bselbs"]
            gat, Bgat, o2, Bo2, otmps, Botmps = L_["gat"], L_["Bgat"], L_["o2"], L_["Bo2"], L_["otmps"], L_["Botmps"]
            tri, dcmp, ovl, Fc = L_["tri"], L_["dcmp"], L_["ovl"], L_["Fc"]
            ssa2, Bssa2, on2, Bon2 = L_["ssa2"], L_["Bssa2"], L_["on2"], L_["Bon2"]
            naob, Bw, mixT, BmixT = L_["naob"], L_["Bw"], L_["mixT"], L_["BmixT"]
            qi = st * 4 + s
            t0 = qi * 128
            ncmp = 1 if (NCT == 1 or qi < 16) else 2
            ob = s % 2
            o_t, Bo = o2[ob], Bo2[ob]
            tasks = []

            def mk_s(kT_ap, Bk, bias_fns, qrhs, extra_reads=()):
                def s_fn():
                    bk, Bb = bank("s3")
                    n = len(bias_fns)
                    for i, (fn, rd) in enumerate(bias_fns):
                        S.pe(lambda e, fn=fn, i=i, bk=bk: fn(e, bk, i == 0), reads=rd, writes=[Bb])
                    S.pe(lambda e, bk=bk, n=n: e.matmul(bk[:], kT_ap, qrhs, start=(n == 0), stop=True), reads=[Bk, BqT] + list(extra_reads), writes=[Bb])
                    i = pt_i[0] % 4
                    pt_i[0] += 1
                    S.act(lambda e, bk=bk, i=i: e.activation(PT[i][:], bk[:], AF.Exp, scale=0.125), reads=[Bb], writes=[BPT[i]])
                    return (PT[i], BPT[i])
                return s_fn

            def mk_pv(acc, Bacc, v_ap, Bv, first, last, impj=None, bimp=None, Bbimp=None, nj=1):
                def pv_fn(PP):
                    P, BP = PP
                    for r in range(4):
                        S.pe(lambda e, r=r: e.matmul(acc[:, r * 65:(r + 1) * 65], P[:, r * 128:(r + 1) * 128], v_ap, start=(first and r == 0), stop=last),
                             reads=[BP, Bv], writes=[Bacc])
                    if impj is not None:
                        for r in range(4):
                            S.pe(lambda e, r=r: e.matmul(bimp[:, r * 64:(r + 1) * 64], P[:, r * 128:(r + 1) * 128], ovl[:, impj * 64:(impj + 1) * 64],
                                                        start=(impj == 0 and r == 0), stop=(impj == nj - 1)),
                                 reads=[BP, Bconst], writes=[Bbimp])
                return pv_fn

            def combine(acc, Bacc, g, b, bi, first):
                sm, Bsm = smx[bi], Bsmx[bi]
                sums = _ap(acc, 64, [[512, 128], [65, 4]])
                S.dve(lambda e: e.tensor_scalar(sm[:, 0:4], sums, 1e-30, None, ALU.max), reads=[Bacc], writes=[Bsm])
                S.dve(lambda e: e.reciprocal(sm[:, 4:8], sm[:, 0:4]), reads=[Bsm], writes=[Bsm])
                gsl = _ap(gat, s * 24 + g * 12 + b, [[96, 128], [3, 4]])
                S.dve(lambda e: e.tensor_tensor(sm[:, 8:12], sm[:, 4:8], gsl, ALU.mult), reads=[Bsm, Bgat], writes=[Bsm])
                in0 = _ap(acc, 0, [[512, 128], [65, 4], [1, 64]])
                in1 = _ap(sm, 8, [[16, 128], [1, 4], [0, 64]])
                if first:
                    S.dve(lambda e: e.tensor_tensor(_ap(o_t, g * 256, [[512, 128], [64, 4], [1, 64]]), in0, in1, ALU.mult), reads=[Bacc, Bsm], writes=[Bo])
                else:
                    ot_, Bot_ = otmps[g], Botmps[g]
                    S.dve(lambda e: e.tensor_tensor(_ap(ot_, 0, [[256, 128], [64, 4], [1, 64]]), in0, in1, ALU.mult), reads=[Bacc, Bsm], writes=[Bot_])
                    S.pool(lambda e: e.tensor_tensor(o_t[:, g * 256:(g + 1) * 256], o_t[:, g * 256:(g + 1) * 256], ot_[:], ALU.add),
                           reads=[Bot_, Bo], writes=[Bo])

            def rep4(t_, off, pstride):
                return _ap(t_, off, [[pstride, 128], [0, 4], [1, 128]])

            pre_cb = []
            for j in range(ncmp):
                cval = 30000.0 * (t0 - 2048 * j - 15)
                pre_cb.append(lambda j=j, cval=cval: S.dve(lambda e: e.tensor_scalar(cbs[j][:], dcmp[:, j * 128:(j + 1) * 128], cval, 0.0, ALU.add, ALU.min),
                                                           reads=[Bconst], writes=[Bcbs[j]]))
            for g in range(2):
                R0 = 64 * g
                qrhs = qT[R0:R0 + 64, s, g, :]
                acc, Bacc = bank("acc")
                bimp, Bbimp = banks[6 + g], bbuf[6 + g]

                def post_cmp(acc=acc, Bacc=Bacc, g=g, bimp=bimp, Bbimp=Bbimp):
                    combine(acc, Bacc, g, 0, g, True)
                    sm, Bsm = smx[g], Bsmx[g]
                    imp, Bimp, imp2, Bimp2, m8, Bm8, selb, Bselb = imps[g], Bimps[g], imp2s[g], Bimp2s[g], m8s[g], Bm8s[g], selbs[g], Bselbs[g]
                    for r in range(4):
                        if r == 0:
                            S.dve(lambda e: e.tensor_scalar(imp[:], bimp[:, 0:64], sm[:, 4:5], None, ALU.mult), reads=[Bbimp, Bsm], writes=[Bimp])
                        else:
                            S.dve(lambda e, r=r: e.scalar_tensor_tensor(imp[:], bimp[:, r * 64:(r + 1) * 64], sm[:, 4 + r:5 + r], imp[:], ALU.mult, ALU.add),
                                  reads=[Bbimp, Bsm, Bimp], writes=[Bimp])
                    S.dve(lambda e: e.tensor_tensor(imp[:], imp[:], Fc[:, 62 - 2 * qi:126 - 2 * qi], ALU.add), reads=[Bimp, Bconst], writes=[Bimp])
                    S.dve(lambda e: e.max(out=m8[:, 0:8], in_=imp[:]), reads=[Bimp], writes=[Bm8])
                    S.dve(lambda e: e.match_replace(out=imp2[:], in_to_replace=m8[:, 0:8], in_values=imp[:], imm_value=-3e9), reads=[Bimp, Bm8], writes=[Bimp2])
                    S.dve(lambda e: e.max(out=m8[:, 8:16], in_=imp2[:]), reads=[Bimp2], writes=[Bm8])
                    S.dve(lambda e: e.tensor_scalar(selb[:, 64 * (1 - g):64 * (1 - g) + 64], imp[:], m8[:, 15:16], None, ALU.is_lt), reads=[Bimp, Bm8], writes=[Bselb])

                for j in range(ncmp):
                    bias = [(lambda e, bk, st_, j=j: e.matmul(bk[:], ident[:], rep4(cbs[j], 0, 128), start=st_, stop=False), [Bconst, Bcbs[j]])]
                    tasks.append(dict(pre=(pre_cb if (g == 0 and j == 0) else []),
                                      s_fn=mk_s(KCX[g][:, j * 128:(j + 1) * 128], BKC[g], bias, qT[:, s, g, :]),
                                      pv_fn=mk_pv(acc, Bacc, vcmpA[:, j, g, :], Bvcmp, j == 0, j == ncmp - 1, impj=j, bimp=bimp, Bbimp=Bbimp, nj=ncmp),
                                      post=(post_cmp if j == ncmp - 1 else None)))

            def selb_transpose(g):
                def f():
                    bk, Bb = bank("mma")
                    S.pe(lambda e: e.matmul(bk[:, 0:128], selbs[g][:], ident[:], start=True, stop=True), reads=[Bselbs[g], Bconst], writes=[Bb])
                    h0 = 64 * (1 - g)
                    dst = _ap(qT, h0 * 4096 + s * 1024 + g * 512, [[4096, 64], [128, 4], [1, 128]])
                    src = _ap(bk, h0 * 512, [[512, 64], [0, 4], [1, 128]])
                    S.act(lambda e: e.activation(dst, src, AF.Copy, scale=NEG), reads=[Bb], writes=[BqS[s][g]])
                return f
            kts = [kt for kt in range(qi - 4, qi + 1) if kt >= 0]
            for g in range(2):
                R0 = 64 * g
                qrhs = qT[R0:R0 + 64, s, g, :]
                acc, Bacc = bank("acc")
                for i, kt in enumerate(kts):
                    bias = []
                    if kt == qi:
                        bias.append((lambda e, bk, st_: e.matmul(bk[:], ident[:], rep4(tri, 0, 256), start=st_, stop=False), [Bconst]))
                    if kt == qi - 4:
                        bias.append((lambda e, bk, st_: e.matmul(bk[:], ident[:], rep4(tri, 128, 256), start=st_, stop=False), [Bconst]))
                    pre = []
                    if g == 1 and i == max(0, len(kts) - 2):
                        pre = [selb_transpose(0)]
                    tasks.append(dict(pre=pre, s_fn=mk_s(KWX[g][:, kt * 128:(kt + 1) * 128], BKW[g], bias, qT[:, s, g, :]),
                                      pv_fn=mk_pv(acc, Bacc, vwinA[:, kt, g, :], Bvwin, i == 0, i == len(kts) - 1),
                                      post=((lambda acc=acc, Bacc=Bacc, g=g: combine(acc, Bacc, g, 2, 2 + g, False)) if i == len(kts) - 1 else None)))
            def finalize():
                if debug and seq == 0 and l == 0:
                    out_dmas.append(S.dma(lambda e: e.dma_start(out=dbg_attn[t0:t0 + 128, :], in_=o_t[:]), reads=[Bo]))
                ssa, Bssa, on, Bon = ssa2[ob], Bssa2[ob], on2[ob], Bon2[ob]
                S.dve(lambda e: e.memset(ssa[:], 0.0), writes=[Bssa])
                S.act(lambda e: e.activation(on[:], o_t[:], AF.Square, accum_out=ssa[:, 0:1]), reads=[Bo], writes=[Bon, Bssa])
                rstd_from_ss(ssa[:, 0:1], ssa[:, 1:2], 512, Bssa, Bssa)
                S.dve(lambda e: e.scalar_tensor_tensor(on[:], o_t[:], ssa[:, 1:2], naob[:], ALU.mult, ALU.mult), reads=[Bo, Bssa, Bw], writes=[Bon])

                def tr():
                    bk, Bb = bank("mma")
                    for c in range(4):
                        S.pe(lambda e, c=c: e.matmul(bk[:, c * 128:(c + 1) * 128], on[:, c * 128:(c + 1) * 128], ident[:], start=True, stop=True),
                             reads=[Bon, Bconst], writes=[Bb])
                    dst = _ap(mixT, 4 * 512 + s * 128, [[4096, 128], [512, 4], [1, 128]])
                    src = _ap(bk, 0, [[512, 128], [128, 4], [1, 128]])
                    S.act(lambda e: e.activation(dst, src, AF.Copy), reads=[Bb], writes=[BmixT])
                pipe["delayed"].append((3, tr))

            for g in range(2):
                R0 = 64 * g
                qrhs = qT[R0:R0 + 64, s, g, :]
                acc, Bacc = bank("acc")
                for kt in range(qi + 1):
                    bias = []
                    if kt == qi:
                        bias.append((lambda e, bk, st_: e.matmul(bk[:], ident[:], rep4(tri, 0, 256), start=st_, stop=False), [Bconst]))
                    pre = []
                    if g == 0 and kt == min(qi, 2):
                        pre = [selb_transpose(1)]

                    def post_sel(acc=acc, Bacc=Bacc, g=g):
                        combine(acc, Bacc, g, 1, 4 + g, False)
                        if g == 1:
                            finalize()
                    tasks.append(dict(pre=pre, s_fn=mk_s(KX[g][:, kt * 128:(kt + 1) * 128], BKX[g], bias, qT[:, s, g, :], extra_reads=[BqS[s][g]]),
                                      pv_fn=mk_pv(acc, Bacc, vselA[:, kt, g, :], Bvsel, kt == 0, kt == qi),
                                      post=(post_sel if kt == qi else None)))
            return tasks

        def phase_F(l, last):
            with contextlib.ExitStack() as es:
                def t(name, shape, dt=F32):
                    return sb("F%d_" % l + name, shape, dt, es)
                wg = t("wg", [128, 8, DFF], BF16); wu = t("wu", [128, 8, DFF], BF16); wd = t("wd", [128, NFC, D], BF16)
                vc_ = t("vecs", [128, 112], F32); Bw = Buf("Fweights")
                nfb = t("nfb", [128, D], F32) if last else None
                xl = [t("xl%d" % i, [128, D], F32) for i in range(2)]; Bxl = [Buf("xl0"), Buf("xl1")]
                xn = [t("xn%d" % i, [128, D], BF16) for i in range(2)]; Bxn = [Buf(), Buf()]
                junk = t("junk", [128, D], BF16); Bjunk = Buf()
                ss = t("ss", [128, 8], F32); Bss = Buf(); rs = t("rs", [128, 8], F32); Brs = Buf()
                hT = t("hT", [128, 8, 512], BF16); BhT = Buf()
                aT = t("aT", [128, NFC, 512], BF16); BaT = Buf()
                gb = [t("gb%d" % i, [128, 514], F32) for i in range(2)]; Bgb = [Buf(), Buf()]
                tA = [t("tA%d" % i, [128, 512], F32) for i in range(2)]; BtA = [Buf(), Buf()]
                tB = [t("tB%d" % i, [128, 512], F32) for i in range(2)]; BtB = [Buf(), Buf()]
                gh = t("gh", [128, 2, NFC, 2], F32); Bgh = Buf()

                Bwg, Bwu, Bwd = Buf("wg"), Buf("wu"), Buf("wd")

                def wdma(dst_ap, src_ap, B_):
                    S.dma(lambda e: e.dma_start(out=dst_ap, in_=src_ap), writes=[B_], q="pool")
                S.dma(lambda e: e.dma_start(out=vc_[:], in_=vecs[l]), writes=[Bw])
                if last:
                    S.dma(lambda e: e.dma_start(out=nfb[:], in_=normf.partition_broadcast(128)), writes=[Bw])
                gv = w_gate[l].rearrange("(k p) c -> p k c", p=128)
                uv = w_up[l].rearrange("(k p) c -> p k c", p=128)
                dv = w_down[l].rearrange("(c p) d -> p c d", p=128)
                for k in range(8):
                    wdma(wg[:, k, :], gv[:, k, :], Bwg)
                for k in range(8):
                    wdma(wu[:, k, :], uv[:, k, :], Bwu)
                for c in range(NFC):
                    wdma(wd[:, c, :], dv[:, c, :], Bwd)
                n2col = vc_[:, 16:24]
                cw = vc_[:, 24:90]
                cbias = vc_[:, 90:112]

                subtiles = [(q_, st_, s_) for q_ in range(NSEQ) for st_ in range(NST) for s_ in range(4)]

                def load_sub(n_):
                    if n_ >= len(subtiles):
                        return
                    q_, st_, s_ = subtiles[n_]
                    S.dma(lambda e: e.dma_start(out=xl[s_ % 2][:], in_=xbuf[q_, st_ * 512 + s_ * 128:st_ * 512 + (s_ + 1) * 128, :]),
                          reads=[xd[q_][st_]], writes=[Bxl[s_ % 2]])
                load_sub(0)
                load_sub(1)
                xr = [t("xr%d" % i, [128, D], F32) for i in range(2)]; Bxr = [Buf(), Buf()]
                for seq in range(NSEQ):
                    S.pool(lambda e: e.memset(gh[:], 0.0), writes=[Bgh])
                    for st in range(NST):
                        T0 = st * 512
                        S.pool(lambda e: e.memset(ss[:, 0:4], 0.0), writes=[Bss])
                        for s in range(4):
                            S.act(lambda e, s=s: e.activation(junk[:], xl[s % 2][:], AF.Square, accum_out=ss[:, s:s + 1]), reads=[Bxl[s % 2]], writes=[Bjunk, Bss])
                            rstd_from_ss(ss[:, s:s + 1], rs[:, s:s + 1], D, Bss, Brs)
                            S.act(lambda e, s=s: e.activation(xn[s % 2][:], xl[s % 2][:], AF.Copy, scale=rs[:, s:s + 1]),
                                  reads=[Bxl[s % 2], Brs], writes=[Bxn[s % 2]])
                            load_sub((seq * NST + st) * 4 + s + 2)
                            for kh in range(2):
                                bk, Bb = bank("mm")
                                for kk in range(4):
                                    k = kh * 4 + kk
                                    S.pe(lambda e, s=s, k=k, kk=kk, bk=bk: e.matmul(bk[:, kk * 128:(kk + 1) * 128], xn[s % 2][:, k * 128:(k + 1) * 128],
                                                                                   ident[:], start=True, stop=True),
                                         reads=[Bxn[s % 2], Bconst], writes=[Bb])
                                for kk in range(4):
                                    k = kh * 4 + kk
                                    if kk % 2 == 0:
                                        S.act(lambda e, s=s, k=k, kk=kk, bk=bk: e.activation(hT[:, k, s * 128:(s + 1) * 128], bk[:, kk * 128:(kk + 1) * 128],
                                                                                          AF.Copy, scale=n2col[:, k:k + 1]), reads=[Bb, Bw], writes=[BhT])
                                    else:
                                        S.dve(lambda e, s=s, k=k, kk=kk, bk=bk: e.tensor_scalar(hT[:, k, s * 128:(s + 1) * 128], bk[:, kk * 128:(kk + 1) * 128],
                                                                                              n2col[:, k:k + 1], None, ALU.mult), reads=[Bb, Bw], writes=[BhT])
                        hi, ho = st % 2, (st + 1) % 2
                        for c in range(NFC):
                            bg, Bbg = bank("mm")
                            for k in range(8):
                                S.pe(lambda e, c=c, k=k, bg=bg: e.matmul(bg[:], wg[:, k, c * 128:(c + 1) * 128], hT[:, k, :], start=(k == 0), stop=(k == 7)),
                                     reads=[Bwg, BhT], writes=[Bbg])
                            bu, Bbu = bank("s")
                            for k in range(8):
                                S.pe(lambda e, c=c, k=k, bu=bu: e.matmul(bu[:], wu[:, k, c * 128:(c + 1) * 128], hT[:, k, :], start=(k == 0), stop=(k == 7)),
                                     reads=[Bwu, BhT], writes=[Bbu])
                            i = c % 2
                            S.act(lambda e, i=i, bg=bg: e.activation(gb[i][:, 2:514], bg[:], AF.Copy), reads=[Bbg], writes=[Bgb[i]])
                            S.pool(lambda e, i=i, c=c, hi=hi: e.tensor_copy(gb[i][:, 0:2], gh[:, hi, c, :]), reads=[Bgh], writes=[Bgb[i]])
                            S.pool(lambda e, i=i, c=c, ho=ho: e.tensor_copy(gh[:, ho, c, :], gb[i][:, 512:514]), reads=[Bgb[i]], writes=[Bgh])
                            S.act(lambda e, i=i, c=c: e.activation(tA[i][:], gb[i][:, 2:514], AF.Identity, scale=cw[:, 3 * c + 2:3 * c + 3], bias=cbias[:, c:c + 1]),
                                  reads=[Bgb[i], Bw], writes=[BtA[i]])
                            S.dve(lambda e, i=i, c=c: e.scalar_tensor_tensor(tB[i][:], gb[i][:, 1:513], cw[:, 3 * c + 1:3 * c + 2], tA[i][:], ALU.mult, ALU.add),
                                  reads=[Bgb[i], Bw, BtA[i]], writes=[BtB[i]])
                            S.dve(lambda e, i=i, c=c: e.scalar_tensor_tensor(tA[i][:], gb[i][:, 0:512], cw[:, 3 * c:3 * c + 1], tB[i][:], ALU.mult, ALU.add),
                                   reads=[Bgb[i], Bw, BtB[i]], writes=[BtA[i]])
                            S.act(lambda e, i=i: e.activation(tB[i][:], tA[i][:], AF.Silu), reads=[BtA[i]], writes=[BtB[i]])
                            S.dve(lambda e, i=i, c=c, bu=bu: e.tensor_tensor(aT[:, c, :], tB[i][:], bu[:], ALU.mult), reads=[BtB[i], Bbu], writes=[BaT])
                        for s in range(4):
                            i = s % 2
                            r0 = T0 + s * 128
                            S.dma(lambda e, i=i, r0=r0, seq=seq: e.dma_start(out=xr[i][:], in_=xbuf[seq, r0:r0 + 128, :]), reads=[xd[seq][st]], writes=[Bxr[i]])
                            for nh in range(2):
                                bk, Bb = bank("acc")
                                for c in range(NFC):
                                    S.pe(lambda e, s=s, nh=nh, c=c, bk=bk: e.matmul(bk[:], aT[:, c, s * 128:(s + 1) * 128], wd[:, c, nh * 512:(nh + 1) * 512],
                                                                                 start=(c == 0), stop=(c == NFC - 1)),
                                         reads=[BaT, Bwd], writes=[Bb])
                                S.dve(lambda e, i=i, nh=nh, bk=bk: e.tensor_tensor(xr[i][:, nh * 512:(nh + 1) * 512], xr[i][:, nh * 512:(nh + 1) * 512], bk[:], ALU.add),
                                      reads=[Bb, Bxr[i]], writes=[Bxr[i]])
                            if not last:
                                S.dma(lambda e, i=i, r0=r0, seq=seq: e.dma_start(out=xbuf[seq, r0:r0 + 128, :], in_=xr[i][:]), reads=[Bxr[i]], writes=[xd[seq][st]])
                            else:
                                S.dve(lambda e, s=s: e.memset(ss[:, 4 + s:5 + s], 0.0), writes=[Bss])
                                S.act(lambda e, i=i, s=s: e.activation(junk[:], xr[i][:], AF.Square, accum_out=ss[:, 4 + s:5 + s]), reads=[Bxr[i]], writes=[Bjunk, Bss])
                                rstd_from_ss(ss[:, 4 + s:5 + s], rs[:, 4 + s:5 + s], D, Bss, Brs)
                                S.dve(lambda e, i=i, s=s: e.scalar_tensor_tensor(xr[i][:], xr[i][:], rs[:, 4 + s:5 + s], nfb[:], ALU.mult, ALU.mult),
                                      reads=[Bxr[i], Brs, Bw], writes=[Bxr[i]])
                                out_dmas.append(S.dma(lambda e, i=i, r0=r0, seq=seq: e.dma_start(out=y_out[seq, r0:r0 + 128, :], in_=xr[i][:]),
                                                      reads=[Bxr[i]], writes=[xd[seq][st]]))

        for l in range(L):
            S.barrier()
            phase_M(l)
            if stop_after_M:
                break
            S.barrier()
            phase_F(l, l == L - 1)
        S.emit(final_wait_ops=out_dmas)
    return nc, dbg_out


def _consts():
    p = np.arange(128)[:, None].astype(np.float64)
    q = np.arange(128)[None, :].astype(np.float64)
    c = {}
    c["c_ident"] = np.eye(128, dtype=np.float32)
    tri = np.where(p <= q, 0.0, NEG)
    tri2 = np.where(p > q, 0.0, NEG)
    c["c_tri"] = np.concatenate([tri, tri2], axis=1).astype(np.float32)
    d1 = 30000.0 * (q - 16.0 * p)
    d0 = d1.copy()
    d0[0, :] = -1e9
    c["c_dcmp"] = np.concatenate([d0, d1], axis=1).astype(np.float32)
    ovl = np.zeros((128, 2, 64), np.float32)
    for j in range(2):
        for pp in range(128):
            s = 128 * j + pp
            if s == 0:
                continue
            n = s - 1
            for b in range(64):
                if (16 * n < 64 * b + 64) and (16 * n + 31 >= 64 * b):
                    ovl[pp, j, b] = 1.0
            ovl[pp, j, 0] = 1e6
    c["c_ovl"] = ovl.reshape(128, 128)
    F = np.zeros((128, 128), np.float32)
    for qq in range(128):
        cur = qq // 64
        for j in range(128):
            br = j - 62
            if br == cur:
                F[qq, j] = 3e6
            elif br == cur - 1:
                F[qq, j] = 2e6
            elif br > cur:
                F[qq, j] = -1e9
    c["c_F"] = F
    c["c_E"] = (np.arange(64)[:, None] == (np.arange(4096)[None, :] // 64)).astype(np.float32)
    cinv = np.zeros((128, 4, 16), np.float32)
    for gi, w in enumerate(POOL_W):
        for j in range(16):
            cinv[:, gi, j] = 1.0 / min(j + 1, w)
    c["c_cinv"] = cinv.reshape(128, 64)
    return c


def _prep_weights(inp, L):
    perm = list(range(0, 512))
    for r in range(4):
        perm += list(range(512 + r * 64, 512 + r * 64 + 64)) + list(range(512 + (4 + r) * 64, 512 + (4 + r) * 64 + 64))
    perm += list(range(1024, 1152)) + list(range(1152, 1280)) + list(range(1280, 1408)) + list(range(1536, 1664))
    perm += list(range(1408, 1536)) + list(range(1664, 1792)) + list(range(1792, 1816))
    perm = np.asarray(perm)
    out = {}
    out["w_in"] = np.ascontiguousarray(np.asarray(inp["w_in"])[:, :, perm])
    for k in ("w_out", "w_gate", "w_up", "w_down"):
        out[k] = np.ascontiguousarray(np.asarray(inp[k]))
    out["w_pool"] = np.ascontiguousarray(np.asarray(inp["w_pool"]).transpose(0, 2, 1, 3).reshape(L, 128, 512))
    w1 = np.stack([np.asarray(inp["cmp_w1_k"]), np.asarray(inp["cmp_w1_v"])], axis=1)
    out["cmp_w1"] = np.ascontiguousarray(w1.reshape(L, 2, 32, 64, 128).transpose(0, 1, 3, 2, 4).reshape(L, 2, 64, 32 * 128))
    out["cmp_w2"] = np.ascontiguousarray(np.stack([np.asarray(inp["cmp_w2_k"]), np.asarray(inp["cmp_w2_v"])], axis=1))
    pe = np.stack([np.asarray(inp["cmp_pe_k"]), np.asarray(inp["cmp_pe_v"])], axis=1)
    out["cmp_peT"] = np.ascontiguousarray(pe.transpose(0, 1, 3, 2))
    vec = np.zeros((L, 128, 112), np.float32)
    vec[:, :, 0:8] = np.asarray(inp["norm1"]).reshape(L, 8, 128).transpose(0, 2, 1)
    vec[:, :, 8:12] = np.asarray(inp["s_pool"]).reshape(L, 4, 128).transpose(0, 2, 1)
    vec[:, :, 12:16] = np.asarray(inp["norm_pool_out"]).reshape(L, 4, 128).transpose(0, 2, 1)
    vec[:, :, 16:24] = np.asarray(inp["norm2"]).reshape(L, 8, 128).transpose(0, 2, 1)
    cwv = np.asarray(inp["conv_w"]).reshape(L, 3, NFC, 128).transpose(0, 3, 2, 1)
    vec[:, :, 24:90] = cwv.reshape(L, 128, 66)
    vec[:, :, 90:112] = np.asarray(inp["conv_b"]).reshape(L, NFC, 128).transpose(0, 2, 1)
    out["vecs"] = vec
    out["nao"] = np.ascontiguousarray(np.asarray(inp["norm_attn_out"]).reshape(L, 1, 512))
    out["normf"] = np.ascontiguousarray(np.asarray(inp["norm_f"]).reshape(1, D))
    out.update(_consts())
    return {k: np.ascontiguousarray(v, dtype=np.float32) for k, v in out.items()}


_CACHE = {}


def kernel(**inputs):
    x = np.asarray(inputs["x"], dtype=np.float32)
    B, T, _ = x.shape
    L = np.asarray(inputs["w_in"]).shape[0]
    n_cores = 8
    nseq = B // n_cores
    key = (T, nseq, L)
    if key not in _CACHE:
        _CACHE[key] = build(T, nseq, L)[0]
    nc = _CACHE[key]
    shared = _prep_weights(inputs, L)
    in_maps = []
    for c in range(n_cores):
        m = dict(shared)
        m["x"] = np.ascontiguousarray(x[c * nseq:(c + 1) * nseq])
        in_maps.append(m)
    res = run_bass_kernel_spmd(nc, in_maps, core_ids=list(range(n_cores)))
    return np.concatenate([np.asarray(r["y"]) for r in res.results], axis=0).astype(np.float32)
```
